# Optimizing a Trainium2 kernel written in Bass

```python
import math
import jax, jax.numpy as jnp
from jax import lax
import numpy as np

D_MODEL = 1024
BATCH = 2
SEQ = 8192
DEPTH = 1

N_HEADS = 8
HEAD_DIM = 64
V_DIM = 2 * HEAD_DIM
QK_WIDTH = N_HEADS * 2 * HEAD_DIM
ATTN_WIDTH = N_HEADS * V_DIM
Q_BLOCK = 128
SUBLN_EPS = 1e-5
LRU_WIDTH = D_MODEL
LRU_BLOCKS = 16
LRU_BLOCK_W = LRU_WIDTH // LRU_BLOCKS
CONV_WIDTH = 4
LRU_C = 8.0
N_GROUPS = 4
EXPERTS_PER_GROUP = 8
N_EXPERTS = N_GROUPS * EXPERTS_PER_GROUP
TOP_K_INNER = 2
EXPERT_FF = 512
ROUTER_BIAS_SCALE = 0.01
PLE_DIM = 256
NORM_EPS = 1e-6

IN_SIZES = (QK_WIDTH, QK_WIDTH, ATTN_WIDTH, LRU_WIDTH, LRU_WIDTH, D_MODEL, D_MODEL)
IN_COLS = sum(IN_SIZES)
IN_SPLITS = tuple(int(s) for s in np.cumsum(IN_SIZES)[:-1])

kernel_name = "hybrid_diffattn_rglru_hiermoe_ple"


def rmsnorm(x, g, eps=NORM_EPS):
    xf = x.astype(jnp.float32)
    y = xf * lax.rsqrt(jnp.mean(xf * xf, axis=-1, keepdims=True) + eps)
    return (y * g.astype(jnp.float32)).astype(x.dtype)


def diff_attention(q, k, v, lam):
    B, S = q.shape[0], q.shape[1]
    n_blk = S // Q_BLOCK
    scale = HEAD_DIM ** -0.5
    qb = jnp.moveaxis(q.reshape(B, n_blk, Q_BLOCK, N_HEADS, 2, HEAD_DIM), 1, 0)
    kf = k.astype(jnp.float32)
    vf = v.astype(jnp.float32)
    kpos = jnp.arange(S)
    neg = jnp.finfo(jnp.float32).min

    def one_block(args):
        qi, blk = args
        s = jnp.einsum('bqhcd,bkhcd->bhcqk', qi.astype(jnp.float32), kf) * scale
        qpos = blk * Q_BLOCK + jnp.arange(Q_BLOCK)
        causal = kpos[None, :] <= qpos[:, None]
        s = jnp.where(causal, s, neg)
        pr = jax.nn.softmax(s, axis=-1)
        a = pr[:, :, 0] - lam * pr[:, :, 1]
        return jnp.einsum('bhqk,bkhe->bqhe', a, vf)

    o = lax.map(one_block, (qb, jnp.arange(n_blk)))
    return jnp.moveaxis(o, 0, 1).reshape(B, S, N_HEADS, V_DIM).astype(q.dtype)


def causal_depthwise_conv(x, w, b):
    S = x.shape[1]
    xp = jnp.pad(x, ((0, 0), (CONV_WIDTH - 1, 0), (0, 0)))
    y = b
    for j in range(CONV_WIDTH):
        y = y + xp[:, j:j + S] * w[j]
    return y


def rg_lru(x, w_rg, b_rg, w_ig, b_ig, lru_lambda):
    B, S, W = x.shape
    xb = x.reshape(B, S, LRU_BLOCKS, LRU_BLOCK_W)
    r = jax.nn.sigmoid(jnp.einsum('bsnw,nwv->bsnv', xb, w_rg) + b_rg).reshape(B, S, W)
    ig = jax.nn.sigmoid(jnp.einsum('bsnw,nwv->bsnv', xb, w_ig) + b_ig).reshape(B, S, W)
    log_a = -LRU_C * r.astype(jnp.float32) * jax.nn.softplus(-lru_lambda.astype(jnp.float32))
    a = jnp.exp(log_a)
    mult = jnp.sqrt(-jnp.expm1(2.0 * log_a))
    u = mult * (ig * x).astype(jnp.float32)

    def combine(left, right):
        a_l, b_l = left
        a_r, b_r = right
        return a_l * a_r, a_r * b_l + b_r

    _, h = lax.associative_scan(combine, (a, u), axis=1)
    return h.astype(x.dtype)


def hier_moe(h, w_rt_group, b_rt_group, w_rt_expert, b_rt_expert, w_e_gate, w_e_up, w_e_down):
    B, S, D = h.shape
    t = h.reshape(B * S, D)
    grp_logits = (t @ w_rt_group + b_rt_group).astype(jnp.float32)
    grp_prob = jax.nn.softmax(grp_logits, axis=-1)
    g_idx = jnp.argmax(grp_logits, axis=-1)
    g_w = jnp.take_along_axis(grp_prob, g_idx[:, None], axis=1)[:, 0]
    fine = (jnp.einsum('nd,gde->nge', t, w_rt_expert) + b_rt_expert).astype(jnp.float32)
    fine_sel = jnp.take_along_axis(fine, g_idx[:, None, None], axis=1)[:, 0]
    top_logit, top_idx = lax.top_k(fine_sel, TOP_K_INNER)
    top_w = jax.nn.softmax(top_logit, axis=-1)
    expert_id = g_idx[:, None] * EXPERTS_PER_GROUP + top_idx
    weights = g_w[:, None] * top_w
    combine = jnp.sum(jax.nn.one_hot(expert_id, N_EXPERTS, dtype=jnp.float32)
                      * weights[..., None], axis=1)

    def expert_step(acc, params):
        wg, wu, wd, c = params
        hid = jax.nn.silu(t @ wg) * (t @ wu)
        return acc + c[:, None] * (hid @ wd).astype(jnp.float32), None

    acc0 = jnp.zeros((B * S, D), jnp.float32)
    y, _ = lax.scan(expert_step, acc0, (w_e_gate, w_e_up, w_e_down, combine.T))
    return y.reshape(B, S, D).astype(h.dtype)


def setup_inputs(seed: int = 0) -> dict:
    key = jax.random.key(seed)
    ks = jax.random.split(key, 32)
    f32 = jnp.float32
    L, D = DEPTH, D_MODEL

    def nrm(k, shape, fan_in):
        return jax.random.normal(k, shape, f32) * (fan_in ** -0.5)

    def gain(k, shape):
        return 1.0 + 0.05 * jax.random.normal(k, shape, f32)

    u = jax.random.uniform(ks[16], (L, LRU_WIDTH), f32, 0.9, 0.999)
    s = u ** (1.0 / LRU_C)
    lru_lambda = jnp.log(s) - jnp.log1p(-s)
    return {
        "x": jax.random.normal(ks[0], (BATCH, SEQ, D), f32),
        "p": jax.random.normal(ks[1], (L, BATCH, SEQ, PLE_DIM), f32),
        "g_mix": gain(ks[2], (L, D)),
        "w_in": nrm(ks[3], (L, D, IN_COLS), D),
        "lam_q1": 0.1 * jax.random.normal(ks[4], (L, HEAD_DIM), f32),
        "lam_k1": 0.1 * jax.random.normal(ks[5], (L, HEAD_DIM), f32),
        "lam_q2": 0.1 * jax.random.normal(ks[6], (L, HEAD_DIM), f32),
        "lam_k2": 0.1 * jax.random.normal(ks[7], (L, HEAD_DIM), f32),
        "g_subln": gain(ks[8], (L, V_DIM)),
        "w_conv": nrm(ks[9], (L, CONV_WIDTH, LRU_WIDTH), CONV_WIDTH),
        "b_conv": 0.02 * jax.random.normal(ks[10], (L, LRU_WIDTH), f32),
        "w_rg": nrm(ks[11], (L, LRU_BLOCKS, LRU_BLOCK_W, LRU_BLOCK_W), LRU_BLOCK_W),
        "b_rg": 0.02 * jax.random.normal(ks[12], (L, LRU_BLOCKS, LRU_BLOCK_W), f32),
        "w_ig": nrm(ks[13], (L, LRU_BLOCKS, LRU_BLOCK_W, LRU_BLOCK_W), LRU_BLOCK_W),
        "b_ig": 0.02 * jax.random.normal(ks[14], (L, LRU_BLOCKS, LRU_BLOCK_W), f32),
        "lru_lambda": lru_lambda,
        "w_attn_br": nrm(ks[17], (L, ATTN_WIDTH, D), ATTN_WIDTH),
        "w_lru_br": nrm(ks[18], (L, LRU_WIDTH, D), LRU_WIDTH),
        "w_out": nrm(ks[19], (L, D, D), D),
        "g_moe": gain(ks[20], (L, D)),
        "w_rt_group": nrm(ks[21], (L, D, N_GROUPS), D),
        "b_rt_group": ROUTER_BIAS_SCALE * jax.random.normal(ks[22], (L, N_GROUPS), f32),
        "w_rt_expert": nrm(ks[23], (L, N_GROUPS, D, EXPERTS_PER_GROUP), D),
        "b_rt_expert": ROUTER_BIAS_SCALE * jax.random.normal(ks[24], (L, N_GROUPS, EXPERTS_PER_GROUP), f32),
        "w_e_gate": nrm(ks[25], (L, N_EXPERTS, D, EXPERT_FF), D),
        "w_e_up": nrm(ks[26], (L, N_EXPERTS, D, EXPERT_FF), D),
        "w_e_down": nrm(ks[27], (L, N_EXPERTS, EXPERT_FF, D), EXPERT_FF),
        "g_ple": gain(ks[28], (L, D)),
        "w_ple_gate": nrm(ks[29], (L, D, D), D),
        "w_ple_proj": nrm(ks[30], (L, PLE_DIM, D), PLE_DIM),
        "g_final": gain(ks[31], (D,)),
    }


def reference(x, p, g_mix, w_in, lam_q1, lam_k1, lam_q2, lam_k2, g_subln, w_conv, b_conv,
              w_rg, b_rg, w_ig, b_ig, lru_lambda, w_attn_br, w_lru_br, w_out, g_moe,
              w_rt_group, b_rt_group, w_rt_expert, b_rt_expert, w_e_gate, w_e_up, w_e_down,
              g_ple, w_ple_gate, w_ple_proj, g_final):
    B, S = x.shape[0], x.shape[1]
    for i in range(DEPTH):
        lam_init = 0.8 - 0.6 * math.exp(-0.3 * i)
        h = rmsnorm(x, g_mix[i])
        z = h @ w_in[i]
        q, k, v, xr, gr, ga, gb = jnp.split(z, IN_SPLITS, axis=-1)
        q = q.reshape(B, S, N_HEADS, 2, HEAD_DIM)
        k = k.reshape(B, S, N_HEADS, 2, HEAD_DIM)
        v = v.reshape(B, S, N_HEADS, V_DIM)
        lam = (jnp.exp(jnp.sum(lam_q1[i].astype(jnp.float32) * lam_k1[i].astype(jnp.float32)))
               - jnp.exp(jnp.sum(lam_q2[i].astype(jnp.float32) * lam_k2[i].astype(jnp.float32)))
               + lam_init)
        o = diff_attention(q, k, v, lam)
        o = rmsnorm(o, g_subln[i], SUBLN_EPS) * (1.0 - lam_init)
        attn_branch = o.reshape(B, S, ATTN_WIDTH) @ w_attn_br[i]
        xc = causal_depthwise_conv(xr, w_conv[i], b_conv[i])
        hr = rg_lru(xc, w_rg[i], b_rg[i], w_ig[i], b_ig[i], lru_lambda[i])
        lru_branch = (hr * jax.nn.gelu(gr)) @ w_lru_br[i]
        mixed = jax.nn.sigmoid(ga) * attn_branch + jax.nn.sigmoid(gb) * lru_branch
        x = x + mixed @ w_out[i]
        x = x + hier_moe(rmsnorm(x, g_moe[i]), w_rt_group[i], b_rt_group[i], w_rt_expert[i],
                         b_rt_expert[i], w_e_gate[i], w_e_up[i], w_e_down[i])
        gate = jax.nn.sigmoid(rmsnorm(x, g_ple[i]) @ w_ple_gate[i])
        x = x + gate * (p[i] @ w_ple_proj[i])
    return rmsnorm(x, g_final)
```

```python
import os
import numpy as np
import ml_dtypes
import concourse.bass as bass
import concourse.mybir as mybir
from concourse.bass_utils import run_bass_kernel_spmd

AF = mybir.ActivationFunctionType
ALU = mybir.AluOpType
AX = mybir.AxisListType
F32 = mybir.dt.float32
BF16 = mybir.dt.bfloat16

S = 8192
D = 1024
NT = 2048
NE = 32
FF = 512
NEG = 0.0


class Prog:
    ENGS = ("sp", "act", "pe", "dve", "pool")

    def __init__(self, nc):
        self.nc = nc
        self.ops = []
        self.lastw = {}
        self.readers = {}
        self._nbar = 0

    def op(self, eng, fn, r=(), w=(), dma=None):
        idx = len(self.ops)
        deps = set()
        for k in r:
            lw = self.lastw.get(k)
            if lw is not None:
                deps.add(lw)
        for k in w:
            lw = self.lastw.get(k)
            if lw is not None:
                deps.add(lw)
            rd = self.readers.get(k)
            if rd:
                deps.update(rd)
        for k in r:
            self.readers.setdefault(k, []).append(idx)
        for k in w:
            self.lastw[k] = idx
            self.readers[k] = []
        self.ops.append(dict(eng=eng, fn=fn, deps=deps, dma=dma))
        return idx

    def barrier(self):
        n = self._nbar
        self._nbar += 1
        last_dma = {}
        for i, o in enumerate(self.ops):
            if o["dma"] is not None:
                last_dma[o["dma"]] = i
        for e in ("act", "pe", "dve", "pool"):
            i = self.op(e, lambda eng: eng.drain(), w=[("bar", n, e)])
            self.ops[i]["deps"].update(last_dma.values())
        for e in self.ENGS:
            self.op(e, lambda eng: eng.nop(), r=[("bar", n, x) for x in ("act", "pe", "dve", "pool")])
        self.lastw = {}
        self.readers = {}

    def emit(self):
        nc = self.nc
        ops = self.ops

        def skip(od, o):
            return od["eng"] == "pe" and o["eng"] == "pe" and od["dma"] is None and o["dma"] is None

        needed = set()
        for i, o in enumerate(ops):
            for d in o["deps"]:
                if not skip(ops[d], o):
                    needed.add(d)
        eng_cnt = {e: 0 for e in self.ENGS}
        dma_cnt = {}
        dma_keys = []
        for i, o in enumerate(ops):
            if o["dma"] is not None:
                k = o["dma"]
                if k not in dma_cnt:
                    dma_cnt[k] = 0
                    dma_keys.append(k)
                dma_cnt[k] += 16
                o["ev"] = ("dma", k, dma_cnt[k])
            elif i in needed:
                eng_cnt[o["eng"]] += 1
                o["ev"] = ("eng", o["eng"], eng_cnt[o["eng"]])
            else:
                o["ev"] = None
        cur = {}
        for i, o in enumerate(ops):
            waits = {}
            for d in o["deps"]:
                od = ops[d]
                if skip(od, o):
                    continue
                ev = od["ev"]
                if ev[0] == "dma":
                    key = ("dma", ev[1])
                    val = cur[ev[1]]
                else:
                    key = ("eng", ev[1])
                    val = ev[2]
                if waits.get(key, 0) < val:
                    waits[key] = val
            o["waits"] = waits
            if o["dma"] is not None:
                cur[o["dma"]] = o["ev"][2]
        sems = {}
        for e in ("act", "pe", "dve", "pool"):
            sems[("eng", e)] = nc.alloc_semaphore("s_" + e)
        for n, k in enumerate(dma_keys):
            sems[("dma", k)] = nc.alloc_semaphore("d%d" % n)
        streams = {e: [] for e in self.ENGS}
        for o in ops:
            streams[o["eng"]].append(o)

        def run_stream(eng_obj, lst):
            waited = {}
            for o in lst:
                for key, val in o["waits"].items():
                    if waited.get(key, 0) >= val:
                        continue
                    waited[key] = val
                    eng_obj.wait_ge(sems[key], val)
                ins = o["fn"](eng_obj)
                ev = o["ev"]
                if ev is not None:
                    if ev[0] == "dma":
                        ins.then_inc(sems[("dma", ev[1])], 16)
                    else:
                        ins.then_inc(sems[("eng", ev[1])], 1)

        with nc.Block() as block:
            block.sync(lambda e: run_stream(e, streams["sp"]))
            block.scalar(lambda e: run_stream(e, streams["act"]))
            block.tensor(lambda e: run_stream(e, streams["pe"]))
            block.vector(lambda e: run_stream(e, streams["dve"]))
            block.gpsimd(lambda e: run_stream(e, streams["pool"]))


class Arena:
    BASE = 16512
    TOP = 229344

    def __init__(self, nc):
        self.nc = nc
        self.off = self.BASE
        self.n = 0

    def alloc(self, name, shape, dt):
        nb = int(np.prod(shape[1:])) * (4 if dt == F32 else 2)
        nb = (nb + 31) // 32 * 32
        assert self.off + nb <= self.TOP, (name, self.off, nb)
        self.n += 1
        t = self.nc.alloc_sbuf_tensor_at("%s_%d" % (name, self.n), list(shape), dt, offset=self.off)
        self.off += nb
        return t

    def mark(self):
        return self.off

    def reset(self, m):
        self.off = m


DEBUG = bool(int(os.environ.get("MK_DEBUG", "0")))
STOP_AFTER = os.environ.get("MK_STOP", "")


def build_program():
    nc = bass.Bass("TRN2", target_bir_lowering=False)
    p = Prog(nc)
    ar = Arena(nc)

    def din(name, shape, dt=F32):
        return nc.dram_tensor(name, list(shape), dt, kind="ExternalInput").ap()

    skind = "ExternalOutput" if DEBUG else "Internal"

    def dscr(name, shape, dt):
        return nc.dram_tensor(name, list(shape), dt, kind=skind).ap()

    xT_full = din("xT_full", [D, S])
    xT_own = din("xT_own", [D, NT])
    pT_own = din("pT_own", [256, NT])
    maskB_d = din("maskB", [128, 16, 512], BF16)
    sel_d = din("sel", [128, 4])
    w_in = din("w_in", [D, 7168])
    vec_d = din("vecs", [128, 8, 12])
    gsub_d = din("gsub", [128, 1])
    lamv_d = din("lamv", [128, 4, 64])
    brt_d = din("brt", [128, 36])
    wrg_d = din("wrg_bd", [128, 8, 128])
    wig_d = din("wig_bd", [128, 8, 128])
    wab_d = din("w_attn_br", [D, D])
    wlb_d = din("w_lru_br", [D, D])
    wout_d = din("w_out", [D, D])
    wrt_d = din("w_rt", [D, 36])
    weg_d = din("w_e_gate", [NE, D, FF])
    weu_d = din("w_e_up", [NE, D, FF])
    wed_d = din("w_e_down", [NE, FF, D])
    wpg_d = din("w_ple_gate", [D, D])
    wpp_d = din("w_ple_proj", [256, D])
    outT = nc.dram_tensor("outT", [D, NT], F32, kind="ExternalOutput").ap()

    KT_s = dscr("KT_s", [8, 128, S], BF16)
    XR_s = dscr("XR_s", [8, 128, S], BF16)
    V_s = dscr("V_s", [8, 128, 64, 128], BF16)
    HTO_s = dscr("HTO_s", [128, 8, NT], BF16)
    LRU_s = dscr("LRU_s", [128, 8, NT], BF16)
    OT_s = dscr("OT_s", [128, 8, NT], BF16)
    X1_s = dscr("X1_s", [128, 8, NT], F32)

    class Bank:
        def __init__(self, t, off):
            self.t, self.off = t, off

        def __getitem__(self, key):
            if not isinstance(key, tuple):
                key = (key, slice(None))
            pk, ck = key
            c0 = ck.start or 0
            c1 = 512 if ck.stop is None else ck.stop
            return self.t[pk, self.off + c0:self.off + c1]

    _p01 = [nc.alloc_psum_tensor("psb%d" % i, [128, 512], F32) for i in range(2)]
    big = [nc.alloc_psum_tensor("psbig%d" % i, [128, 1024], F32) for i in range(2)]
    _p67 = [nc.alloc_psum_tensor("psb%d" % i, [128, 512], F32) for i in (6, 7)]
    ps = _p01 + [Bank(big[0], 0), Bank(big[0], 512), Bank(big[1], 0), Bank(big[1], 512)] + _p67
    rr = [0]

    def nextbank(lo=1, hi=8):
        b = lo + rr[0] % (hi - lo)
        rr[0] += 1
        return b

    def PK(i):
        return ("ps", i)

    ones_bf = ar.alloc("ones_bf", [128, 128], BF16)
    ident_bf = ar.alloc("ident_bf", [128, 128], BF16)
    ident_f = ar.alloc("ident_f", [128, 128], F32)
    maskB = ar.alloc("maskB", [128, 16, 512], BF16)
    vec = ar.alloc("vec", [128, 8, 12], F32)
    gsub = ar.alloc("gsub", [128, 1], F32)
    lamv = ar.alloc("lamv", [128, 4, 64], F32)
    brt = ar.alloc("brt", [128, 36], F32)
    sel = ar.alloc("sel", [128, 4], F32)
    der = ar.alloc("der", [128, 8, 4], F32)
    sc = ar.alloc("sc", [128, 8], F32)
    lprod = ar.alloc("lprod", [128, 2, 64], F32)
    GM, GMOE, GPLE, GFIN, BCONV, BRG, BIG, LAM, WC0 = 0, 1, 2, 3, 4, 5, 6, 7, 8

    p.op("sp", lambda e: e.dma_start(out=maskB[:], in_=maskB_d), w=["maskB"], dma="c0")
    p.op("sp", lambda e: e.dma_start(out=vec[:], in_=vec_d), w=["vec"], dma="c1")
    p.op("sp", lambda e: e.dma_start(out=gsub[:], in_=gsub_d), w=["gsub"], dma="c1")
    p.op("sp", lambda e: e.dma_start(out=lamv[:], in_=lamv_d), w=["lamv"], dma="c1")
    p.op("sp", lambda e: e.dma_start(out=brt[:], in_=brt_d), w=["brt"], dma="c1")
    p.op("sp", lambda e: e.dma_start(out=sel[:], in_=sel_d), w=["sel"], dma="c1")
    p.op("pool", lambda e: e.memset(ident_f[:], 1.0), w=["ident_f"])
    p.op("pool", lambda e: e.affine_select(out=ident_f[:], in_=ident_f[:], pattern=[[1, 128]], compare_op=ALU.is_equal,
                                           fill=0.0, base=0, channel_multiplier=-1), r=["ident_f"], w=["ident_f"])
    p.op("dve", lambda e: e.tensor_copy(out=ident_bf[:], in_=ident_f[:]), r=["ident_f"], w=["ident_bf"])
    p.op("dve", lambda e: e.memset(ones_bf[:], 1.0), w=["ones_bf"])
    p.op("dve", lambda e: e.tensor_scalar(out=der[:, :, 0], in0=vec[:, :, BRG], scalar1=0.5, scalar2=None, op0=ALU.mult), r=["vec"], w=["der0"])
    p.op("dve", lambda e: e.tensor_scalar(out=der[:, :, 1], in0=vec[:, :, BIG], scalar1=0.5, scalar2=None, op0=ALU.mult), r=["vec"], w=["der1"])
    p.op("act", lambda e: e.activation(out=der[:, :, 3], in_=vec[:, :, LAM], func=AF.Exp, scale=-1.0), r=["vec"], w=["der3"])
    p.op("dve", lambda e: e.tensor_scalar(out=der[:, :, 2], in0=der[:, :, 3], scalar1=0.2, scalar2=None, op0=ALU.mult), r=["der3"], w=["der2"])
    for cst in (-0.25, 1.0 / 3.0, -0.5, 1.0):
        p.op("dve", lambda e, cst=cst: e.scalar_tensor_tensor(out=der[:, :, 2], in0=der[:, :, 2], scalar=cst, in1=der[:, :, 3], op0=ALU.add, op1=ALU.mult),
             r=["der2", "der3"], w=["der2"])
    p.op("dve", lambda e: e.tensor_scalar(out=der[:, :, 2], in0=der[:, :, 2], scalar1=-4.0, scalar2=None, op0=ALU.mult), r=["der2"], w=["der2"])
    p.op("dve", lambda e: e.tensor_tensor(out=lprod[:, 0, :], in0=lamv[:, 0, :], in1=lamv[:, 1, :], op=ALU.mult), r=["lamv"], w=["lprod"])
    p.op("dve", lambda e: e.tensor_tensor(out=lprod[:, 1, :], in0=lamv[:, 2, :], in1=lamv[:, 3, :], op=ALU.mult), r=["lamv", "lprod"], w=["lprod"])
    p.op("dve", lambda e: e.reduce_sum(out=sc[:, 2:4], in_=lprod[:], axis=AX.X), r=["lprod"], w=["sc23"])
    p.op("act", lambda e: e.activation(out=sc[:, 4:6], in_=sc[:, 2:4], func=AF.Exp), r=["sc23"], w=["sc45"])
    p.op("dve", lambda e: e.tensor_tensor(out=sc[:, 0:1], in0=sc[:, 5:6], in1=sc[:, 4:5], op=ALU.subtract), r=["sc45"], w=["sc0"])
    p.op("dve", lambda e: e.tensor_scalar(out=sc[:, 0:1], in0=sc[:, 0:1], scalar1=-0.2, scalar2=None, op0=ALU.add), r=["sc0"], w=["sc0"])
    p.op("dve", lambda e: e.tensor_scalar(out=sc[:, 1:2], in0=gsub[:], scalar1=0.8, scalar2=None, op0=ALU.mult), r=["gsub"], w=["sc1"])
    CONST_KEYS = ["maskB", "vec", "sel", "brt", "ident_f", "ident_bf", "ones_bf", "der0", "der1", "der2", "sc0", "sc1"]

    def consts_barrier():
        p.barrier()

    consts_barrier()
    gmark = ar.mark()

    def load_weight(dst, dst_c0, src2d, nk, col0, ncols, stg, piece):
        view = src2d.rearrange("(kc p) c -> p kc c", p=128)
        for c in range(0, ncols, piece):
            s = stg["i"] % 2
            stg["i"] += 1
            st = stg["t"][s]
            key = ("stg", stg["name"], s)
            p.op("sp", lambda e, st=st, c=c: e.dma_start(out=st[:, 0:nk, 0:piece], in_=view[:, :, col0 + c:col0 + c + piece]),
                 w=[key], dma=key)
            p.op("pool", lambda e, st=st, c=c: e.tensor_copy(out=dst[:, 0:nk, dst_c0 + c:dst_c0 + c + piece], in_=st[:, 0:nk, 0:piece]),
                 r=[key], w=[("w", id(dst), dst_c0 + c)])

    def wkeys(dst, c0, n, piece):
        return [("w", id(dst), c) for c in range(c0 // piece * piece, c0 + n, piece)]

    evq = [0]

    def evac(out_ap, in_ap, rk, wk):
        evq[0] += 1
        if evq[0] % 2:
            p.op("act", lambda e: e.copy(out=out_ap, in_=in_ap), r=rk, w=wk)
        else:
            p.op("dve", lambda e: e.tensor_copy(out=out_ap, in_=in_ap), r=rk, w=wk)

    def rms_stats(src, nkc, nblk, sq, t1, rstd, rk, tag, eps=1e-6, dim=1024.0):
        p.op("act", lambda e: e.activation(out=sq, in_=src, func=AF.Square), r=rk, w=[tag + "sq"])
        for kc in range(nkc):
            p.op("pe", lambda e, kc=kc: e.matmul(ps[0][:, 0:nblk], lhsT=ones_bf[:], rhs=sq[:, kc, :], start=(kc == 0), stop=(kc == nkc - 1)),
                 r=[tag + "sq"], w=[PK(0)])
        p.op("act", lambda e: e.activation(out=t1, in_=ps[0][:, 0:nblk], func=AF.Ln, bias=eps, scale=1.0 / dim), r=[PK(0)], w=[tag + "t1"])
        p.op("act", lambda e: e.activation(out=rstd, in_=t1, func=AF.Exp, scale=-0.5), r=[tag + "t1"], w=[tag + "rstd"])

    Wk = ar.alloc("Wk", [128, 8, 1024], BF16)
    Wv = ar.alloc("Wv", [128, 8, 1024], BF16)
    Wx = ar.alloc("Wx", [128, 8, 1024], BF16)
    stgA = {"t": [ar.alloc("stgA", [128, 8, 256], F32) for _ in range(2)], "i": 0, "name": "A"}
    load_weight(Wk, 0, w_in, 8, 1024, 1024, stgA, 256)
    load_weight(Wv, 0, w_in, 8, 2048, 1024, stgA, 256)
    load_weight(Wx, 0, w_in, 8, 3072, 1024, stgA, 256)
    xf = [ar.alloc("xf", [128, 8, 512], F32) for _ in range(2)]
    sq = ar.alloc("sq", [128, 8, 512], BF16)
    t1 = ar.alloc("t1", [128, 512], F32)
    rstd = ar.alloc("rstd", [128, 512], F32)
    hT = [ar.alloc("hT", [128, 8, 512], BF16) for _ in range(2)]
    kst = [ar.alloc("kst", [128, 8, 512], BF16) for _ in range(2)]
    xst = [ar.alloc("xst", [128, 8, 512], BF16) for _ in range(2)]
    vst = [ar.alloc("vst", [128, 4, 1024], BF16) for _ in range(2)]
    xTf_v = xT_full.rearrange("(kc p) t -> p kc t", p=128)
    xTo_v = xT_own.rearrange("(kc p) t -> p kc t", p=128)
    KT_v = KT_s.rearrange("h p t -> p h t")
    XR_v = XR_s.rearrange("h p t -> p h t")
    V_v = V_s.rearrange("h p t v -> p t h v")

    def norm_block(src_view, tb, s):
        p.op("sp", lambda e: e.dma_start(out=xf[s][:], in_=src_view[:, :, tb * 512:(tb + 1) * 512]), w=[("xf", s)], dma=("xf", s))
        rms_stats(xf[s][:], 8, 512, sq[:], t1[:], rstd[:], [("xf", s)], "A")
        for kc in range(8):
            p.op("dve", lambda e, kc=kc: e.scalar_tensor_tensor(out=hT[s][:, kc, :], in0=xf[s][:, kc, :], scalar=vec[:, kc, GM:GM + 1],
                                                              in1=rstd[:], op0=ALU.mult, op1=ALU.mult),
                 r=[("xf", s), "Arstd"], w=[("hT", s, kc)])

    def a1_compute(kind, tb, s):
        if kind == "own":
            p.op("pool", lambda e: e.dma_start(out=HTO_s[:, :, tb * 512:(tb + 1) * 512], in_=hT[s][:]),
                 r=[("hT", s, kc) for kc in range(8)], w=[("HTO", tb)], dma=("hTst", s))
            return
        for (W, st, keyn) in ((Wk, kst, "kst"), (Wx, xst, "xst")):
            for h in range(8):
                b = nextbank()
                for kc in range(8):
                    p.op("pe", lambda e, W=W, b=b, kc=kc, h=h: e.matmul(ps[b][:], lhsT=W[:, kc, h * 128:(h + 1) * 128], rhs=hT[s][:, kc, :],
                                                                   start=(kc == 0), stop=(kc == 7)),
                         r=[("hT", s, kc)] + wkeys(W, h * 128, 128, 256), w=[PK(b)])
                evac(st[s][:, h, :], ps[b][:], [PK(b)], [(keyn, s, h)])
        p.op("pool", lambda e: e.dma_start(out=KT_v[:, :, tb * 512:(tb + 1) * 512], in_=kst[s][:]), r=[("kst", s, h) for h in range(8)], w=[("KT", tb)], dma=("kst", s))
        p.op("pool", lambda e: e.dma_start(out=XR_v[:, :, tb * 512:(tb + 1) * 512], in_=xst[s][:]), r=[("xst", s, h) for h in range(8)], w=[("XR", tb)], dma=("xst", s))
        for tt in range(4):
            for half in range(2):
                b = nextbank()
                for kc in range(8):
                    p.op("pe", lambda e, b=b, kc=kc, tt=tt, half=half: e.matmul(ps[b][:], lhsT=hT[s][:, kc, tt * 128:(tt + 1) * 128],
                                                                           rhs=Wv[:, kc, half * 512:(half + 1) * 512], start=(kc == 0), stop=(kc == 7)),
                         r=[("hT", s, kc)] + wkeys(Wv, half * 512, 512, 256), w=[PK(b)])
                evac(vst[s][:, tt, half * 512:(half + 1) * 512], ps[b][:], [PK(b)], [("vst", s, tt, half)])
        for tt in range(4):
            p.op("pool", lambda e, tt=tt: e.dma_start(out=V_v[:, tb * 4 + tt], in_=vst[s][:, tt, :].rearrange("p (h v) -> p h v", h=8)),
                 r=[("vst", s, tt, half) for half in range(2)], w=[("V", tb, tt)], dma=("vst", s))

    jobs = [("own", tb) for tb in range(4)] + [("full", tb) for tb in range(16)]
    norm_block(xTo_v, 0, 0)
    for n, (kind, tb) in enumerate(jobs):
        if n + 1 < len(jobs):
            k2, tb2 = jobs[n + 1]
            norm_block(xTo_v if k2 == "own" else xTf_v, tb2, (n + 1) % 2)
        a1_compute(kind, tb, n % 2)
    p.barrier()
    ar.reset(gmark)
    if STOP_AFTER == "A1":
        return finish(nc, p, outT, ar)

    Wgr = ar.alloc("Wgr", [128, 8, 1024], BF16)
    Wrg = ar.alloc("Wrg", [128, 8, 128], BF16)
    Wig = ar.alloc("Wig", [128, 8, 128], BF16)
    stgB = {"t": [ar.alloc("stgB", [128, 8, 128], F32) for _ in range(2)], "i": 0, "name": "B"}
    load_weight(Wgr, 0, w_in, 8, 4096, 1024, stgB, 128)
    for (Wt, src, nm) in ((Wrg, wrg_d, "wrg"), (Wig, wig_d, "wig")):
        s = stgB["i"] % 2
        stgB["i"] += 1
        st = stgB["t"][s]
        key = ("stg", "B", s)
        p.op("sp", lambda e, st=st, src=src: e.dma_start(out=st[:, :, 0:128], in_=src), w=[key], dma=key)
        p.op("pool", lambda e, st=st, Wt=Wt: e.tensor_copy(out=Wt[:], in_=st[:, :, 0:128]), r=[key], w=[nm])
    Dm = ar.alloc("Dm", [128, 8, 4, 128], BF16)
    for cc in range(8):
        for j in range(4):
            p.op("dve", lambda e, cc=cc, j=j: e.tensor_scalar(out=Dm[:, cc, j, :], in0=ident_f[:], scalar1=vec[:, cc, WC0 + j:WC0 + j + 1], scalar2=None, op0=ALU.mult),
                 w=[("Dm", cc, j)])
    xrb = [ar.alloc("xrb", [128, 4, 2048], BF16) for _ in range(2)]
    xcb = [ar.alloc("xcb", [128, 2048], BF16) for _ in range(2)]
    rt = [ar.alloc("rt", [128, 2048], F32) for _ in range(2)]
    it = [ar.alloc("it", [128, 2048], BF16) for _ in range(2)]
    aa = [ar.alloc("aa", [128, 2048], F32) for _ in range(2)]
    uu = ar.alloc("uu", [128, 2048], F32)
    hh = ar.alloc("hh", [128, 2048], F32)
    hTo2 = [ar.alloc("hTo", [128, 8, 512], BF16) for _ in range(2)]
    carry = ar.alloc("carry", [128, 1], F32)
    hsel = ar.alloc("hsel", [128, 512], F32)
    gz2 = [ar.alloc("gz", [128, 512], F32) for _ in range(2)]
    gw = ar.alloc("gw", [128, 512], F32)
    gt2 = [ar.alloc("gt", [128, 512], F32) for _ in range(2)]
    lst = [ar.alloc("lst", [128, 512], BF16) for _ in range(2)]

    def a2_stage1(cc, tb2, s):
        K = lambda nm: (nm, s)
        t0 = tb2 * 2048
        hTo = hTo2[s]
        gt = gt2[s]
        p.op("sp", lambda e: e.dma_start(out=hTo[:], in_=HTO_s[:, :, tb2 * 512:(tb2 + 1) * 512]), w=[("hTo", s)], dma=("hTo", s))
        for j in range(4):
            if tb2 == 0 and j < 3:
                p.op("dve", lambda e, j=j: e.memset(xrb[s][:, j, 0:3 - j], 0.0), w=[("xrb", s, j)])
                p.op("sp", lambda e, j=j: e.dma_start(out=xrb[s][:, j, 3 - j:2048], in_=XR_s[cc, :, 0:2048 - (3 - j)]), w=[("xrb", s, j)], dma=("xrb", s))
            else:
                p.op("sp", lambda e, j=j: e.dma_start(out=xrb[s][:, j, :], in_=XR_s[cc, :, t0 - 3 + j:t0 - 3 + j + 2048]), w=[("xrb", s, j)], dma=("xrb", s))
        cb = []
        for nb in range(4):
            sl = slice(nb * 512, (nb + 1) * 512)
            b = nextbank()
            cb.append(b)
            for j in range(4):
                p.op("pe", lambda e, b=b, j=j, sl=sl: e.matmul(ps[b][:], lhsT=Dm[:, cc, j, :], rhs=xrb[s][:, j, sl], start=(j == 0), stop=(j == 3)),
                     r=[("xrb", s, j), ("Dm", cc, j)], w=[PK(b)])
        for nb in range(4):
            sl = slice(nb * 512, (nb + 1) * 512)
            b = cb[nb]
            if nb % 2 == 0:
                p.op("act", lambda e, b=b, sl=sl: e.activation(out=xcb[s][:, sl], in_=ps[b][:], func=AF.Identity, bias=vec[:, cc, BCONV:BCONV + 1], scale=1.0),
                     r=[PK(b)], w=[("xcb", s, nb)])
            else:
                p.op("dve", lambda e, b=b, sl=sl: e.tensor_scalar(out=xcb[s][:, sl], in0=ps[b][:], scalar1=vec[:, cc, BCONV:BCONV + 1], scalar2=None, op0=ALU.add),
                     r=[PK(b)], w=[("xcb", s, nb)])
        bq = nextbank()
        for kc in range(8):
            p.op("pe", lambda e, kc=kc: e.matmul(ps[bq][:], lhsT=Wgr[:, kc, cc * 128:(cc + 1) * 128], rhs=hTo[:, kc, :], start=(kc == 0), stop=(kc == 7)),
                 r=[("hTo", s)] + wkeys(Wgr, cc * 128, 128, 128), w=[PK(bq)])
        gz = gz2[s]
        p.op("act", lambda e: e.copy(out=gz[:], in_=ps[bq][:]), r=[PK(bq)], w=[("gz", s)])
        p.op("act", lambda e: e.activation(out=gw[:], in_=ps[bq][:], func=AF.Square), r=[PK(bq)], w=["gw"])
        p.op("pool", lambda e: e.tensor_scalar(out=gw[:], in0=gw[:], scalar1=0.044715, scalar2=1.0, op0=ALU.mult, op1=ALU.add), r=["gw"], w=["gw"])
        p.op("pool", lambda e: e.tensor_tensor(out=gw[:], in0=gw[:], in1=gz[:], op=ALU.mult), r=["gw", ("gz", s)], w=["gw"])
        for nb in range(4):
            sl = slice(nb * 512, (nb + 1) * 512)
            for (Wt, nm, dst, hb) in ((Wrg, "wrg", rt, 0), (Wig, "wig", it, 1)):
                b2 = nextbank()
                p.op("pe", lambda e, b2=b2, Wt=Wt, sl=sl: e.matmul(ps[b2][:], lhsT=Wt[:, cc, :], rhs=xcb[s][:, sl], start=True, stop=True),
                     r=[("xcb", s, nb), nm], w=[PK(b2)])
                p.op("act", lambda e, b2=b2, dst=dst, sl=sl, hb=hb: e.activation(out=dst[s][:, sl], in_=ps[b2][:], func=AF.Tanh, bias=der[:, cc, hb:hb + 1], scale=0.5),
                     r=[PK(b2)], w=[("g", s, hb, nb)])
        p.op("act", lambda e: e.activation(out=gt[:], in_=gw[:], func=AF.Tanh, scale=0.7978845608028654), r=["gw"], w=[("gt", s)])
        gk = [("g", s, 0, nb) for nb in range(4)]
        p.op("act", lambda e: e.activation(out=aa[s][:], in_=rt[s][:], func=AF.Exp, bias=der[:, cc, 2:3], scale=der[:, cc, 2:3]), r=gk, w=[K("aa")])
        p.op("pool", lambda e: e.tensor_tensor(out=rt[s][:], in0=aa[s][:], in1=aa[s][:], op=ALU.mult), r=[K("aa")], w=gk)
        p.op("act", lambda e: e.activation(out=rt[s][:], in_=rt[s][:], func=AF.Sqrt, bias=0.25, scale=-0.25), r=gk, w=gk)

    def a2_stage2(cc, tb2, s, n):
        K = lambda nm: (nm, s)
        gk = [("g", s, 0, nb) for nb in range(4)]
        ik = [("g", s, 1, nb) for nb in range(4)]
        xk = [("xcb", s, nb) for nb in range(4)]
        gz = gz2[s]
        gt = gt2[s]
        p.op("dve", lambda e: e.scalar_tensor_tensor(out=gt[:], in0=gt[:], scalar=1.0, in1=gz[:], op0=ALU.add, op1=ALU.mult), r=[("gt", s), ("gz", s)], w=[("gt", s)])
        p.op("dve", lambda e: e.scalar_tensor_tensor(out=uu[:], in0=it[s][:], scalar=1.0, in1=xcb[s][:], op0=ALU.add, op1=ALU.mult), r=ik + xk, w=["uu"])
        p.op("dve", lambda e: e.tensor_tensor(out=uu[:], in0=uu[:], in1=rt[s][:], op=ALU.mult), r=["uu"] + gk, w=["uu"])
        if tb2 == 0:
            p.op("dve", lambda e: e.tensor_tensor_scan(out=hh[:], data0=aa[s][:], data1=uu[:], initial=0.0, op0=ALU.mult, op1=ALU.add), r=[K("aa"), "uu"], w=["hh"])
        else:
            p.op("dve", lambda e: e.tensor_tensor_scan(out=hh[:], data0=aa[s][:], data1=uu[:], initial=carry[:, 0:1], op0=ALU.mult, op1=ALU.add),
                 r=[K("aa"), "uu", "carry"], w=["hh"])
        p.op("dve", lambda e: e.tensor_copy(out=carry[:], in_=hh[:, 2047:2048]), r=["hh"], w=["carry"])
        h4 = hh[:].rearrange("p (i r q) -> p i r q", i=4, r=4)
        hs3 = hsel[:].rearrange("p (i q) -> p i q", i=4)
        p.op("dve", lambda e: e.tensor_scalar(out=hs3, in0=h4[:, :, 0, :], scalar1=sel[:, 0:1], scalar2=None, op0=ALU.mult), r=["hh"], w=["hsel"])
        for r_ in range(1, 4):
            p.op("dve", lambda e, r_=r_: e.scalar_tensor_tensor(out=hs3, in0=h4[:, :, r_, :], scalar=sel[:, r_:r_ + 1], in1=hs3, op0=ALU.mult, op1=ALU.add),
                 r=["hh", "hsel"], w=["hsel"])
        gt = gt2[s]
        ls = lst[n % 2]
        lk = ("lst", n % 2)
        p.op("dve", lambda e: e.scalar_tensor_tensor(out=ls[:], in0=gt[:], scalar=0.5, in1=hsel[:], op0=ALU.mult, op1=ALU.mult), r=[("gt", s), "hsel"], w=[lk])
        p.op("pool", lambda e: e.dma_start(out=LRU_s[:, cc, tb2 * 512:(tb2 + 1) * 512], in_=ls[:]), r=[lk], w=[("LRU", cc, tb2)], dma=lk)

    blocks = [(cc, tb2) for cc in range(8) for tb2 in range(4)]
    a2_stage1(blocks[0][0], blocks[0][1], 0)
    for n, (cc, tb2) in enumerate(blocks):
        if n + 1 < len(blocks):
            a2_stage1(blocks[n + 1][0], blocks[n + 1][1], (n + 1) % 2)
        a2_stage2(cc, tb2, n % 2, n)
    p.barrier()
    ar.reset(gmark)
    if STOP_AFTER == "A2":
        return finish(nc, p, outT, ar)

    Wq = ar.alloc("Wq", [128, 8, 1024], BF16)
    stgC = {"t": [ar.alloc("stgC", [128, 8, 256], F32) for _ in range(2)], "i": 0, "name": "C"}
    load_weight(Wq, 0, w_in, 8, 0, 1024, stgC, 256)
    hTa = ar.alloc("hTa", [128, 8, NT], BF16)
    p.op("sp", lambda e: e.dma_start(out=hTa[:], in_=HTO_s), w=["hTa"], dma="hTa")
    ktb = [ar.alloc("ktb", [128, S], BF16) for _ in range(2)]
    vtb = [ar.alloc("vtb", [128, 64, 128], BF16) for _ in range(2)]
    qT = [ar.alloc("qT", [128, NT], BF16) for _ in range(2)]
    NPT = 6
    pT = [ar.alloc("pT", [128, 1024], BF16) for _ in range(NPT)]
    rl2 = [ar.alloc("rl2", [128, 512], F32) for _ in range(2)]
    acc0 = ar.alloc("acc0", [128, 512], F32)
    acchi = ar.alloc("acchi", [128, 512], BF16)
    acclo = ar.alloc("acclo", [128, 512], BF16)
    oc = [ar.alloc("oc", [128, 512], F32) for _ in range(2)]
    rl = ar.alloc("rl", [128, 512], F32)
    oo = ar.alloc("oo", [128, 512], F32)
    osq = ar.alloc("osq", [128, 512], BF16)
    ot1 = ar.alloc("ot1", [128, 512], F32)
    ors = ar.alloc("ors", [128, 512], F32)
    ost = [ar.alloc("ost", [128, 512], BF16) for _ in range(2)]
    OB = (6, 7)
    LBS = (0, 1)
    LB, QB = 0, 1
    npt = 0
    nst = 0
    for h in range(8):
        hs = h % 2
        p.op("sp", lambda e, hs=hs, h=h: e.dma_start(out=ktb[hs][:], in_=KT_s[h]), w=[("ktb", hs)], dma=("ktb", hs))
        p.op("sp", lambda e, hs=hs, h=h: e.dma_start(out=vtb[hs][:], in_=V_s[h]), w=[("vtb", hs)], dma=("vtb", hs))
        for tb in range(4):
            b = QB
            for kc in range(8):
                p.op("pe", lambda e, b=b, kc=kc, h=h, tb=tb: e.matmul(ps[b][:], lhsT=Wq[:, kc, h * 128:(h + 1) * 128], rhs=hTa[:, kc, tb * 512:(tb + 1) * 512],
                                                                 start=(kc == 0), stop=(kc == 7)),
                     r=["hTa"] + wkeys(Wq, h * 128, 128, 256), w=[PK(b)])
            evac(qT[hs][:, tb * 512:(tb + 1) * 512], ps[b][:], [PK(b)], [("qT", hs, tb)])
        for g in range(4):
            nkt = 16 * g + 16

            def col0(kt, g=g):
                return 0 if kt < 16 * g else 128 * ((kt - 16 * g) // 4)

            def s_mm(kt, g=g, hs=hs):
                sb = kt % 2
                c0 = col0(kt)
                for c in range(2):
                    p.op("pe", lambda e, c=c: e.matmul(big[sb][:, c * 512 + c0:(c + 1) * 512], lhsT=ktb[hs][64 * c:64 * c + 64, kt * 128:(kt + 1) * 128],
                                                       rhs=qT[hs][64 * c:64 * c + 64, g * 512 + c0:(g + 1) * 512], start=True, stop=True),
                         r=[("ktb", hs), ("qT", hs, g)], w=[PK(2 + 2 * sb + c)])
            s_mm(0)
            s_mm(1)
            for kt in range(nkt):
                sb = kt % 2
                pt = pT[npt % NPT]
                pk = ("pT", npt % NPT)
                npt += 1
                c0 = col0(kt)
                if c0 == 0:
                    p.op("act", lambda e, sb=sb, pt=pt: e.activation(out=pt[:], in_=big[sb][:], func=AF.Exp, scale=0.125), r=[PK(2 + 2 * sb), PK(3 + 2 * sb)], w=[(pk, 0), (pk, 1)])
                else:
                    p.op("act", lambda e, sb=sb, pt=pt, c0=c0: e.activation(out=pt[:].rearrange("p (c q) -> p c q", c=2)[:, :, c0:512],
                                                                           in_=big[sb][:].rearrange("p (c q) -> p c q", c=2)[:, :, c0:512], func=AF.Exp, scale=0.125),
                         r=[PK(2 + 2 * sb), PK(3 + 2 * sb)], w=[(pk, 0), (pk, 1)])
                if kt >= 16 * g:
                    tau = kt - 16 * g
                    p.op("dve", lambda e, pt=pt, tau=tau, c0=c0: e.tensor_tensor(out=pt[:, c0:512], in0=pt[:, c0:512], in1=maskB[:, tau, c0:512], op=ALU.mult), r=[(pk, 0)], w=[(pk, 0)])
                    p.op("pool", lambda e, pt=pt, tau=tau, c0=c0: e.tensor_tensor(out=pt[:, 512 + c0:1024], in0=pt[:, 512 + c0:1024], in1=maskB[:, tau, c0:512], op=ALU.mult), r=[(pk, 1)], w=[(pk, 1)])
                if kt + 2 < nkt:
                    s_mm(kt + 2)
                for c in range(2):
                    p.op("pe", lambda e, pt=pt, kt=kt, hs=hs, nkt=nkt, c=c, c0=c0: e.matmul(ps[OB[c]][:, c0:512], lhsT=vtb[hs][:, kt, :], rhs=pt[:, c * 512 + c0:(c + 1) * 512], start=(kt == 0), stop=(kt == nkt - 1)),
                         r=[(pk, c), ("vtb", hs)], w=[PK(OB[c])])
                    if c == 1:
                        p.op("pe", lambda e, pt=pt, kt=kt, nkt=nkt, c=c, c0=c0: e.matmul(ps[LBS[c]][:, c0:512], lhsT=ones_bf[:], rhs=pt[:, c * 512 + c0:(c + 1) * 512], start=(kt == 0), stop=(kt == nkt - 1)),
                             r=[(pk, c)], w=[PK(LBS[c])])
                    elif kt == 0:
                        p.op("dve", lambda e, pt=pt: e.tensor_copy(out=acc0[:], in_=pt[:, 0:512]), r=[(pk, 0)], w=["acc0"])
                    else:
                        p.op("dve", lambda e, pt=pt, c0=c0: e.tensor_tensor(out=acc0[:, c0:512], in0=acc0[:, c0:512], in1=pt[:, c0:512], op=ALU.add), r=[(pk, 0), "acc0"], w=["acc0"])
            p.op("act", lambda e: e.copy(out=acchi[:], in_=acc0[:]), r=["acc0"], w=["acchi"])
            p.op("dve", lambda e: e.tensor_tensor(out=acclo[:], in0=acc0[:], in1=acchi[:], op=ALU.subtract), r=["acc0", "acchi"], w=["acclo"])
            p.op("pe", lambda e: e.matmul(ps[LBS[0]][:], lhsT=ones_bf[:], rhs=acchi[:], start=True, stop=False), r=["acchi"], w=[PK(LBS[0])])
            p.op("pe", lambda e: e.matmul(ps[LBS[0]][:], lhsT=ones_bf[:], rhs=acclo[:], start=False, stop=True), r=["acclo"], w=[PK(LBS[0])])
            for c in range(2):
                p.op("act", lambda e, lb=LBS[c], c=c: e.activation(out=rl2[c][:], in_=ps[lb][:], func=AF.Ln), r=[PK(LBS[c])], w=[("rl", c)])
                p.op("act", lambda e, c=c: e.activation(out=rl2[c][:], in_=rl2[c][:], func=AF.Exp, scale=-1.0), r=[("rl", c)], w=[("rl", c)])
                p.op("dve", lambda e, c=c: e.tensor_tensor(out=oc[c][:], in0=ps[OB[c]][:], in1=rl2[c][:], op=ALU.mult), r=[PK(OB[c]), ("rl", c)], w=[("oc", c)])
            p.op("dve", lambda e: e.scalar_tensor_tensor(out=oo[:], in0=oc[1][:], scalar=sc[:, 0:1], in1=oc[0][:], op0=ALU.mult, op1=ALU.add),
                 r=[("oc", 0), ("oc", 1)], w=["oo"])
            p.op("act", lambda e: e.activation(out=osq[:], in_=oo[:], func=AF.Square), r=["oo"], w=["osq"])
            p.op("pe", lambda e: e.matmul(ps[LB][:], lhsT=ones_bf[:], rhs=osq[:], start=True, stop=True), r=["osq"], w=[PK(LB)])
            p.op("act", lambda e: e.activation(out=ot1[:], in_=ps[LB][:], func=AF.Ln, bias=1e-5, scale=1.0 / 128.0), r=[PK(LB)], w=["ot1"])
            p.op("act", lambda e: e.activation(out=ors[:], in_=ot1[:], func=AF.Exp, scale=-0.5), r=["ot1"], w=["ors"])
            os_ = ost[nst % 2]
            ok = ("ost", nst % 2)
            nst += 1
            p.op("dve", lambda e, os_=os_: e.scalar_tensor_tensor(out=os_[:], in0=oo[:], scalar=sc[:, 1:2], in1=ors[:], op0=ALU.mult, op1=ALU.mult), r=["oo", "ors"], w=[ok])
            p.op("pool", lambda e, os_=os_, h=h, g=g: e.dma_start(out=OT_s[:, h, g * 512:(g + 1) * 512], in_=os_[:]), r=[ok], w=[("OT", h, g)], dma=ok)
    p.barrier()
    ar.reset(gmark)
    if STOP_AFTER == "A3":
        return finish(nc, p, outT, ar)

    tT = ar.alloc("tT", [128, 8, NT], BF16)
    b2mark = ar.mark()
    mixed = ar.alloc("mixed", [128, 8, NT], BF16)
    bmark = ar.mark()
    Wga = ar.alloc("Wga", [128, 8, 1024], BF16)
    Wgb = ar.alloc("Wgb", [128, 8, 1024], BF16)
    Wab = ar.alloc("Wab", [128, 8, 1024], BF16)
    Wlb = ar.alloc("Wlb", [128, 8, 1024], BF16)
    stgD = {"t": [ar.alloc("stgD", [128, 8, 128], F32) for _ in range(2)], "i": 0, "name": "D"}
    load_weight(Wga, 0, w_in, 8, 5120, 1024, stgD, 128)
    load_weight(Wgb, 0, w_in, 8, 6144, 1024, stgD, 128)
    load_weight(Wab, 0, wab_d, 8, 0, 1024, stgD, 128)
    load_weight(Wlb, 0, wlb_d, 8, 0, 1024, stgD, 128)
    hb_ = [ar.alloc("hb", [128, 8, 512], BF16)] * 2
    ob_ = [ar.alloc("ob", [128, 8, 512], BF16)] * 2
    lb_ = [ar.alloc("lb", [128, 8, 512], BF16)] * 2
    sga = ar.alloc("sga", [128, 512], F32)
    sgb = ar.alloc("sgb", [128, 512], F32)
    m1 = ar.alloc("m1", [128, 512], F32)
    m2 = ar.alloc("m2", [128, 512], F32)
    for tb in range(4):
        s = 0
        sl = slice(tb * 512, (tb + 1) * 512)
        p.op("sp", lambda e, s=s, sl=sl: e.dma_start(out=hb_[s][:], in_=HTO_s[:, :, sl]), w=[("hb", s)], dma=("hb", s))
        p.op("sp", lambda e, s=s, sl=sl: e.dma_start(out=ob_[s][:], in_=OT_s[:, :, sl]), w=[("ob", s)], dma=("ob", s))
        p.op("sp", lambda e, s=s, sl=sl: e.dma_start(out=lb_[s][:], in_=LRU_s[:, :, sl]), w=[("lb", s)], dma=("lb", s))
        for fc in range(8):
            banks = [nextbank() for _ in range(4)]
            for (W, src, sk, b) in ((Wga, hb_, "hb", banks[0]), (Wgb, hb_, "hb", banks[1]), (Wab, ob_, "ob", banks[2]), (Wlb, lb_, "lb", banks[3])):
                for kc in range(8):
                    p.op("pe", lambda e, W=W, src=src, b=b, kc=kc, fc=fc, s=s: e.matmul(ps[b][:], lhsT=W[:, kc, fc * 128:(fc + 1) * 128], rhs=src[s][:, kc, :],
                                                                                   start=(kc == 0), stop=(kc == 7)),
                         r=[(sk, s)] + wkeys(W, fc * 128, 128, 128), w=[PK(b)])
            p.op("act", lambda e, b=banks[0]: e.activation(out=sga[:], in_=ps[b][:], func=AF.Sigmoid), r=[PK(banks[0])], w=["sga"])
            p.op("act", lambda e, b=banks[1]: e.activation(out=sgb[:], in_=ps[b][:], func=AF.Sigmoid), r=[PK(banks[1])], w=["sgb"])
            p.op("dve", lambda e, b=banks[2]: e.tensor_tensor(out=m1[:], in0=ps[b][:], in1=sga[:], op=ALU.mult), r=[PK(banks[2]), "sga"], w=["m1"])
            p.op("dve", lambda e, b=banks[3]: e.tensor_tensor(out=m2[:], in0=ps[b][:], in1=sgb[:], op=ALU.mult), r=[PK(banks[3]), "sgb"], w=["m2"])
            p.op("pool", lambda e, fc=fc, sl=sl: e.tensor_tensor(out=mixed[:, fc, sl], in0=m1[:], in1=m2[:], op=ALU.add), r=["m1", "m2"], w=[("mixed", tb)])
    p.barrier()
    ar.reset(bmark)

    Wo = ar.alloc("Wo", [128, 8, 1024], BF16)
    stgE = {"t": [ar.alloc("stgE", [128, 8, 256], F32) for _ in range(2)], "i": 0, "name": "E"}
    load_weight(Wo, 0, wout_d, 8, 0, 1024, stgE, 256)
    xo = [ar.alloc("xo", [128, 8, 512], F32) for _ in range(2)]
    sq2 = ar.alloc("sq2", [128, 8, 512], BF16)
    t1b = ar.alloc("t1b", [128, 512], F32)
    rstdb = ar.alloc("rstdb", [128, 512], F32)
    for tb in range(4):
        s = tb % 2
        sl = slice(tb * 512, (tb + 1) * 512)
        xk = [("xo", s, fc) for fc in range(8)]
        p.op("sp", lambda e, s=s, sl=sl: e.dma_start(out=xo[s][:], in_=xTo_v[:, :, sl]), w=xk, dma=("xo", s))
        for fc in range(8):
            b = nextbank()
            for kc in range(8):
                p.op("pe", lambda e, b=b, kc=kc, fc=fc, sl=sl: e.matmul(ps[b][:], lhsT=Wo[:, kc, fc * 128:(fc + 1) * 128], rhs=mixed[:, kc, sl], start=(kc == 0), stop=(kc == 7)),
                     r=wkeys(Wo, fc * 128, 128, 256), w=[PK(b)])
            p.op("dve", lambda e, b=b, fc=fc, s=s: e.tensor_tensor(out=xo[s][:, fc, :], in0=ps[b][:], in1=xo[s][:, fc, :], op=ALU.add),
                 r=[PK(b), ("xo", s, fc)], w=[("xo", s, fc)])
        rms_stats(xo[s][:], 8, 512, sq2[:], t1b[:], rstdb[:], xk, "B")
        for kc in range(8):
            p.op("dve", lambda e, kc=kc, sl=sl, s=s: e.scalar_tensor_tensor(out=tT[:, kc, sl], in0=xo[s][:, kc, :], scalar=vec[:, kc, GMOE:GMOE + 1], in1=rstdb[:],
                                                                      op0=ALU.mult, op1=ALU.mult), r=[("xo", s, kc), "Brstd"], w=[("tT", tb, kc)])
        p.op("pool", lambda e, sl=sl, s=s: e.dma_start(out=X1_s[:, :, sl], in_=xo[s][:]), r=xk, w=[("X1", tb)], dma=("xo", s))
    p.barrier()
    ar.reset(b2mark)
    if STOP_AFTER == "B1":
        return finish(nc, p, outT, ar)

    yy = ar.alloc("yy", [128, 16, 1024], F32)
    b3mark = ar.mark()
    Wr = ar.alloc("Wr", [128, 8, 36], BF16)
    wrs = ar.alloc("wrs", [128, 8, 36], F32)
    p.op("sp", lambda e: e.dma_start(out=wrs[:], in_=wrt_d.rearrange("(kc p) c -> p kc c", p=128)), w=["wrs"], dma="wrs")
    p.op("pool", lambda e: e.tensor_copy(out=Wr[:], in_=wrs[:]), r=["wrs"], w=["Wr"])
    lg = ar.alloc("lg", [128, 16, 36], F32)
    comb = ar.alloc("comb", [128, 16, 32], F32)
    gmx = ar.alloc("gmx", [128, 16, 1], F32)
    goh = ar.alloc("goh", [128, 16, 4], F32)
    gex = ar.alloc("gex", [128, 16, 4], F32)
    gsm = ar.alloc("gsm", [128, 16, 1], F32)
    gw_ = ar.alloc("gw_", [128, 16, 1], F32)
    fsel = ar.alloc("fsel", [128, 16, 8], F32)
    ftmp = ar.alloc("ftmp", [128, 16, 8], F32)
    m1_ = ar.alloc("m1_", [128, 16, 1], F32)
    m2_ = ar.alloc("m2_", [128, 16, 1], F32)
    oh1 = ar.alloc("oh1", [128, 16, 8], F32)
    oh2 = ar.alloc("oh2", [128, 16, 8], F32)
    w12 = ar.alloc("w12", [128, 16, 2], F32)
    wexp = ar.alloc("wexp", [128, 16, 8], F32)
    for tt in range(16):
        b = nextbank()
        for kc in range(8):
            p.op("pe", lambda e, b=b, kc=kc, tt=tt: e.matmul(ps[b][:, 0:36], lhsT=tT[:, kc, tt * 128:(tt + 1) * 128], rhs=Wr[:, kc, :], start=(kc == 0), stop=(kc == 7)),
                 r=["Wr"], w=[PK(b)])
        p.op("dve", lambda e, b=b, tt=tt: e.tensor_tensor(out=lg[:, tt, :], in0=ps[b][:, 0:36], in1=brt[:], op=ALU.add), r=[PK(b)], w=["lg"])
    p.op("dve", lambda e: e.tensor_reduce(out=gmx[:], in_=lg[:, :, 0:4], axis=AX.X, op=ALU.max), r=["lg"], w=["gmx"])
    p.op("dve", lambda e: e.tensor_tensor(out=goh[:], in0=lg[:, :, 0:4], in1=gmx[:].to_broadcast([128, 16, 4]), op=ALU.is_equal), r=["lg", "gmx"], w=["goh"])
    p.op("dve", lambda e: e.tensor_tensor(out=gex[:], in0=lg[:, :, 0:4], in1=gmx[:].to_broadcast([128, 16, 4]), op=ALU.subtract), r=["lg", "gmx"], w=["gex"])
    p.op("act", lambda e: e.activation(out=gex[:], in_=gex[:], func=AF.Exp), r=["gex"], w=["gex"])
    p.op("dve", lambda e: e.tensor_reduce(out=gsm[:], in_=gex[:], axis=AX.X, op=ALU.add), r=["gex"], w=["gsm"])
    p.op("dve", lambda e: e.reciprocal(out=gw_[:], in_=gsm[:]), r=["gsm"], w=["gw_"])
    for g in range(4):
        fin = lg[:, :, 4 + 8 * g:12 + 8 * g]
        if g == 0:
            p.op("dve", lambda e, fin=fin: e.tensor_tensor(out=fsel[:], in0=fin, in1=goh[:, :, 0:1].to_broadcast([128, 16, 8]), op=ALU.mult), r=["lg", "goh"], w=["fsel"])
        else:
            p.op("dve", lambda e, fin=fin, g=g: e.tensor_tensor(out=ftmp[:], in0=fin, in1=goh[:, :, g:g + 1].to_broadcast([128, 16, 8]), op=ALU.mult), r=["lg", "goh"], w=["ftmp"])
            p.op("dve", lambda e: e.tensor_tensor(out=fsel[:], in0=fsel[:], in1=ftmp[:], op=ALU.add), r=["fsel", "ftmp"], w=["fsel"])
    p.op("dve", lambda e: e.tensor_reduce(out=m1_[:], in_=fsel[:], axis=AX.X, op=ALU.max), r=["fsel"], w=["m1_"])
    p.op("dve", lambda e: e.tensor_tensor(out=oh1[:], in0=fsel[:], in1=m1_[:].to_broadcast([128, 16, 8]), op=ALU.is_equal), r=["fsel", "m1_"], w=["oh1"])
    p.op("dve", lambda e: e.scalar_tensor_tensor(out=ftmp[:], in0=oh1[:], scalar=-1e30, in1=fsel[:], op0=ALU.mult, op1=ALU.add), r=["oh1", "fsel"], w=["ftmp"])
    p.op("dve", lambda e: e.tensor_reduce(out=m2_[:], in_=ftmp[:], axis=AX.X, op=ALU.max), r=["ftmp"], w=["m2_"])
    p.op("dve", lambda e: e.tensor_tensor(out=oh2[:], in0=ftmp[:], in1=m2_[:].to_broadcast([128, 16, 8]), op=ALU.is_equal), r=["ftmp", "m2_"], w=["oh2"])
    p.op("dve", lambda e: e.tensor_tensor(out=w12[:, :, 0:1], in0=m2_[:], in1=m1_[:], op=ALU.subtract), r=["m1_", "m2_"], w=["w12"])
    p.op("act", lambda e: e.activation(out=w12[:, :, 0:1], in_=w12[:, :, 0:1], func=AF.Exp), r=["w12"], w=["w12"])
    p.op("dve", lambda e: e.tensor_scalar(out=w12[:, :, 0:1], in0=w12[:, :, 0:1], scalar1=1.0, scalar2=None, op0=ALU.add), r=["w12"], w=["w12"])
    p.op("dve", lambda e: e.reciprocal(out=w12[:, :, 0:1], in_=w12[:, :, 0:1]), r=["w12"], w=["w12"])
    p.op("dve", lambda e: e.tensor_tensor(out=w12[:, :, 0:1], in0=w12[:, :, 0:1], in1=gw_[:], op=ALU.mult), r=["w12", "gw_"], w=["w12"])
    p.op("dve", lambda e: e.tensor_tensor(out=w12[:, :, 1:2], in0=gw_[:], in1=w12[:, :, 0:1], op=ALU.subtract), r=["w12", "gw_"], w=["w12"])
    p.op("dve", lambda e: e.tensor_tensor(out=wexp[:], in0=oh1[:], in1=w12[:, :, 0:1].to_broadcast([128, 16, 8]), op=ALU.mult), r=["oh1", "w12"], w=["wexp"])
    p.op("dve", lambda e: e.tensor_tensor(out=ftmp[:], in0=oh2[:], in1=w12[:, :, 1:2].to_broadcast([128, 16, 8]), op=ALU.mult), r=["oh2", "w12"], w=["ftmp"])
    p.op("dve", lambda e: e.tensor_tensor(out=wexp[:], in0=wexp[:], in1=ftmp[:], op=ALU.add), r=["wexp", "ftmp"], w=["wexp"])
    for g in range(4):
        p.op("dve", lambda e, g=g: e.tensor_tensor(out=comb[:, :, 8 * g:8 * g + 8], in0=wexp[:], in1=goh[:, :, g:g + 1].to_broadcast([128, 16, 8]), op=ALU.mult),
             r=["wexp", "goh"], w=["comb"])
    p.op("dve", lambda e: e.memset(yy[:], 0.0), w=[("yy", tt_, hf_) for tt_ in range(16) for hf_ in range(2)])
    Wg_ = [ar.alloc("Wg_", [128, 8, 512], BF16) for _ in range(2)]
    Wu_ = [ar.alloc("Wu_", [128, 8, 512], BF16) for _ in range(2)]
    Wd_ = [ar.alloc("Wd_", [128, 4, 1024], BF16) for _ in range(2)]
    stgF = {"t": [ar.alloc("stgF", [128, 8, 128], F32) for _ in range(2)], "i": 0, "name": "F"}
    hid = ar.alloc("hid", [128, 4, NT], BF16)
    sg_ = [ar.alloc("sg_", [128, 512], F32) for _ in range(2)]
    nsg = 0
    for ex in range(NE):
        s = ex % 2
        load_weight(Wg_[s], 0, weg_d[ex], 8, 0, 512, stgF, 128)
        load_weight(Wu_[s], 0, weu_d[ex], 8, 0, 512, stgF, 128)
        viewd = wed_d[ex].rearrange("(kc p) c -> p kc c", p=128)
        for c in range(0, 1024, 256):
            s2 = stgF["i"] % 2
            stgF["i"] += 1
            st = stgF["t"][s2]
            key = ("stg", "F", s2)
            stv = st[:].rearrange("p k c -> p (k c)").rearrange("p (k c) -> p k c", k=4)
            p.op("sp", lambda e, stv=stv, c=c, viewd=viewd: e.dma_start(out=stv, in_=viewd[:, :, c:c + 256]), w=[key], dma=key)
            p.op("pool", lambda e, stv=stv, c=c, s=s: e.tensor_copy(out=Wd_[s][:, :, c:c + 256], in_=stv), r=[key], w=[("w", id(Wd_[s]), c)])
        for fc in range(4):
            for tb in range(4):
                sl = slice(tb * 512, (tb + 1) * 512)
                bg = nextbank()
                bu = nextbank()
                for (W, b) in ((Wg_[s], bg), (Wu_[s], bu)):
                    for kc in range(8):
                        p.op("pe", lambda e, W=W, b=b, kc=kc, fc=fc, sl=sl: e.matmul(ps[b][:], lhsT=W[:, kc, fc * 128:(fc + 1) * 128], rhs=tT[:, kc, sl], start=(kc == 0), stop=(kc == 7)),
                             r=wkeys(W, fc * 128, 128, 128), w=[PK(b)])
                sg = sg_[nsg % 2]
                sk = ("sg_", nsg % 2)
                nsg += 1
                p.op("act", lambda e, sg=sg, bg=bg: e.activation(out=sg[:], in_=ps[bg][:], func=AF.Silu), r=[PK(bg)], w=[sk])
                p.op("dve", lambda e, sg=sg, bu=bu, fc=fc, sl=sl: e.tensor_tensor(out=hid[:, fc, sl], in0=ps[bu][:], in1=sg[:], op=ALU.mult), r=[PK(bu), sk], w=[("hid", fc, tb)])
        for tt in range(16):
            for half in range(2):
                b = nextbank()
                for fc in range(4):
                    p.op("pe", lambda e, b=b, fc=fc, tt=tt, half=half, s=s: e.matmul(ps[b][:], lhsT=hid[:, fc, tt * 128:(tt + 1) * 128], rhs=Wd_[s][:, fc, half * 512:(half + 1) * 512],
                                                                                start=(fc == 0), stop=(fc == 3)),
                         r=[("hid", fc, tt // 4)] + wkeys(Wd_[s], half * 512, 512, 256), w=[PK(b)])
                ysl = yy[:, tt, half * 512:(half + 1) * 512]
                p.op("dve", lambda e, b=b, ysl=ysl, tt=tt, ex=ex: e.scalar_tensor_tensor(out=ysl, in0=ps[b][:], scalar=comb[:, tt, ex:ex + 1], in1=ysl, op0=ALU.mult, op1=ALU.add),
                     r=[PK(b), "comb", ("yy", tt, half)], w=[("yy", tt, half)])
    p.barrier()
    ar.reset(b3mark)

    Wpg = ar.alloc("Wpg", [128, 8, 1024], BF16)
    Wpp = ar.alloc("Wpp", [128, 2, 1024], BF16)
    stgG = {"t": [ar.alloc("stgG", [128, 8, 128], F32) for _ in range(2)], "i": 0, "name": "G"}
    load_weight(Wpg, 0, wpg_d, 8, 0, 1024, stgG, 128)
    load_weight(Wpp, 0, wpp_d, 2, 0, 1024, stgG, 128)
    pf = ar.alloc("pf", [128, 2, 512], F32)
    pb = ar.alloc("pb", [128, 2, 512], BF16)
    sq3 = ar.alloc("sq3", [128, 8, 512], BF16)
    t1c = ar.alloc("t1c", [128, 512], F32)
    rstdc = ar.alloc("rstdc", [128, 512], F32)
    t3 = ar.alloc("t3", [128, 8, 512], BF16)
    gte = ar.alloc("gte", [128, 512], F32)
    x1b = [ar.alloc("x1b", [128, 8, 512], F32) for _ in range(2)]
    pT_v = pT_own.rearrange("(kc p) t -> p kc t", p=128)
    outT_v = outT.rearrange("(kc p) t -> p kc t", p=128)
    for tb in range(4):
        s = tb % 2
        sl = slice(tb * 512, (tb + 1) * 512)
        xk = [("xb", s, fc) for fc in range(8)]
        p.op("sp", lambda e, s=s, sl=sl: e.dma_start(out=x1b[s][:], in_=X1_s[:, :, sl]), w=xk, dma=("x1b", s))
        p.op("sp", lambda e, sl=sl: e.dma_start(out=pf[:], in_=pT_v[:, :, sl]), w=["pf"], dma="pf")
        p.op("pool", lambda e: e.tensor_copy(out=pb[:], in_=pf[:]), r=["pf"], w=["pb"])
        for fc in range(8):
            b = nextbank()
            for q in range(4):
                tt = tb * 4 + q
                p.op("pe", lambda e, b=b, q=q, tt=tt, fc=fc: e.transpose(out=ps[b][:, q * 128:(q + 1) * 128], in_=yy[:, tt, fc * 128:(fc + 1) * 128], identity=ident_f[:]),
                     r=[], w=[PK(b)])
            p.op("dve", lambda e, b=b, fc=fc, s=s: e.tensor_tensor(out=x1b[s][:, fc, :], in0=ps[b][:], in1=x1b[s][:, fc, :], op=ALU.add), r=[PK(b), ("xb", s, fc)], w=[("xb", s, fc)])
        rms_stats(x1b[s][:], 8, 512, sq3[:], t1c[:], rstdc[:], xk, "C")
        for kc in range(8):
            p.op("dve", lambda e, kc=kc, s=s: e.scalar_tensor_tensor(out=t3[:, kc, :], in0=x1b[s][:, kc, :], scalar=vec[:, kc, GPLE:GPLE + 1], in1=rstdc[:], op0=ALU.mult, op1=ALU.mult),
                 r=["Crstd", ("xb", s, kc)], w=[("t3", kc)])
        for fc in range(8):
            bg = nextbank()
            bp = nextbank()
            for kc in range(8):
                p.op("pe", lambda e, bg=bg, kc=kc, fc=fc: e.matmul(ps[bg][:], lhsT=Wpg[:, kc, fc * 128:(fc + 1) * 128], rhs=t3[:, kc, :], start=(kc == 0), stop=(kc == 7)),
                     r=[("t3", kc)] + wkeys(Wpg, fc * 128, 128, 128), w=[PK(bg)])
            for kc in range(2):
                p.op("pe", lambda e, bp=bp, kc=kc, fc=fc: e.matmul(ps[bp][:], lhsT=Wpp[:, kc, fc * 128:(fc + 1) * 128], rhs=pb[:, kc, :], start=(kc == 0), stop=(kc == 1)),
                     r=["pb"] + wkeys(Wpp, fc * 128, 128, 128), w=[PK(bp)])
            p.op("act", lambda e, bg=bg: e.activation(out=gte[:], in_=ps[bg][:], func=AF.Sigmoid), r=[PK(bg)], w=["gte"])
            p.op("dve", lambda e, bp=bp: e.tensor_tensor(out=gte[:], in0=ps[bp][:], in1=gte[:], op=ALU.mult), r=[PK(bp), "gte"], w=["gte"])
            p.op("pool", lambda e, fc=fc, s=s: e.tensor_tensor(out=x1b[s][:, fc, :], in0=gte[:], in1=x1b[s][:, fc, :], op=ALU.add), r=["gte", ("xb", s, fc)], w=[("xb", s, fc)])
        rms_stats(x1b[s][:], 8, 512, sq3[:], t1c[:], rstdc[:], xk, "C")
        for kc in range(8):
            p.op("dve", lambda e, kc=kc, s=s: e.scalar_tensor_tensor(out=x1b[s][:, kc, :], in0=x1b[s][:, kc, :], scalar=vec[:, kc, GFIN:GFIN + 1], in1=rstdc[:], op0=ALU.mult, op1=ALU.mult),
                 r=[("xb", s, kc), "Crstd"], w=[("xb", s, kc)])
        p.op("sp", lambda e, s=s, sl=sl: e.dma_start(out=outT_v[:, :, sl], in_=x1b[s][:]), r=xk, w=[("out", tb)], dma=("x1b", s))
    return finish(nc, p, outT, ar)


def finish(nc, p, outT, ar):
    p.barrier()
    p.emit()
    return nc


def _mask_for(r):
    m = np.zeros((128, 16, 512), np.float32)
    ki = np.arange(128)[:, None]
    qi = np.arange(128)[None, :]
    tri = np.where(ki > qi, 0.0, 1.0).astype(np.float32)
    m[:] = 1.0
    for tau in range(16):
        for mq in range(4):
            tq = 4 * mq + r
            if tau > tq:
                m[:, tau, mq * 128:(mq + 1) * 128] = 0.0
            elif tau == tq:
                m[:, tau, mq * 128:(mq + 1) * 128] = tri
    return m.astype(ml_dtypes.bfloat16)


def _chunked(v):
    return np.ascontiguousarray(np.asarray(v, np.float32).reshape(8, 128).T)


def prepare_inputs(x, p, g_mix, w_in, lam_q1, lam_k1, lam_q2, lam_k2, g_subln, w_conv, b_conv,
                   w_rg, b_rg, w_ig, b_ig, lru_lambda, w_attn_br, w_lru_br, w_out, g_moe,
                   w_rt_group, b_rt_group, w_rt_expert, b_rt_expert, w_e_gate, w_e_up, w_e_down,
                   g_ple, w_ple_gate, w_ple_proj, g_final):
    f = lambda a: np.asarray(a, np.float32)
    x = f(x); p = f(p)
    vecs = np.zeros((128, 8, 12), np.float32)
    vecs[:, :, 0] = _chunked(f(g_mix)[0]); vecs[:, :, 1] = _chunked(f(g_moe)[0]); vecs[:, :, 2] = _chunked(f(g_ple)[0])
    vecs[:, :, 3] = _chunked(f(g_final)); vecs[:, :, 4] = _chunked(f(b_conv)[0]); vecs[:, :, 5] = _chunked(f(b_rg)[0].reshape(-1))
    vecs[:, :, 6] = _chunked(f(b_ig)[0].reshape(-1)); vecs[:, :, 7] = _chunked(f(lru_lambda)[0])
    for j in range(4):
        vecs[:, :, 8 + j] = _chunked(f(w_conv)[0, j])

    def blockdiag(w):
        w = f(w)[0]
        o = np.zeros((128, 8, 128), np.float32)
        for n in range(16):
            cc, hf = n // 2, n % 2
            o[hf * 64:(hf + 1) * 64, cc, hf * 64:(hf + 1) * 64] = w[n]
        return o

    w_rt = np.concatenate([f(w_rt_group)[0]] + [f(w_rt_expert)[0, g] for g in range(4)], axis=1)
    b_rt = np.concatenate([f(b_rt_group)[0], f(b_rt_expert)[0].reshape(-1)])[None, :]
    common = dict(
        w_in=np.ascontiguousarray(f(w_in)[0]),
        vecs=vecs,
        gsub=np.ascontiguousarray(f(g_subln)[0][:, None]),
        lamv=np.ascontiguousarray(np.broadcast_to(np.stack([f(lam_q1)[0], f(lam_k1)[0], f(lam_q2)[0], f(lam_k2)[0]])[None], (128, 4, 64))),
        brt=np.ascontiguousarray(np.broadcast_to(b_rt, (128, 36))),
        wrg_bd=blockdiag(w_rg), wig_bd=blockdiag(w_ig),
        w_attn_br=np.ascontiguousarray(f(w_attn_br)[0]), w_lru_br=np.ascontiguousarray(f(w_lru_br)[0]), w_out=np.ascontiguousarray(f(w_out)[0]),
        w_rt=np.ascontiguousarray(w_rt),
        w_e_gate=np.ascontiguousarray(f(w_e_gate)[0]), w_e_up=np.ascontiguousarray(f(w_e_up)[0]), w_e_down=np.ascontiguousarray(f(w_e_down)[0]),
        w_ple_gate=np.ascontiguousarray(f(w_ple_gate)[0]), w_ple_proj=np.ascontiguousarray(f(w_ple_proj)[0]),
    )
    in_maps = []
    toks = []
    for c in range(8):
        b, r = c // 4, c % 4
        own = (np.arange(16)[:, None] * 4 + r) * 128 + np.arange(128)[None, :]
        own = own.reshape(-1)
        toks.append((b, own))
        selv = np.zeros((128, 4), np.float32)
        selv[:, r] = 1.0
        m = dict(common)
        m.update(
            xT_full=np.ascontiguousarray(x[b].T),
            xT_own=np.ascontiguousarray(x[b][own].T),
            pT_own=np.ascontiguousarray(p[0, b][own].T),
            maskB=_mask_for(r),
            sel=selv,
        )
        in_maps.append(m)
    return in_maps, toks


_NC_CACHE = {}


def kernel(**inputs):
    in_maps, toks = prepare_inputs(**inputs)
    if "nc" not in _NC_CACHE:
        _NC_CACHE["nc"] = build_program()
    nc = _NC_CACHE["nc"]
    res = run_bass_kernel_spmd(nc, in_maps, core_ids=list(range(8)))
    out = np.zeros((2, S, D), np.float32)
    for c in range(8):
        b, own = toks[c]
        out[b, own, :] = res.results[c]["outT"].T
    if DEBUG:
        kernel.last = res.results
    return out
```

```python
import os
import numpy as np
import ml_dtypes
import concourse.bass as bass
import concourse.mybir as mybir
from concourse.bass_utils import run_bass_kernel_spmd

AF = mybir.ActivationFunctionType
ALU = mybir.AluOpType
AX = mybir.AxisListType
F32 = mybir.dt.float32
BF16 = mybir.dt.bfloat16

S = 8192
D = 1024
NT = 2048
NE = 32
FF = 512
NEG = 0.0


class Prog:
    ENGS = ("sp", "act", "pe", "dve", "pool")

    def __init__(self, nc):
        self.nc = nc
        self.ops = []
        self.lastw = {}
        self.readers = {}
        self._nbar = 0

    def op(self, eng, fn, r=(), w=(), dma=None):
        idx = len(self.ops)
        deps = set()
        for k in r:
            lw = self.lastw.get(k)
            if lw is not None:
                deps.add(lw)
        for k in w:
            lw = self.lastw.get(k)
            if lw is not None:
                deps.add(lw)
            rd = self.readers.get(k)
            if rd:
                deps.update(rd)
        for k in r:
            self.readers.setdefault(k, []).append(idx)
        for k in w:
            self.lastw[k] = idx
            self.readers[k] = []
        self.ops.append(dict(eng=eng, fn=fn, deps=deps, dma=dma))
        return idx

    def barrier(self):
        n = self._nbar
        self._nbar += 1
        last_dma = {}
        for i, o in enumerate(self.ops):
            if o["dma"] is not None:
                last_dma[o["dma"]] = i
        for e in ("act", "pe", "dve", "pool"):
            i = self.op(e, lambda eng: eng.drain(), w=[("bar", n, e)])
            self.ops[i]["deps"].update(last_dma.values())
        for e in self.ENGS:
            self.op(e, lambda eng: eng.nop(), r=[("bar", n, x) for x in ("act", "pe", "dve", "pool")])
        self.lastw = {}
        self.readers = {}

    def emit(self):
        nc = self.nc
        ops = self.ops

        def skip(od, o):
            return od["eng"] == "pe" and o["eng"] == "pe" and od["dma"] is None and o["dma"] is None

        needed = set()
        for i, o in enumerate(ops):
            for d in o["deps"]:
                if not skip(ops[d], o):
                    needed.add(d)
        eng_cnt = {e: 0 for e in self.ENGS}
        dma_cnt = {}
        dma_keys = []
        for i, o in enumerate(ops):
            if o["dma"] is not None:
                k = o["dma"]
                if k not in dma_cnt:
                    dma_cnt[k] = 0
                    dma_keys.append(k)
                dma_cnt[k] += 16
                o["ev"] = ("dma", k, dma_cnt[k])
            elif i in needed:
                eng_cnt[o["eng"]] += 1
                o["ev"] = ("eng", o["eng"], eng_cnt[o["eng"]])
            else:
                o["ev"] = None
        cur = {}
        for i, o in enumerate(ops):
            waits = {}
            for d in o["deps"]:
                od = ops[d]
                if skip(od, o):
                    continue
                ev = od["ev"]
                if ev[0] == "dma":
                    key = ("dma", ev[1])
                    val = cur[ev[1]]
                else:
                    key = ("eng", ev[1])
                    val = ev[2]
                if waits.get(key, 0) < val:
                    waits[key] = val
            o["waits"] = waits
            if o["dma"] is not None:
                cur[o["dma"]] = o["ev"][2]
        sems = {}
        for e in ("act", "pe", "dve", "pool"):
            sems[("eng", e)] = nc.alloc_semaphore("s_" + e)
        for n, k in enumerate(dma_keys):
            sems[("dma", k)] = nc.alloc_semaphore("d%d" % n)
        streams = {e: [] for e in self.ENGS}
        for o in ops:
            streams[o["eng"]].append(o)

        def run_stream(eng_obj, lst):
            waited = {}
            for o in lst:
                for key, val in o["waits"].items():
                    if waited.get(key, 0) >= val:
                        continue
                    waited[key] = val
                    eng_obj.wait_ge(sems[key], val)
                ins = o["fn"](eng_obj)
                ev = o["ev"]
                if ev is not None:
                    if ev[0] == "dma":
                        ins.then_inc(sems[("dma", ev[1])], 16)
                    else:
                        ins.then_inc(sems[("eng", ev[1])], 1)

        with nc.Block() as block:
            block.sync(lambda e: run_stream(e, streams["sp"]))
            block.scalar(lambda e: run_stream(e, streams["act"]))
            block.tensor(lambda e: run_stream(e, streams["pe"]))
            block.vector(lambda e: run_stream(e, streams["dve"]))
            block.gpsimd(lambda e: run_stream(e, streams["pool"]))


class Arena:
    BASE = 16512
    TOP = 229344

    def __init__(self, nc):
        self.nc = nc
        self.off = self.BASE
        self.n = 0

    def alloc(self, name, shape, dt):
        nb = int(np.prod(shape[1:])) * (4 if dt == F32 else 2)
        nb = (nb + 31) // 32 * 32
        assert self.off + nb <= self.TOP, (name, self.off, nb)
        self.n += 1
        t = self.nc.alloc_sbuf_tensor_at("%s_%d" % (name, self.n), list(shape), dt, offset=self.off)
        self.off += nb
        return t

    def mark(self):
        return self.off

    def reset(self, m):
        self.off = m


DEBUG = bool(int(os.environ.get("MK_DEBUG", "0")))
STOP_AFTER = os.environ.get("MK_STOP", "")


def build_program():
    nc = bass.Bass("TRN2", target_bir_lowering=False)
    p = Prog(nc)
    ar = Arena(nc)

    def din(name, shape, dt=F32):
        return nc.dram_tensor(name, list(shape), dt, kind="ExternalInput").ap()

    skind = "ExternalOutput" if DEBUG else "Internal"

    def dscr(name, shape, dt):
        return nc.dram_tensor(name, list(shape), dt, kind=skind).ap()

    xT_full = din("xT_full", [D, S])
    xT_own = din("xT_own", [D, NT])
    pT_own = din("pT_own", [256, NT])
    maskB_d = din("maskB", [128, 16, 512], BF16)
    sel_d = din("sel", [128, 4])
    w_in = din("w_in", [D, 7168])
    vec_d = din("vecs", [128, 8, 12])
    gsub_d = din("gsub", [128, 1])
    lamv_d = din("lamv", [128, 4, 64])
    brt_d = din("brt", [128, 36])
    wrg_d = din("wrg_bd", [128, 8, 128])
    wig_d = din("wig_bd", [128, 8, 128])
    wab_d = din("w_attn_br", [D, D])
    wlb_d = din("w_lru_br", [D, D])
    wout_d = din("w_out", [D, D])
    wrt_d = din("w_rt", [D, 36])
    weg_d = din("w_e_gate", [NE, D, FF])
    weu_d = din("w_e_up", [NE, D, FF])
    wed_d = din("w_e_down", [NE, FF, D])
    wpg_d = din("w_ple_gate", [D, D])
    wpp_d = din("w_ple_proj", [256, D])
    outT = nc.dram_tensor("outT", [D, NT], F32, kind="ExternalOutput").ap()

    KT_s = dscr("KT_s", [8, 128, S], BF16)
    XR_s = dscr("XR_s", [8, 128, S], BF16)
    V_s = dscr("V_s", [8, 128, 64, 128], BF16)
    HTO_s = dscr("HTO_s", [128, 8, NT], BF16)
    LRU_s = dscr("LRU_s", [128, 8, NT], BF16)
    OT_s = dscr("OT_s", [128, 8, NT], BF16)
    X1_s = dscr("X1_s", [128, 8, NT], F32)

    class Bank:
        def __init__(self, t, off):
            self.t, self.off = t, off

        def __getitem__(self, key):
            if not isinstance(key, tuple):
                key = (key, slice(None))
            pk, ck = key
            c0 = ck.start or 0
            c1 = 512 if ck.stop is None else ck.stop
            return self.t[pk, self.off + c0:self.off + c1]

    _p01 = [nc.alloc_psum_tensor("psb%d" % i, [128, 512], F32) for i in range(2)]
    big = [nc.alloc_psum_tensor("psbig%d" % i, [128, 1024], F32) for i in range(2)]
    _p67 = [nc.alloc_psum_tensor("psb%d" % i, [128, 512], F32) for i in (6, 7)]
    ps = _p01 + [Bank(big[0], 0), Bank(big[0], 512), Bank(big[1], 0), Bank(big[1], 512)] + _p67
    rr = [0]

    def nextbank(lo=1, hi=8):
        b = lo + rr[0] % (hi - lo)
        rr[0] += 1
        return b

    def PK(i):
        return ("ps", i)

    ones_bf = ar.alloc("ones_bf", [128, 128], BF16)
    ident_bf = ar.alloc("ident_bf", [128, 128], BF16)
    ident_f = ar.alloc("ident_f", [128, 128], F32)
    maskB = ar.alloc("maskB", [128, 16, 512], BF16)
    vec = ar.alloc("vec", [128, 8, 12], F32)
    gsub = ar.alloc("gsub", [128, 1], F32)
    lamv = ar.alloc("lamv", [128, 4, 64], F32)
    brt = ar.alloc("brt", [128, 36], F32)
    sel = ar.alloc("sel", [128, 4], F32)
    der = ar.alloc("der", [128, 8, 4], F32)
    sc = ar.alloc("sc", [128, 8], F32)
    lprod = ar.alloc("lprod", [128, 2, 64], F32)
    GM, GMOE, GPLE, GFIN, BCONV, BRG, BIG, LAM, WC0 = 0, 1, 2, 3, 4, 5, 6, 7, 8

    p.op("sp", lambda e: e.dma_start(out=maskB[:], in_=maskB_d), w=["maskB"], dma="c0")
    p.op("sp", lambda e: e.dma_start(out=vec[:], in_=vec_d), w=["vec"], dma="c1")
    p.op("sp", lambda e: e.dma_start(out=gsub[:], in_=gsub_d), w=["gsub"], dma="c1")
    p.op("sp", lambda e: e.dma_start(out=lamv[:], in_=lamv_d), w=["lamv"], dma="c1")
    p.op("sp", lambda e: e.dma_start(out=brt[:], in_=brt_d), w=["brt"], dma="c1")
    p.op("sp", lambda e: e.dma_start(out=sel[:], in_=sel_d), w=["sel"], dma="c1")
    p.op("pool", lambda e: e.memset(ident_f[:], 1.0), w=["ident_f"])
    p.op("pool", lambda e: e.affine_select(out=ident_f[:], in_=ident_f[:], pattern=[[1, 128]], compare_op=ALU.is_equal,
                                           fill=0.0, base=0, channel_multiplier=-1), r=["ident_f"], w=["ident_f"])
    p.op("dve", lambda e: e.tensor_copy(out=ident_bf[:], in_=ident_f[:]), r=["ident_f"], w=["ident_bf"])
    p.op("dve", lambda e: e.memset(ones_bf[:], 1.0), w=["ones_bf"])
    p.op("dve", lambda e: e.tensor_scalar(out=der[:, :, 0], in0=vec[:, :, BRG], scalar1=0.5, scalar2=None, op0=ALU.mult), r=["vec"], w=["der0"])
    p.op("dve", lambda e: e.tensor_scalar(out=der[:, :, 1], in0=vec[:, :, BIG], scalar1=0.5, scalar2=None, op0=ALU.mult), r=["vec"], w=["der1"])
    p.op("act", lambda e: e.activation(out=der[:, :, 3], in_=vec[:, :, LAM], func=AF.Exp, scale=-1.0), r=["vec"], w=["der3"])
    p.op("dve", lambda e: e.tensor_scalar(out=der[:, :, 2], in0=der[:, :, 3], scalar1=0.2, scalar2=None, op0=ALU.mult), r=["der3"], w=["der2"])
    for cst in (-0.25, 1.0 / 3.0, -0.5, 1.0):
        p.op("dve", lambda e, cst=cst: e.scalar_tensor_tensor(out=der[:, :, 2], in0=der[:, :, 2], scalar=cst, in1=der[:, :, 3], op0=ALU.add, op1=ALU.mult),
             r=["der2", "der3"], w=["der2"])
    p.op("dve", lambda e: e.tensor_scalar(out=der[:, :, 2], in0=der[:, :, 2], scalar1=-4.0, scalar2=None, op0=ALU.mult), r=["der2"], w=["der2"])
    p.op("dve", lambda e: e.tensor_tensor(out=lprod[:, 0, :], in0=lamv[:, 0, :], in1=lamv[:, 1, :], op=ALU.mult), r=["lamv"], w=["lprod"])
    p.op("dve", lambda e: e.tensor_tensor(out=lprod[:, 1, :], in0=lamv[:, 2, :], in1=lamv[:, 3, :], op=ALU.mult), r=["lamv", "lprod"], w=["lprod"])
    p.op("dve", lambda e: e.reduce_sum(out=sc[:, 2:4], in_=lprod[:], axis=AX.X), r=["lprod"], w=["sc23"])
    p.op("act", lambda e: e.activation(out=sc[:, 4:6], in_=sc[:, 2:4], func=AF.Exp), r=["sc23"], w=["sc45"])
    p.op("dve", lambda e: e.tensor_tensor(out=sc[:, 0:1], in0=sc[:, 5:6], in1=sc[:, 4:5], op=ALU.subtract), r=["sc45"], w=["sc0"])
    p.op("dve", lambda e: e.tensor_scalar(out=sc[:, 0:1], in0=sc[:, 0:1], scalar1=-0.2, scalar2=None, op0=ALU.add), r=["sc0"], w=["sc0"])
    p.op("dve", lambda e: e.tensor_scalar(out=sc[:, 1:2], in0=gsub[:], scalar1=0.8, scalar2=None, op0=ALU.mult), r=["gsub"], w=["sc1"])
    CONST_KEYS = ["maskB", "vec", "sel", "brt", "ident_f", "ident_bf", "ones_bf", "der0", "der1", "der2", "sc0", "sc1"]

    def consts_barrier():
        p.barrier()

    consts_barrier()
    gmark = ar.mark()

    def load_weight(dst, dst_c0, src2d, nk, col0, ncols, stg, piece, engines=("pool",)):
        view = src2d.rearrange("(kc p) c -> p kc c", p=128)
        ns = len(stg["t"])
        for c in range(0, ncols, piece):
            s = stg["i"] % ns
            eng = engines[stg["i"] % len(engines)]
            stg["i"] += 1
            st = stg["t"][s]
            key = ("stg", stg["name"], s)
            p.op("sp", lambda e, st=st, c=c: e.dma_start(out=st[:, 0:nk, 0:piece], in_=view[:, :, col0 + c:col0 + c + piece]),
                 w=[key], dma=key)
            if eng == "act":
                p.op("act", lambda e, st=st, c=c: e.copy(out=dst[:, 0:nk, dst_c0 + c:dst_c0 + c + piece], in_=st[:, 0:nk, 0:piece]),
                     r=[key], w=[("w", id(dst), dst_c0 + c)])
            else:
                p.op(eng, lambda e, st=st, c=c: e.tensor_copy(out=dst[:, 0:nk, dst_c0 + c:dst_c0 + c + piece], in_=st[:, 0:nk, 0:piece]),
                     r=[key], w=[("w", id(dst), dst_c0 + c)])

    def wkeys(dst, c0, n, piece):
        return [("w", id(dst), c) for c in range(c0 // piece * piece, c0 + n, piece)]

    evq = [0]

    def evac(out_ap, in_ap, rk, wk):
        evq[0] += 1
        if evq[0] % 2:
            p.op("act", lambda e: e.copy(out=out_ap, in_=in_ap), r=rk, w=wk)
        else:
            p.op("dve", lambda e: e.tensor_copy(out=out_ap, in_=in_ap), r=rk, w=wk)

    def rms_stats(src, nkc, nblk, sq, t1, rstd, rk, tag, eps=1e-6, dim=1024.0):
        p.op("act", lambda e: e.activation(out=sq, in_=src, func=AF.Square), r=rk, w=[tag + "sq"])
        for kc in range(nkc):
            p.op("pe", lambda e, kc=kc: e.matmul(ps[0][:, 0:nblk], lhsT=ones_bf[:], rhs=sq[:, kc, :], start=(kc == 0), stop=(kc == nkc - 1)),
                 r=[tag + "sq"], w=[PK(0)])
        p.op("act", lambda e: e.activation(out=t1, in_=ps[0][:, 0:nblk], func=AF.Ln, bias=eps, scale=1.0 / dim), r=[PK(0)], w=[tag + "t1"])
        p.op("act", lambda e: e.activation(out=rstd, in_=t1, func=AF.Exp, scale=-0.5), r=[tag + "t1"], w=[tag + "rstd"])

    Wk = ar.alloc("Wk", [128, 8, 1024], BF16)
    Wv = ar.alloc("Wv", [128, 8, 1024], BF16)
    Wx = ar.alloc("Wx", [128, 8, 1024], BF16)
    stgA = {"t": [ar.alloc("stgA", [128, 8, 256], F32) for _ in range(3)], "i": 0, "name": "A"}
    load_weight(Wk, 0, w_in, 8, 1024, 1024, stgA, 256, engines=("pool", "act", "dve"))
    load_weight(Wv, 0, w_in, 8, 2048, 1024, stgA, 256, engines=("pool", "act", "dve"))
    load_weight(Wx, 0, w_in, 8, 3072, 1024, stgA, 256, engines=("pool", "act", "dve"))
    xf = [ar.alloc("xf", [128, 8, 512], F32) for _ in range(2)]
    sq = ar.alloc("sq", [128, 8, 512], BF16)
    t1 = ar.alloc("t1", [128, 512], F32)
    rstd = ar.alloc("rstd", [128, 512], F32)
    hT = [ar.alloc("hT", [128, 8, 512], BF16) for _ in range(2)]
    kst = [ar.alloc("kst", [128, 8, 512], BF16) for _ in range(2)]
    xst = [ar.alloc("xst", [128, 8, 512], BF16) for _ in range(2)]
    vst = [ar.alloc("vst", [128, 4, 1024], BF16) for _ in range(2)]
    xTf_v = xT_full.rearrange("(kc p) t -> p kc t", p=128)
    xTo_v = xT_own.rearrange("(kc p) t -> p kc t", p=128)
    KT_v = KT_s.rearrange("h p t -> p h t")
    XR_v = XR_s.rearrange("h p t -> p h t")
    V_v = V_s.rearrange("h p t v -> p t h v")

    def norm_block(src_view, tb, s):
        p.op("sp", lambda e: e.dma_start(out=xf[s][:], in_=src_view[:, :, tb * 512:(tb + 1) * 512]), w=[("xf", s)], dma=("xf", s))
        rms_stats(xf[s][:], 8, 512, sq[:], t1[:], rstd[:], [("xf", s)], "A")
        for kc in range(8):
            p.op("dve", lambda e, kc=kc: e.scalar_tensor_tensor(out=hT[s][:, kc, :], in0=xf[s][:, kc, :], scalar=vec[:, kc, GM:GM + 1],
                                                              in1=rstd[:], op0=ALU.mult, op1=ALU.mult),
                 r=[("xf", s), "Arstd"], w=[("hT", s, kc)])

    def a1_compute(kind, tb, s):
        if kind == "own":
            p.op("pool", lambda e: e.dma_start(out=HTO_s[:, :, tb * 512:(tb + 1) * 512], in_=hT[s][:]),
                 r=[("hT", s, kc) for kc in range(8)], w=[("HTO", tb)], dma=("hTst", s))
            return
        for (W, st, keyn) in ((Wk, kst, "kst"), (Wx, xst, "xst")):
            for h in range(8):
                b = nextbank()
                for kc in range(8):
                    p.op("pe", lambda e, W=W, b=b, kc=kc, h=h: e.matmul(ps[b][:], lhsT=W[:, kc, h * 128:(h + 1) * 128], rhs=hT[s][:, kc, :],
                                                                   start=(kc == 0), stop=(kc == 7)),
                         r=[("hT", s, kc)] + wkeys(W, h * 128, 128, 256), w=[PK(b)])
                evac(st[s][:, h, :], ps[b][:], [PK(b)], [(keyn, s, h)])
        p.op("pool", lambda e: e.dma_start(out=KT_v[:, :, tb * 512:(tb + 1) * 512], in_=kst[s][:]), r=[("kst", s, h) for h in range(8)], w=[("KT", tb)], dma=("kst", s))
        p.op("pool", lambda e: e.dma_start(out=XR_v[:, :, tb * 512:(tb + 1) * 512], in_=xst[s][:]), r=[("xst", s, h) for h in range(8)], w=[("XR", tb)], dma=("xst", s))
        for tt in range(4):
            for half in range(2):
                b = nextbank()
                for kc in range(8):
                    p.op("pe", lambda e, b=b, kc=kc, tt=tt, half=half: e.matmul(ps[b][:], lhsT=hT[s][:, kc, tt * 128:(tt + 1) * 128],
                                                                           rhs=Wv[:, kc, half * 512:(half + 1) * 512], start=(kc == 0), stop=(kc == 7)),
                         r=[("hT", s, kc)] + wkeys(Wv, half * 512, 512, 256), w=[PK(b)])
                evac(vst[s][:, tt, half * 512:(half + 1) * 512], ps[b][:], [PK(b)], [("vst", s, tt, half)])
        for tt in range(4):
            p.op("pool", lambda e, tt=tt: e.dma_start(out=V_v[:, tb * 4 + tt], in_=vst[s][:, tt, :].rearrange("p (h v) -> p h v", h=8)),
                 r=[("vst", s, tt, half) for half in range(2)], w=[("V", tb, tt)], dma=("vst", s))

    jobs = [("own", tb) for tb in range(4)] + [("full", tb) for tb in range(16)]
    norm_block(xTo_v, 0, 0)
    for n, (kind, tb) in enumerate(jobs):
        if n + 1 < len(jobs):
            k2, tb2 = jobs[n + 1]
            norm_block(xTo_v if k2 == "own" else xTf_v, tb2, (n + 1) % 2)
        a1_compute(kind, tb, n % 2)
    p.barrier()
    ar.reset(gmark)
    if STOP_AFTER == "A1":
        return finish(nc, p, outT, ar)

    Wgr = ar.alloc("Wgr", [128, 8, 1024], BF16)
    Wrg = ar.alloc("Wrg", [128, 8, 128], BF16)
    Wig = ar.alloc("Wig", [128, 8, 128], BF16)
    stgB = {"t": [ar.alloc("stgB", [128, 8, 128], F32) for _ in range(3)], "i": 0, "name": "B"}
    load_weight(Wgr, 0, w_in, 8, 4096, 1024, stgB, 128, engines=("pool", "act", "dve"))
    for (Wt, src, nm) in ((Wrg, wrg_d, "wrg"), (Wig, wig_d, "wig")):
        s = stgB["i"] % len(stgB["t"])
        stgB["i"] += 1
        st = stgB["t"][s]
        key = ("stg", "B", s)
        p.op("sp", lambda e, st=st, src=src: e.dma_start(out=st[:, :, 0:128], in_=src), w=[key], dma=key)
        p.op("pool", lambda e, st=st, Wt=Wt: e.tensor_copy(out=Wt[:], in_=st[:, :, 0:128]), r=[key], w=[nm])
    Dm = ar.alloc("Dm", [128, 8, 4, 128], BF16)
    for cc in range(8):
        for j in range(4):
            p.op("dve", lambda e, cc=cc, j=j: e.tensor_scalar(out=Dm[:, cc, j, :], in0=ident_f[:], scalar1=vec[:, cc, WC0 + j:WC0 + j + 1], scalar2=None, op0=ALU.mult),
                 w=[("Dm", cc, j)])
    xrb = [ar.alloc("xrb", [128, 4, 2048], BF16) for _ in range(2)]
    xcb = [ar.alloc("xcb", [128, 2048], BF16) for _ in range(2)]
    rt = [ar.alloc("rt", [128, 2048], F32) for _ in range(2)]
    it = [ar.alloc("it", [128, 2048], BF16) for _ in range(2)]
    aa = [ar.alloc("aa", [128, 2048], F32) for _ in range(2)]
    uu = ar.alloc("uu", [128, 2048], F32)
    hh = ar.alloc("hh", [128, 2048], F32)
    hTo2 = [ar.alloc("hTo", [128, 8, 512], BF16) for _ in range(2)]
    carry = ar.alloc("carry", [128, 1], F32)
    hsel = ar.alloc("hsel", [128, 512], F32)
    gz2 = [ar.alloc("gz", [128, 512], F32) for _ in range(2)]
    gw = ar.alloc("gw", [128, 512], F32)
    gt2 = [ar.alloc("gt", [128, 512], F32) for _ in range(2)]
    lst = [ar.alloc("lst", [128, 512], BF16) for _ in range(2)]

    def a2_stage1(cc, tb2, s):
        K = lambda nm: (nm, s)
        t0 = tb2 * 2048
        hTo = hTo2[s]
        gt = gt2[s]
        p.op("sp", lambda e: e.dma_start(out=hTo[:], in_=HTO_s[:, :, tb2 * 512:(tb2 + 1) * 512]), w=[("hTo", s)], dma=("hTo", s))
        for j in range(4):
            if tb2 == 0 and j < 3:
                p.op("dve", lambda e, j=j: e.memset(xrb[s][:, j, 0:3 - j], 0.0), w=[("xrb", s, j)])
                p.op("sp", lambda e, j=j: e.dma_start(out=xrb[s][:, j, 3 - j:2048], in_=XR_s[cc, :, 0:2048 - (3 - j)]), w=[("xrb", s, j)], dma=("xrb", s))
            else:
                p.op("sp", lambda e, j=j: e.dma_start(out=xrb[s][:, j, :], in_=XR_s[cc, :, t0 - 3 + j:t0 - 3 + j + 2048]), w=[("xrb", s, j)], dma=("xrb", s))
        cb = []
        for nb in range(4):
            sl = slice(nb * 512, (nb + 1) * 512)
            b = nextbank()
            cb.append(b)
            for j in range(4):
                p.op("pe", lambda e, b=b, j=j, sl=sl: e.matmul(ps[b][:], lhsT=Dm[:, cc, j, :], rhs=xrb[s][:, j, sl], start=(j == 0), stop=(j == 3)),
                     r=[("xrb", s, j), ("Dm", cc, j)], w=[PK(b)])
        for nb in range(4):
            sl = slice(nb * 512, (nb + 1) * 512)
            b = cb[nb]
            if nb % 2 == 0:
                p.op("act", lambda e, b=b, sl=sl: e.activation(out=xcb[s][:, sl], in_=ps[b][:], func=AF.Identity, bias=vec[:, cc, BCONV:BCONV + 1], scale=1.0),
                     r=[PK(b)], w=[("xcb", s, nb)])
            else:
                p.op("dve", lambda e, b=b, sl=sl: e.tensor_scalar(out=xcb[s][:, sl], in0=ps[b][:], scalar1=vec[:, cc, BCONV:BCONV + 1], scalar2=None, op0=ALU.add),
                     r=[PK(b)], w=[("xcb", s, nb)])
        bq = nextbank()
        for kc in range(8):
            p.op("pe", lambda e, kc=kc: e.matmul(ps[bq][:], lhsT=Wgr[:, kc, cc * 128:(cc + 1) * 128], rhs=hTo[:, kc, :], start=(kc == 0), stop=(kc == 7)),
                 r=[("hTo", s)] + wkeys(Wgr, cc * 128, 128, 128), w=[PK(bq)])
        gz = gz2[s]
        p.op("act", lambda e: e.copy(out=gz[:], in_=ps[bq][:]), r=[PK(bq)], w=[("gz", s)])
        p.op("act", lambda e: e.activation(out=gw[:], in_=ps[bq][:], func=AF.Square), r=[PK(bq)], w=["gw"])
        p.op("pool", lambda e: e.tensor_scalar(out=gw[:], in0=gw[:], scalar1=0.044715, scalar2=1.0, op0=ALU.mult, op1=ALU.add), r=["gw"], w=["gw"])
        p.op("pool", lambda e: e.tensor_tensor(out=gw[:], in0=gw[:], in1=gz[:], op=ALU.mult), r=["gw", ("gz", s)], w=["gw"])
        for nb in range(4):
            sl = slice(nb * 512, (nb + 1) * 512)
            for (Wt, nm, dst, hb) in ((Wrg, "wrg", rt, 0), (Wig, "wig", it, 1)):
                b2 = nextbank()
                p.op("pe", lambda e, b2=b2, Wt=Wt, sl=sl: e.matmul(ps[b2][:], lhsT=Wt[:, cc, :], rhs=xcb[s][:, sl], start=True, stop=True),
                     r=[("xcb", s, nb), nm], w=[PK(b2)])
                p.op("act", lambda e, b2=b2, dst=dst, sl=sl, hb=hb: e.activation(out=dst[s][:, sl], in_=ps[b2][:], func=AF.Tanh, bias=der[:, cc, hb:hb + 1], scale=0.5),
                     r=[PK(b2)], w=[("g", s, hb, nb)])
        p.op("act", lambda e: e.activation(out=gt[:], in_=gw[:], func=AF.Tanh, scale=0.7978845608028654), r=["gw"], w=[("gt", s)])
        gk = [("g", s, 0, nb) for nb in range(4)]
        p.op("act", lambda e: e.activation(out=aa[s][:], in_=rt[s][:], func=AF.Exp, bias=der[:, cc, 2:3], scale=der[:, cc, 2:3]), r=gk, w=[K("aa")])
        p.op("pool", lambda e: e.tensor_tensor(out=rt[s][:], in0=aa[s][:], in1=aa[s][:], op=ALU.mult), r=[K("aa")], w=gk)
        p.op("act", lambda e: e.activation(out=rt[s][:], in_=rt[s][:], func=AF.Sqrt, bias=0.25, scale=-0.25), r=gk, w=gk)

    def a2_stage2(cc, tb2, s, n):
        K = lambda nm: (nm, s)
        gk = [("g", s, 0, nb) for nb in range(4)]
        ik = [("g", s, 1, nb) for nb in range(4)]
        xk = [("xcb", s, nb) for nb in range(4)]
        gz = gz2[s]
        gt = gt2[s]
        p.op("dve", lambda e: e.scalar_tensor_tensor(out=gt[:], in0=gt[:], scalar=1.0, in1=gz[:], op0=ALU.add, op1=ALU.mult), r=[("gt", s), ("gz", s)], w=[("gt", s)])
        p.op("dve", lambda e: e.scalar_tensor_tensor(out=uu[:], in0=it[s][:], scalar=1.0, in1=xcb[s][:], op0=ALU.add, op1=ALU.mult), r=ik + xk, w=["uu"])
        p.op("dve", lambda e: e.tensor_tensor(out=uu[:], in0=uu[:], in1=rt[s][:], op=ALU.mult), r=["uu"] + gk, w=["uu"])
        if tb2 == 0:
            p.op("dve", lambda e: e.tensor_tensor_scan(out=hh[:], data0=aa[s][:], data1=uu[:], initial=0.0, op0=ALU.mult, op1=ALU.add), r=[K("aa"), "uu"], w=["hh"])
        else:
            p.op("dve", lambda e: e.tensor_tensor_scan(out=hh[:], data0=aa[s][:], data1=uu[:], initial=carry[:, 0:1], op0=ALU.mult, op1=ALU.add),
                 r=[K("aa"), "uu", "carry"], w=["hh"])
        p.op("dve", lambda e: e.tensor_copy(out=carry[:], in_=hh[:, 2047:2048]), r=["hh"], w=["carry"])
        h4 = hh[:].rearrange("p (i r q) -> p i r q", i=4, r=4)
        hs3 = hsel[:].rearrange("p (i q) -> p i q", i=4)
        p.op("dve", lambda e: e.tensor_scalar(out=hs3, in0=h4[:, :, 0, :], scalar1=sel[:, 0:1], scalar2=None, op0=ALU.mult), r=["hh"], w=["hsel"])
        for r_ in range(1, 4):
            p.op("dve", lambda e, r_=r_: e.scalar_tensor_tensor(out=hs3, in0=h4[:, :, r_, :], scalar=sel[:, r_:r_ + 1], in1=hs3, op0=ALU.mult, op1=ALU.add),
                 r=["hh", "hsel"], w=["hsel"])
        gt = gt2[s]
        ls = lst[n % 2]
        lk = ("lst", n % 2)
        p.op("dve", lambda e: e.scalar_tensor_tensor(out=ls[:], in0=gt[:], scalar=0.5, in1=hsel[:], op0=ALU.mult, op1=ALU.mult), r=[("gt", s), "hsel"], w=[lk])
        p.op("pool", lambda e: e.dma_start(out=LRU_s[:, cc, tb2 * 512:(tb2 + 1) * 512], in_=ls[:]), r=[lk], w=[("LRU", cc, tb2)], dma=lk)

    blocks = [(cc, tb2) for cc in range(8) for tb2 in range(4)]
    a2_stage1(blocks[0][0], blocks[0][1], 0)
    for n, (cc, tb2) in enumerate(blocks):
        if n + 1 < len(blocks):
            a2_stage1(blocks[n + 1][0], blocks[n + 1][1], (n + 1) % 2)
        a2_stage2(cc, tb2, n % 2, n)
    p.barrier()
    ar.reset(gmark)
    if STOP_AFTER == "A2":
        return finish(nc, p, outT, ar)

    Wq = ar.alloc("Wq", [128, 8, 1024], BF16)
    stgC = {"t": [ar.alloc("stgC", [128, 8, 256], F32) for _ in range(3)], "i": 0, "name": "C"}
    load_weight(Wq, 0, w_in, 8, 0, 1024, stgC, 256, engines=("pool", "act", "dve"))
    hTa = ar.alloc("hTa", [128, 8, NT], BF16)
    p.op("sp", lambda e: e.dma_start(out=hTa[:], in_=HTO_s), w=["hTa"], dma="hTa")
    ktb = [ar.alloc("ktb", [128, S], BF16) for _ in range(2)]
    vtb = [ar.alloc("vtb", [128, 64, 128], BF16) for _ in range(2)]
    qT = [ar.alloc("qT", [128, NT], BF16) for _ in range(2)]
    NPT = 3
    pT = [ar.alloc("pT", [128, 1024], BF16) for _ in range(NPT)]
    rl2 = [ar.alloc("rl2", [128, 512], F32) for _ in range(2)]
    oc = [ar.alloc("oc", [128, 512], F32) for _ in range(2)]
    rl = ar.alloc("rl", [128, 512], F32)
    oo = ar.alloc("oo", [128, 512], F32)
    osq = ar.alloc("osq", [128, 512], BF16)
    ot1 = ar.alloc("ot1", [128, 512], F32)
    ors = ar.alloc("ors", [128, 512], F32)
    ost = [ar.alloc("ost", [128, 512], BF16) for _ in range(2)]
    OB = (6, 7)
    LBS = (0, 1)
    LB, QB = 0, 1
    npt = 0
    nst = 0
    for h in range(8):
        hs = h % 2
        p.op("sp", lambda e, hs=hs, h=h: e.dma_start(out=ktb[hs][:], in_=KT_s[h]), w=[("ktb", hs)], dma=("ktb", hs))
        p.op("sp", lambda e, hs=hs, h=h: e.dma_start(out=vtb[hs][:], in_=V_s[h]), w=[("vtb", hs)], dma=("vtb", hs))
        for tb in range(4):
            b = QB
            for kc in range(8):
                p.op("pe", lambda e, b=b, kc=kc, h=h, tb=tb: e.matmul(ps[b][:], lhsT=Wq[:, kc, h * 128:(h + 1) * 128], rhs=hTa[:, kc, tb * 512:(tb + 1) * 512],
                                                                 start=(kc == 0), stop=(kc == 7)),
                     r=["hTa"] + wkeys(Wq, h * 128, 128, 256), w=[PK(b)])
            evac(qT[hs][:, tb * 512:(tb + 1) * 512], ps[b][:], [PK(b)], [("qT", hs, tb)])
        for g in range(4):
            nkt = 16 * g + 16

            def col0(kt, g=g):
                return 0 if kt < 16 * g else 128 * ((kt - 16 * g) // 4)

            def s_mm(kt, g=g, hs=hs):
                sb = kt % 2
                c0 = col0(kt)
                for c in range(2):
                    p.op("pe", lambda e, c=c: e.matmul(big[sb][:, c * 512 + c0:(c + 1) * 512], lhsT=ktb[hs][64 * c:64 * c + 64, kt * 128:(kt + 1) * 128],
                                                       rhs=qT[hs][64 * c:64 * c + 64, g * 512 + c0:(g + 1) * 512], start=True, stop=True),
                         r=[("ktb", hs), ("qT", hs, g)], w=[PK(2 + 2 * sb + c)])
            s_mm(0)
            s_mm(1)
            for kt in range(nkt):
                sb = kt % 2
                pt = pT[npt % NPT]
                pk = ("pT", npt % NPT)
                npt += 1
                c0 = col0(kt)
                if c0 == 0:
                    p.op("act", lambda e, sb=sb, pt=pt: e.activation(out=pt[:], in_=big[sb][:], func=AF.Exp, scale=0.125), r=[PK(2 + 2 * sb), PK(3 + 2 * sb)], w=[(pk, 0), (pk, 1)])
                else:
                    p.op("act", lambda e, sb=sb, pt=pt, c0=c0: e.activation(out=pt[:].rearrange("p (c q) -> p c q", c=2)[:, :, c0:512],
                                                                           in_=big[sb][:].rearrange("p (c q) -> p c q", c=2)[:, :, c0:512], func=AF.Exp, scale=0.125),
                         r=[PK(2 + 2 * sb), PK(3 + 2 * sb)], w=[(pk, 0), (pk, 1)])
                if kt >= 16 * g:
                    tau = kt - 16 * g
                    p.op("dve", lambda e, pt=pt, tau=tau, c0=c0: e.tensor_tensor(out=pt[:, c0:512], in0=pt[:, c0:512], in1=maskB[:, tau, c0:512], op=ALU.mult), r=[(pk, 0)], w=[(pk, 0)])
                    p.op("pool", lambda e, pt=pt, tau=tau, c0=c0: e.tensor_tensor(out=pt[:, 512 + c0:1024], in0=pt[:, 512 + c0:1024], in1=maskB[:, tau, c0:512], op=ALU.mult), r=[(pk, 1)], w=[(pk, 1)])
                if kt + 2 < nkt:
                    s_mm(kt + 2)
                for c in range(2):
                    p.op("pe", lambda e, pt=pt, kt=kt, hs=hs, nkt=nkt, c=c, c0=c0: e.matmul(ps[OB[c]][:, c0:512], lhsT=vtb[hs][:, kt, :], rhs=pt[:, c * 512 + c0:(c + 1) * 512], start=(kt == 0), stop=(kt == nkt - 1)),
                         r=[(pk, c), ("vtb", hs)], w=[PK(OB[c])])
                    p.op("pe", lambda e, pt=pt, kt=kt, nkt=nkt, c=c, c0=c0: e.matmul(ps[LBS[c]][:, c0:512], lhsT=ones_bf[:], rhs=pt[:, c * 512 + c0:(c + 1) * 512], start=(kt == 0), stop=(kt == nkt - 1)),
                         r=[(pk, c)], w=[PK(LBS[c])])
            for c in range(2):
                p.op("act", lambda e, lb=LBS[c], c=c: e.activation(out=rl2[c][:], in_=ps[lb][:], func=AF.Ln), r=[PK(LBS[c])], w=[("rl", c)])
                p.op("act", lambda e, c=c: e.activation(out=rl2[c][:], in_=rl2[c][:], func=AF.Exp, scale=-1.0), r=[("rl", c)], w=[("rl", c)])
                p.op("dve", lambda e, c=c: e.tensor_tensor(out=oc[c][:], in0=ps[OB[c]][:], in1=rl2[c][:], op=ALU.mult), r=[PK(OB[c]), ("rl", c)], w=[("oc", c)])
            p.op("dve", lambda e: e.scalar_tensor_tensor(out=oo[:], in0=oc[1][:], scalar=sc[:, 0:1], in1=oc[0][:], op0=ALU.mult, op1=ALU.add),
                 r=[("oc", 0), ("oc", 1)], w=["oo"])
            p.op("act", lambda e: e.activation(out=osq[:], in_=oo[:], func=AF.Square), r=["oo"], w=["osq"])
            p.op("pe", lambda e: e.matmul(ps[LB][:], lhsT=ones_bf[:], rhs=osq[:], start=True, stop=True), r=["osq"], w=[PK(LB)])
            p.op("act", lambda e: e.activation(out=ot1[:], in_=ps[LB][:], func=AF.Ln, bias=1e-5, scale=1.0 / 128.0), r=[PK(LB)], w=["ot1"])
            p.op("act", lambda e: e.activation(out=ors[:], in_=ot1[:], func=AF.Exp, scale=-0.5), r=["ot1"], w=["ors"])
            os_ = ost[nst % 2]
            ok = ("ost", nst % 2)
            nst += 1
            p.op("dve", lambda e, os_=os_: e.scalar_tensor_tensor(out=os_[:], in0=oo[:], scalar=sc[:, 1:2], in1=ors[:], op0=ALU.mult, op1=ALU.mult), r=["oo", "ors"], w=[ok])
            p.op("pool", lambda e, os_=os_, h=h, g=g: e.dma_start(out=OT_s[:, h, g * 512:(g + 1) * 512], in_=os_[:]), r=[ok], w=[("OT", h, g)], dma=ok)
    p.barrier()
    ar.reset(gmark)
    if STOP_AFTER == "A3":
        return finish(nc, p, outT, ar)

    tT = ar.alloc("tT", [128, 8, NT], BF16)
    b2mark = ar.mark()
    mixed = ar.alloc("mixed", [128, 8, NT], BF16)
    bmark = ar.mark()
    Wga = ar.alloc("Wga", [128, 8, 1024], BF16)
    Wgb = ar.alloc("Wgb", [128, 8, 1024], BF16)
    Wab = ar.alloc("Wab", [128, 8, 1024], BF16)
    Wlb = ar.alloc("Wlb", [128, 8, 1024], BF16)
    stgD = {"t": [ar.alloc("stgD", [128, 8, 128], F32) for _ in range(3)], "i": 0, "name": "D"}
    load_weight(Wga, 0, w_in, 8, 5120, 1024, stgD, 128, engines=("pool", "act", "dve"))
    load_weight(Wgb, 0, w_in, 8, 6144, 1024, stgD, 128, engines=("pool", "act", "dve"))
    load_weight(Wab, 0, wab_d, 8, 0, 1024, stgD, 128, engines=("pool", "act", "dve"))
    load_weight(Wlb, 0, wlb_d, 8, 0, 1024, stgD, 128, engines=("pool", "act", "dve"))
    hb_ = [ar.alloc("hb", [128, 8, 512], BF16)] * 2
    ob_ = [ar.alloc("ob", [128, 8, 512], BF16)] * 2
    lb_ = [ar.alloc("lb", [128, 8, 512], BF16)] * 2
    sga = ar.alloc("sga", [128, 512], F32)
    sgb = ar.alloc("sgb", [128, 512], F32)
    m1 = ar.alloc("m1", [128, 512], F32)
    m2 = ar.alloc("m2", [128, 512], F32)
    for tb in range(4):
        s = 0
        sl = slice(tb * 512, (tb + 1) * 512)
        p.op("sp", lambda e, s=s, sl=sl: e.dma_start(out=hb_[s][:], in_=HTO_s[:, :, sl]), w=[("hb", s)], dma=("hb", s))
        p.op("sp", lambda e, s=s, sl=sl: e.dma_start(out=ob_[s][:], in_=OT_s[:, :, sl]), w=[("ob", s)], dma=("ob", s))
        p.op("sp", lambda e, s=s, sl=sl: e.dma_start(out=lb_[s][:], in_=LRU_s[:, :, sl]), w=[("lb", s)], dma=("lb", s))
        for fc in range(8):
            banks = [nextbank() for _ in range(4)]
            for (W, src, sk, b) in ((Wga, hb_, "hb", banks[0]), (Wgb, hb_, "hb", banks[1]), (Wab, ob_, "ob", banks[2]), (Wlb, lb_, "lb", banks[3])):
                for kc in range(8):
                    p.op("pe", lambda e, W=W, src=src, b=b, kc=kc, fc=fc, s=s: e.matmul(ps[b][:], lhsT=W[:, kc, fc * 128:(fc + 1) * 128], rhs=src[s][:, kc, :],
                                                                                   start=(kc == 0), stop=(kc == 7)),
                         r=[(sk, s)] + wkeys(W, fc * 128, 128, 128), w=[PK(b)])
            p.op("act", lambda e, b=banks[0]: e.activation(out=sga[:], in_=ps[b][:], func=AF.Sigmoid), r=[PK(banks[0])], w=["sga"])
            p.op("act", lambda e, b=banks[1]: e.activation(out=sgb[:], in_=ps[b][:], func=AF.Sigmoid), r=[PK(banks[1])], w=["sgb"])
            p.op("dve", lambda e, b=banks[2]: e.tensor_tensor(out=m1[:], in0=ps[b][:], in1=sga[:], op=ALU.mult), r=[PK(banks[2]), "sga"], w=["m1"])
            p.op("dve", lambda e, b=banks[3]: e.tensor_tensor(out=m2[:], in0=ps[b][:], in1=sgb[:], op=ALU.mult), r=[PK(banks[3]), "sgb"], w=["m2"])
            p.op("pool", lambda e, fc=fc, sl=sl: e.tensor_tensor(out=mixed[:, fc, sl], in0=m1[:], in1=m2[:], op=ALU.add), r=["m1", "m2"], w=[("mixed", tb)])
    p.barrier()
    ar.reset(bmark)

    Wo = ar.alloc("Wo", [128, 8, 1024], BF16)
    stgE = {"t": [ar.alloc("stgE", [128, 8, 256], F32) for _ in range(3)], "i": 0, "name": "E"}
    load_weight(Wo, 0, wout_d, 8, 0, 1024, stgE, 256, engines=("pool", "act", "dve"))
    xo = [ar.alloc("xo", [128, 8, 512], F32) for _ in range(2)]
    sq2 = ar.alloc("sq2", [128, 8, 512], BF16)
    t1b = ar.alloc("t1b", [128, 512], F32)
    rstdb = ar.alloc("rstdb", [128, 512], F32)
    for tb in range(4):
        s = tb % 2
        sl = slice(tb * 512, (tb + 1) * 512)
        xk = [("xo", s, fc) for fc in range(8)]
        p.op("sp", lambda e, s=s, sl=sl: e.dma_start(out=xo[s][:], in_=xTo_v[:, :, sl]), w=xk, dma=("xo", s))
        for fc in range(8):
            b = nextbank()
            for kc in range(8):
                p.op("pe", lambda e, b=b, kc=kc, fc=fc, sl=sl: e.matmul(ps[b][:], lhsT=Wo[:, kc, fc * 128:(fc + 1) * 128], rhs=mixed[:, kc, sl], start=(kc == 0), stop=(kc == 7)),
                     r=wkeys(Wo, fc * 128, 128, 256), w=[PK(b)])
            p.op("dve", lambda e, b=b, fc=fc, s=s: e.tensor_tensor(out=xo[s][:, fc, :], in0=ps[b][:], in1=xo[s][:, fc, :], op=ALU.add),
                 r=[PK(b), ("xo", s, fc)], w=[("xo", s, fc)])
        rms_stats(xo[s][:], 8, 512, sq2[:], t1b[:], rstdb[:], xk, "B")
        for kc in range(8):
            p.op("dve", lambda e, kc=kc, sl=sl, s=s: e.scalar_tensor_tensor(out=tT[:, kc, sl], in0=xo[s][:, kc, :], scalar=vec[:, kc, GMOE:GMOE + 1], in1=rstdb[:],
                                                                      op0=ALU.mult, op1=ALU.mult), r=[("xo", s, kc), "Brstd"], w=[("tT", tb, kc)])
        p.op("pool", lambda e, sl=sl, s=s: e.dma_start(out=X1_s[:, :, sl], in_=xo[s][:]), r=xk, w=[("X1", tb)], dma=("xo", s))
    p.barrier()
    ar.reset(b2mark)
    if STOP_AFTER == "B1":
        return finish(nc, p, outT, ar)

    yy = ar.alloc("yy", [128, 16, 1024], F32)
    b3mark = ar.mark()
    Wr = ar.alloc("Wr", [128, 8, 36], BF16)
    wrs = ar.alloc("wrs", [128, 8, 36], F32)
    p.op("sp", lambda e: e.dma_start(out=wrs[:], in_=wrt_d.rearrange("(kc p) c -> p kc c", p=128)), w=["wrs"], dma="wrs")
    p.op("pool", lambda e: e.tensor_copy(out=Wr[:], in_=wrs[:]), r=["wrs"], w=["Wr"])
    lg = ar.alloc("lg", [128, 16, 36], F32)
    comb = ar.alloc("comb", [128, 16, 32], F32)
    gmx = ar.alloc("gmx", [128, 16, 1], F32)
    goh = ar.alloc("goh", [128, 16, 4], F32)
    gex = ar.alloc("gex", [128, 16, 4], F32)
    gsm = ar.alloc("gsm", [128, 16, 1], F32)
    gw_ = ar.alloc("gw_", [128, 16, 1], F32)
    fsel = ar.alloc("fsel", [128, 16, 8], F32)
    ftmp = ar.alloc("ftmp", [128, 16, 8], F32)
    m1_ = ar.alloc("m1_", [128, 16, 1], F32)
    m2_ = ar.alloc("m2_", [128, 16, 1], F32)
    oh1 = ar.alloc("oh1", [128, 16, 8], F32)
    oh2 = ar.alloc("oh2", [128, 16, 8], F32)
    w12 = ar.alloc("w12", [128, 16, 2], F32)
    wexp = ar.alloc("wexp", [128, 16, 8], F32)
    for tt in range(16):
        b = nextbank()
        for kc in range(8):
            p.op("pe", lambda e, b=b, kc=kc, tt=tt: e.matmul(ps[b][:, 0:36], lhsT=tT[:, kc, tt * 128:(tt + 1) * 128], rhs=Wr[:, kc, :], start=(kc == 0), stop=(kc == 7)),
                 r=["Wr"], w=[PK(b)])
        p.op("dve", lambda e, b=b, tt=tt: e.tensor_tensor(out=lg[:, tt, :], in0=ps[b][:, 0:36], in1=brt[:], op=ALU.add), r=[PK(b)], w=["lg"])
    p.op("dve", lambda e: e.tensor_reduce(out=gmx[:], in_=lg[:, :, 0:4], axis=AX.X, op=ALU.max), r=["lg"], w=["gmx"])
    p.op("dve", lambda e: e.tensor_tensor(out=goh[:], in0=lg[:, :, 0:4], in1=gmx[:].to_broadcast([128, 16, 4]), op=ALU.is_equal), r=["lg", "gmx"], w=["goh"])
    p.op("dve", lambda e: e.tensor_tensor(out=gex[:], in0=lg[:, :, 0:4], in1=gmx[:].to_broadcast([128, 16, 4]), op=ALU.subtract), r=["lg", "gmx"], w=["gex"])
    p.op("act", lambda e: e.activation(out=gex[:], in_=gex[:], func=AF.Exp), r=["gex"], w=["gex"])
    p.op("dve", lambda e: e.tensor_reduce(out=gsm[:], in_=gex[:], axis=AX.X, op=ALU.add), r=["gex"], w=["gsm"])
    p.op("dve", lambda e: e.reciprocal(out=gw_[:], in_=gsm[:]), r=["gsm"], w=["gw_"])
    for g in range(4):
        fin = lg[:, :, 4 + 8 * g:12 + 8 * g]
        if g == 0:
            p.op("dve", lambda e, fin=fin: e.tensor_tensor(out=fsel[:], in0=fin, in1=goh[:, :, 0:1].to_broadcast([128, 16, 8]), op=ALU.mult), r=["lg", "goh"], w=["fsel"])
        else:
            p.op("dve", lambda e, fin=fin, g=g: e.tensor_tensor(out=ftmp[:], in0=fin, in1=goh[:, :, g:g + 1].to_broadcast([128, 16, 8]), op=ALU.mult), r=["lg", "goh"], w=["ftmp"])
            p.op("dve", lambda e: e.tensor_tensor(out=fsel[:], in0=fsel[:], in1=ftmp[:], op=ALU.add), r=["fsel", "ftmp"], w=["fsel"])
    p.op("dve", lambda e: e.tensor_reduce(out=m1_[:], in_=fsel[:], axis=AX.X, op=ALU.max), r=["fsel"], w=["m1_"])
    p.op("dve", lambda e: e.tensor_tensor(out=oh1[:], in0=fsel[:], in1=m1_[:].to_broadcast([128, 16, 8]), op=ALU.is_equal), r=["fsel", "m1_"], w=["oh1"])
    p.op("dve", lambda e: e.scalar_tensor_tensor(out=ftmp[:], in0=oh1[:], scalar=-1e30, in1=fsel[:], op0=ALU.mult, op1=ALU.add), r=["oh1", "fsel"], w=["ftmp"])
    p.op("dve", lambda e: e.tensor_reduce(out=m2_[:], in_=ftmp[:], axis=AX.X, op=ALU.max), r=["ftmp"], w=["m2_"])
    p.op("dve", lambda e: e.tensor_tensor(out=oh2[:], in0=ftmp[:], in1=m2_[:].to_broadcast([128, 16, 8]), op=ALU.is_equal), r=["ftmp", "m2_"], w=["oh2"])
    p.op("dve", lambda e: e.tensor_tensor(out=w12[:, :, 0:1], in0=m2_[:], in1=m1_[:], op=ALU.subtract), r=["m1_", "m2_"], w=["w12"])
    p.op("act", lambda e: e.activation(out=w12[:, :, 0:1], in_=w12[:, :, 0:1], func=AF.Exp), r=["w12"], w=["w12"])
    p.op("dve", lambda e: e.tensor_scalar(out=w12[:, :, 0:1], in0=w12[:, :, 0:1], scalar1=1.0, scalar2=None, op0=ALU.add), r=["w12"], w=["w12"])
    p.op("dve", lambda e: e.reciprocal(out=w12[:, :, 0:1], in_=w12[:, :, 0:1]), r=["w12"], w=["w12"])
    p.op("dve", lambda e: e.tensor_tensor(out=w12[:, :, 0:1], in0=w12[:, :, 0:1], in1=gw_[:], op=ALU.mult), r=["w12", "gw_"], w=["w12"])
    p.op("dve", lambda e: e.tensor_tensor(out=w12[:, :, 1:2], in0=gw_[:], in1=w12[:, :, 0:1], op=ALU.subtract), r=["w12", "gw_"], w=["w12"])
    p.op("dve", lambda e: e.tensor_tensor(out=wexp[:], in0=oh1[:], in1=w12[:, :, 0:1].to_broadcast([128, 16, 8]), op=ALU.mult), r=["oh1", "w12"], w=["wexp"])
    p.op("dve", lambda e: e.tensor_tensor(out=ftmp[:], in0=oh2[:], in1=w12[:, :, 1:2].to_broadcast([128, 16, 8]), op=ALU.mult), r=["oh2", "w12"], w=["ftmp"])
    p.op("dve", lambda e: e.tensor_tensor(out=wexp[:], in0=wexp[:], in1=ftmp[:], op=ALU.add), r=["wexp", "ftmp"], w=["wexp"])
    for g in range(4):
        p.op("dve", lambda e, g=g: e.tensor_tensor(out=comb[:, :, 8 * g:8 * g + 8], in0=wexp[:], in1=goh[:, :, g:g + 1].to_broadcast([128, 16, 8]), op=ALU.mult),
             r=["wexp", "goh"], w=["comb"])
    p.op("dve", lambda e: e.memset(yy[:], 0.0), w=[("yy", tt_, hf_) for tt_ in range(16) for hf_ in range(2)])
    Wg_ = [ar.alloc("Wg_", [128, 8, 512], BF16) for _ in range(2)]
    Wu_ = [ar.alloc("Wu_", [128, 8, 512], BF16) for _ in range(2)]
    Wd_ = [ar.alloc("Wd_", [128, 4, 1024], BF16) for _ in range(2)]
    stgF = {"t": [ar.alloc("stgF", [128, 8, 128], F32) for _ in range(2)], "i": 0, "name": "F"}
    hid = ar.alloc("hid", [128, 4, NT], BF16)
    sg_ = [ar.alloc("sg_", [128, 512], F32) for _ in range(2)]
    nsg = 0
    for ex in range(NE):
        s = ex % 2
        load_weight(Wg_[s], 0, weg_d[ex], 8, 0, 512, stgF, 128)
        load_weight(Wu_[s], 0, weu_d[ex], 8, 0, 512, stgF, 128)
        viewd = wed_d[ex].rearrange("(kc p) c -> p kc c", p=128)
        for c in range(0, 1024, 256):
            s2 = stgF["i"] % 2
            stgF["i"] += 1
            st = stgF["t"][s2]
            key = ("stg", "F", s2)
            stv = st[:].rearrange("p k c -> p (k c)").rearrange("p (k c) -> p k c", k=4)
            p.op("sp", lambda e, stv=stv, c=c, viewd=viewd: e.dma_start(out=stv, in_=viewd[:, :, c:c + 256]), w=[key], dma=key)
            p.op("pool", lambda e, stv=stv, c=c, s=s: e.tensor_copy(out=Wd_[s][:, :, c:c + 256], in_=stv), r=[key], w=[("w", id(Wd_[s]), c)])
        for fc in range(4):
            for tb in range(4):
                sl = slice(tb * 512, (tb + 1) * 512)
                bg = nextbank()
                bu = nextbank()
                for (W, b) in ((Wg_[s], bg), (Wu_[s], bu)):
                    for kc in range(8):
                        p.op("pe", lambda e, W=W, b=b, kc=kc, fc=fc, sl=sl: e.matmul(ps[b][:], lhsT=W[:, kc, fc * 128:(fc + 1) * 128], rhs=tT[:, kc, sl], start=(kc == 0), stop=(kc == 7)),
                             r=wkeys(W, fc * 128, 128, 128), w=[PK(b)])
                sg = sg_[nsg % 2]
                sk = ("sg_", nsg % 2)
                nsg += 1
                p.op("act", lambda e, sg=sg, bg=bg: e.activation(out=sg[:], in_=ps[bg][:], func=AF.Silu), r=[PK(bg)], w=[sk])
                p.op("dve", lambda e, sg=sg, bu=bu, fc=fc, sl=sl: e.tensor_tensor(out=hid[:, fc, sl], in0=ps[bu][:], in1=sg[:], op=ALU.mult), r=[PK(bu), sk], w=[("hid", fc, tb)])
        for tt in range(16):
            for half in range(2):
                b = nextbank()
                for fc in range(4):
                    p.op("pe", lambda e, b=b, fc=fc, tt=tt, half=half, s=s: e.matmul(ps[b][:], lhsT=hid[:, fc, tt * 128:(tt + 1) * 128], rhs=Wd_[s][:, fc, half * 512:(half + 1) * 512],
                                                                                start=(fc == 0), stop=(fc == 3)),
                         r=[("hid", fc, tt // 4)] + wkeys(Wd_[s], half * 512, 512, 256), w=[PK(b)])
                ysl = yy[:, tt, half * 512:(half + 1) * 512]
                p.op("dve", lambda e, b=b, ysl=ysl, tt=tt, ex=ex: e.scalar_tensor_tensor(out=ysl, in0=ps[b][:], scalar=comb[:, tt, ex:ex + 1], in1=ysl, op0=ALU.mult, op1=ALU.add),
                     r=[PK(b), "comb", ("yy", tt, half)], w=[("yy", tt, half)])
    p.barrier()
    ar.reset(b3mark)

    Wpg = ar.alloc("Wpg", [128, 8, 1024], BF16)
    Wpp = ar.alloc("Wpp", [128, 2, 1024], BF16)
    stgG = {"t": [ar.alloc("stgG", [128, 8, 128], F32) for _ in range(3)], "i": 0, "name": "G"}
    load_weight(Wpg, 0, wpg_d, 8, 0, 1024, stgG, 128, engines=("pool", "act", "dve"))
    load_weight(Wpp, 0, wpp_d, 2, 0, 1024, stgG, 128, engines=("pool", "act", "dve"))
    pf = ar.alloc("pf", [128, 2, 512], F32)
    pb = ar.alloc("pb", [128, 2, 512], BF16)
    sq3 = ar.alloc("sq3", [128, 8, 512], BF16)
    t1c = ar.alloc("t1c", [128, 512], F32)
    rstdc = ar.alloc("rstdc", [128, 512], F32)
    t3 = ar.alloc("t3", [128, 8, 512], BF16)
    gte = ar.alloc("gte", [128, 512], F32)
    x1b = [ar.alloc("x1b", [128, 8, 512], F32) for _ in range(2)]
    pT_v = pT_own.rearrange("(kc p) t -> p kc t", p=128)
    outT_v = outT.rearrange("(kc p) t -> p kc t", p=128)
    for tb in range(4):
        s = tb % 2
        sl = slice(tb * 512, (tb + 1) * 512)
        xk = [("xb", s, fc) for fc in range(8)]
        p.op("sp", lambda e, s=s, sl=sl: e.dma_start(out=x1b[s][:], in_=X1_s[:, :, sl]), w=xk, dma=("x1b", s))
        p.op("sp", lambda e, sl=sl: e.dma_start(out=pf[:], in_=pT_v[:, :, sl]), w=["pf"], dma="pf")
        p.op("pool", lambda e: e.tensor_copy(out=pb[:], in_=pf[:]), r=["pf"], w=["pb"])
        for fc in range(8):
            b = nextbank()
            for q in range(4):
                tt = tb * 4 + q
                p.op("pe", lambda e, b=b, q=q, tt=tt, fc=fc: e.transpose(out=ps[b][:, q * 128:(q + 1) * 128], in_=yy[:, tt, fc * 128:(fc + 1) * 128], identity=ident_f[:]),
                     r=[], w=[PK(b)])
            p.op("dve", lambda e, b=b, fc=fc, s=s: e.tensor_tensor(out=x1b[s][:, fc, :], in0=ps[b][:], in1=x1b[s][:, fc, :], op=ALU.add), r=[PK(b), ("xb", s, fc)], w=[("xb", s, fc)])
        rms_stats(x1b[s][:], 8, 512, sq3[:], t1c[:], rstdc[:], xk, "C")
        for kc in range(8):
            p.op("dve", lambda e, kc=kc, s=s: e.scalar_tensor_tensor(out=t3[:, kc, :], in0=x1b[s][:, kc, :], scalar=vec[:, kc, GPLE:GPLE + 1], in1=rstdc[:], op0=ALU.mult, op1=ALU.mult),
                 r=["Crstd", ("xb", s, kc)], w=[("t3", kc)])
        for fc in range(8):
            bg = nextbank()
            bp = nextbank()
            for kc in range(8):
                p.op("pe", lambda e, bg=bg, kc=kc, fc=fc: e.matmul(ps[bg][:], lhsT=Wpg[:, kc, fc * 128:(fc + 1) * 128], rhs=t3[:, kc, :], start=(kc == 0), stop=(kc == 7)),
                     r=[("t3", kc)] + wkeys(Wpg, fc * 128, 128, 128), w=[PK(bg)])
            for kc in range(2):
                p.op("pe", lambda e, bp=bp, kc=kc, fc=fc: e.matmul(ps[bp][:], lhsT=Wpp[:, kc, fc * 128:(fc + 1) * 128], rhs=pb[:, kc, :], start=(kc == 0), stop=(kc == 1)),
                     r=["pb"] + wkeys(Wpp, fc * 128, 128, 128), w=[PK(bp)])
            p.op("act", lambda e, bg=bg: e.activation(out=gte[:], in_=ps[bg][:], func=AF.Sigmoid), r=[PK(bg)], w=["gte"])
            p.op("dve", lambda e, bp=bp: e.tensor_tensor(out=gte[:], in0=ps[bp][:], in1=gte[:], op=ALU.mult), r=[PK(bp), "gte"], w=["gte"])
            p.op("pool", lambda e, fc=fc, s=s: e.tensor_tensor(out=x1b[s][:, fc, :], in0=gte[:], in1=x1b[s][:, fc, :], op=ALU.add), r=["gte", ("xb", s, fc)], w=[("xb", s, fc)])
        rms_stats(x1b[s][:], 8, 512, sq3[:], t1c[:], rstdc[:], xk, "C")
        for kc in range(8):
            p.op("dve", lambda e, kc=kc, s=s: e.scalar_tensor_tensor(out=x1b[s][:, kc, :], in0=x1b[s][:, kc, :], scalar=vec[:, kc, GFIN:GFIN + 1], in1=rstdc[:], op0=ALU.mult, op1=ALU.mult),
                 r=[("xb", s, kc), "Crstd"], w=[("xb", s, kc)])
        p.op("sp", lambda e, s=s, sl=sl: e.dma_start(out=outT_v[:, :, sl], in_=x1b[s][:]), r=xk, w=[("out", tb)], dma=("x1b", s))
    return finish(nc, p, outT, ar)


def finish(nc, p, outT, ar):
    p.barrier()
    p.emit()
    return nc


def _mask_for(r):
    m = np.zeros((128, 16, 512), np.float32)
    ki = np.arange(128)[:, None]
    qi = np.arange(128)[None, :]
    tri = np.where(ki > qi, 0.0, 1.0).astype(np.float32)
    m[:] = 1.0
    for tau in range(16):
        for mq in range(4):
            tq = 4 * mq + r
            if tau > tq:
                m[:, tau, mq * 128:(mq + 1) * 128] = 0.0
            elif tau == tq:
                m[:, tau, mq * 128:(mq + 1) * 128] = tri
    return m.astype(ml_dtypes.bfloat16)


def _chunked(v):
    return np.ascontiguousarray(np.asarray(v, np.float32).reshape(8, 128).T)


def prepare_inputs(x, p, g_mix, w_in, lam_q1, lam_k1, lam_q2, lam_k2, g_subln, w_conv, b_conv,
                   w_rg, b_rg, w_ig, b_ig, lru_lambda, w_attn_br, w_lru_br, w_out, g_moe,
                   w_rt_group, b_rt_group, w_rt_expert, b_rt_expert, w_e_gate, w_e_up, w_e_down,
                   g_ple, w_ple_gate, w_ple_proj, g_final):
    f = lambda a: np.asarray(a, np.float32)
    x = f(x); p = f(p)
    vecs = np.zeros((128, 8, 12), np.float32)
    vecs[:, :, 0] = _chunked(f(g_mix)[0]); vecs[:, :, 1] = _chunked(f(g_moe)[0]); vecs[:, :, 2] = _chunked(f(g_ple)[0])
    vecs[:, :, 3] = _chunked(f(g_final)); vecs[:, :, 4] = _chunked(f(b_conv)[0]); vecs[:, :, 5] = _chunked(f(b_rg)[0].reshape(-1))
    vecs[:, :, 6] = _chunked(f(b_ig)[0].reshape(-1)); vecs[:, :, 7] = _chunked(f(lru_lambda)[0])
    for j in range(4):
        vecs[:, :, 8 + j] = _chunked(f(w_conv)[0, j])

    def blockdiag(w):
        w = f(w)[0]
        o = np.zeros((128, 8, 128), np.float32)
        for n in range(16):
            cc, hf = n // 2, n % 2
            o[hf * 64:(hf + 1) * 64, cc, hf * 64:(hf + 1) * 64] = w[n]
        return o

    w_rt = np.concatenate([f(w_rt_group)[0]] + [f(w_rt_expert)[0, g] for g in range(4)], axis=1)
    b_rt = np.concatenate([f(b_rt_group)[0], f(b_rt_expert)[0].reshape(-1)])[None, :]
    common = dict(
        w_in=np.ascontiguousarray(f(w_in)[0]),
        vecs=vecs,
        gsub=np.ascontiguousarray(f(g_subln)[0][:, None]),
        lamv=np.ascontiguousarray(np.broadcast_to(np.stack([f(lam_q1)[0], f(lam_k1)[0], f(lam_q2)[0], f(lam_k2)[0]])[None], (128, 4, 64))),
        brt=np.ascontiguousarray(np.broadcast_to(b_rt, (128, 36))),
        wrg_bd=blockdiag(w_rg), wig_bd=blockdiag(w_ig),
        w_attn_br=np.ascontiguousarray(f(w_attn_br)[0]), w_lru_br=np.ascontiguousarray(f(w_lru_br)[0]), w_out=np.ascontiguousarray(f(w_out)[0]),
        w_rt=np.ascontiguousarray(w_rt),
        w_e_gate=np.ascontiguousarray(f(w_e_gate)[0]), w_e_up=np.ascontiguousarray(f(w_e_up)[0]), w_e_down=np.ascontiguousarray(f(w_e_down)[0]),
        w_ple_gate=np.ascontiguousarray(f(w_ple_gate)[0]), w_ple_proj=np.ascontiguousarray(f(w_ple_proj)[0]),
    )
    in_maps = []
    toks = []
    for c in range(8):
        b, r = c // 4, c % 4
        own = (np.arange(16)[:, None] * 4 + r) * 128 + np.arange(128)[None, :]
        own = own.reshape(-1)
        toks.append((b, own))
        selv = np.zeros((128, 4), np.float32)
        selv[:, r] = 1.0
        m = dict(common)
        m.update(
            xT_full=np.ascontiguousarray(x[b].T),
            xT_own=np.ascontiguousarray(x[b][own].T),
            pT_own=np.ascontiguousarray(p[0, b][own].T),
            maskB=_mask_for(r),
            sel=selv,
        )
        in_maps.append(m)
    return in_maps, toks


_NC_CACHE = {}


def kernel(**inputs):
    in_maps, toks = prepare_inputs(**inputs)
    if "nc" not in _NC_CACHE:
        _NC_CACHE["nc"] = build_program()
    nc = _NC_CACHE["nc"]
    res = run_bass_kernel_spmd(nc, in_maps, core_ids=list(range(8)))
    out = np.zeros((2, S, D), np.float32)
    for c in range(8):
        b, own = toks[c]
        out[b, own, :] = res.results[c]["outT"].T
    if DEBUG:
        kernel.last = res.results
    return out
```

```python
import os
import numpy as np
import ml_dtypes
import concourse.bass as bass
import concourse.mybir as mybir
from concourse.bass_utils import run_bass_kernel_spmd

AF = mybir.ActivationFunctionType
ALU = mybir.AluOpType
AX = mybir.AxisListType
F32 = mybir.dt.float32
BF16 = mybir.dt.bfloat16

S = 8192
D = 1024
NT = 2048
NE = 32
FF = 512
NEG = 0.0


class Prog:
    ENGS = ("sp", "act", "pe", "dve", "pool")

    def __init__(self, nc):
        self.nc = nc
        self.ops = []
        self.lastw = {}
        self.readers = {}
        self._nbar = 0

    def op(self, eng, fn, r=(), w=(), dma=None):
        idx = len(self.ops)
        deps = set()
        for k in r:
            lw = self.lastw.get(k)
            if lw is not None:
                deps.add(lw)
        for k in w:
            lw = self.lastw.get(k)
            if lw is not None:
                deps.add(lw)
            rd = self.readers.get(k)
            if rd:
                deps.update(rd)
        for k in r:
            self.readers.setdefault(k, []).append(idx)
        for k in w:
            self.lastw[k] = idx
            self.readers[k] = []
        self.ops.append(dict(eng=eng, fn=fn, deps=deps, dma=dma))
        return idx

    def barrier(self):
        n = self._nbar
        self._nbar += 1
        last_dma = {}
        for i, o in enumerate(self.ops):
            if o["dma"] is not None:
                last_dma[o["dma"]] = i
        for e in ("act", "pe", "dve", "pool"):
            i = self.op(e, lambda eng: eng.drain(), w=[("bar", n, e)])
            self.ops[i]["deps"].update(last_dma.values())
        for e in self.ENGS:
            self.op(e, lambda eng: eng.nop(), r=[("bar", n, x) for x in ("act", "pe", "dve", "pool")])
        self.lastw = {}
        self.readers = {}

    def emit(self):
        nc = self.nc
        ops = self.ops

        def skip(od, o):
            return od["eng"] == "pe" and o["eng"] == "pe" and od["dma"] is None and o["dma"] is None

        needed = set()
        for i, o in enumerate(ops):
            for d in o["deps"]:
                if not skip(ops[d], o):
                    needed.add(d)
        eng_cnt = {e: 0 for e in self.ENGS}
        dma_cnt = {}
        dma_keys = []
        for i, o in enumerate(ops):
            if o["dma"] is not None:
                k = o["dma"]
                if k not in dma_cnt:
                    dma_cnt[k] = 0
                    dma_keys.append(k)
                dma_cnt[k] += 16
                o["ev"] = ("dma", k, dma_cnt[k])
            elif i in needed:
                eng_cnt[o["eng"]] += 1
                o["ev"] = ("eng", o["eng"], eng_cnt[o["eng"]])
            else:
                o["ev"] = None
        cur = {}
        for i, o in enumerate(ops):
            waits = {}
            for d in o["deps"]:
                od = ops[d]
                if skip(od, o):
                    continue
                ev = od["ev"]
                if ev[0] == "dma":
                    key = ("dma", ev[1])
                    val = cur[ev[1]]
                else:
                    key = ("eng", ev[1])
                    val = ev[2]
                if waits.get(key, 0) < val:
                    waits[key] = val
            o["waits"] = waits
            if o["dma"] is not None:
                cur[o["dma"]] = o["ev"][2]
        sems = {}
        for e in ("act", "pe", "dve", "pool"):
            sems[("eng", e)] = nc.alloc_semaphore("s_" + e)
        for n, k in enumerate(dma_keys):
            sems[("dma", k)] = nc.alloc_semaphore("d%d" % n)
        streams = {e: [] for e in self.ENGS}
        for o in ops:
            streams[o["eng"]].append(o)

        def run_stream(eng_obj, lst):
            waited = {}
            for o in lst:
                for key, val in o["waits"].items():
                    if waited.get(key, 0) >= val:
                        continue
                    waited[key] = val
                    eng_obj.wait_ge(sems[key], val)
                ins = o["fn"](eng_obj)
                ev = o["ev"]
                if ev is not None:
                    if ev[0] == "dma":
                        ins.then_inc(sems[("dma", ev[1])], 16)
                    else:
                        ins.then_inc(sems[("eng", ev[1])], 1)

        with nc.Block() as block:
            block.sync(lambda e: run_stream(e, streams["sp"]))
            block.scalar(lambda e: run_stream(e, streams["act"]))
            block.tensor(lambda e: run_stream(e, streams["pe"]))
            block.vector(lambda e: run_stream(e, streams["dve"]))
            block.gpsimd(lambda e: run_stream(e, streams["pool"]))


class Arena:
    BASE = 16512
    TOP = 229344

    def __init__(self, nc):
        self.nc = nc
        self.off = self.BASE
        self.n = 0

    def alloc(self, name, shape, dt):
        nb = int(np.prod(shape[1:])) * (4 if dt == F32 else 2)
        nb = (nb + 31) // 32 * 32
        assert self.off + nb <= self.TOP, (name, self.off, nb)
        self.n += 1
        t = self.nc.alloc_sbuf_tensor_at("%s_%d" % (name, self.n), list(shape), dt, offset=self.off)
        self.off += nb
        return t

    def mark(self):
        return self.off

    def reset(self, m):
        self.off = m


DEBUG = bool(int(os.environ.get("MK_DEBUG", "0")))
STOP_AFTER = os.environ.get("MK_STOP", "")


def build_program():
    nc = bass.Bass("TRN2", target_bir_lowering=False)
    p = Prog(nc)
    ar = Arena(nc)

    def din(name, shape, dt=F32):
        return nc.dram_tensor(name, list(shape), dt, kind="ExternalInput").ap()

    skind = "ExternalOutput" if DEBUG else "Internal"

    def dscr(name, shape, dt):
        return nc.dram_tensor(name, list(shape), dt, kind=skind).ap()

    xT_full = din("xT_full", [D, S])
    xT_own = din("xT_own", [D, NT])
    pT_own = din("pT_own", [256, NT])
    maskB_d = din("maskB", [128, 16, 512], BF16)
    sel_d = din("sel", [128, 4])
    w_in = din("w_in", [D, 7168])
    vec_d = din("vecs", [128, 8, 12])
    gsub_d = din("gsub", [128, 1])
    lamv_d = din("lamv", [128, 4, 64])
    brt_d = din("brt", [128, 36])
    wrg_d = din("wrg_bd", [128, 8, 128])
    wig_d = din("wig_bd", [128, 8, 128])
    wab_d = din("w_attn_br", [D, D])
    wlb_d = din("w_lru_br", [D, D])
    wout_d = din("w_out", [D, D])
    wrt_d = din("w_rt", [D, 36])
    weg_d = din("w_e_gate", [NE, D, FF])
    weu_d = din("w_e_up", [NE, D, FF])
    wed_d = din("w_e_down", [NE, FF, D])
    wpg_d = din("w_ple_gate", [D, D])
    wpp_d = din("w_ple_proj", [256, D])
    outT = nc.dram_tensor("outT", [D, NT], F32, kind="ExternalOutput").ap()

    KT_s = dscr("KT_s", [8, 128, S], BF16)
    XR_s = dscr("XR_s", [8, 128, S], BF16)
    V_s = dscr("V_s", [8, 128, 64, 128], BF16)
    HTO_s = dscr("HTO_s", [128, 8, NT], BF16)
    LRU_s = dscr("LRU_s", [128, 8, NT], BF16)
    OT_s = dscr("OT_s", [128, 8, NT], BF16)
    X1_s = dscr("X1_s", [128, 8, NT], F32)

    class Bank:
        def __init__(self, t, off):
            self.t, self.off = t, off

        def __getitem__(self, key):
            if not isinstance(key, tuple):
                key = (key, slice(None))
            pk, ck = key
            c0 = ck.start or 0
            c1 = 512 if ck.stop is None else ck.stop
            return self.t[pk, self.off + c0:self.off + c1]

    _p01 = [nc.alloc_psum_tensor("psb%d" % i, [128, 512], F32) for i in range(2)]
    big = [nc.alloc_psum_tensor("psbig%d" % i, [128, 1024], F32) for i in range(2)]
    _p67 = [nc.alloc_psum_tensor("psb%d" % i, [128, 512], F32) for i in (6, 7)]
    ps = _p01 + [Bank(big[0], 0), Bank(big[0], 512), Bank(big[1], 0), Bank(big[1], 512)] + _p67
    rr = [0]

    def nextbank(lo=1, hi=8):
        b = lo + rr[0] % (hi - lo)
        rr[0] += 1
        return b

    def PK(i):
        return ("ps", i)

    ones_bf = ar.alloc("ones_bf", [128, 128], BF16)
    ident_bf = ar.alloc("ident_bf", [128, 128], BF16)
    ident_f = ar.alloc("ident_f", [128, 128], F32)
    maskB = ar.alloc("maskB", [128, 16, 512], BF16)
    vec = ar.alloc("vec", [128, 8, 12], F32)
    gsub = ar.alloc("gsub", [128, 1], F32)
    lamv = ar.alloc("lamv", [128, 4, 64], F32)
    brt = ar.alloc("brt", [128, 36], F32)
    sel = ar.alloc("sel", [128, 4], F32)
    der = ar.alloc("der", [128, 8, 4], F32)
    sc = ar.alloc("sc", [128, 8], F32)
    lprod = ar.alloc("lprod", [128, 2, 64], F32)
    GM, GMOE, GPLE, GFIN, BCONV, BRG, BIG, LAM, WC0 = 0, 1, 2, 3, 4, 5, 6, 7, 8

    p.op("sp", lambda e: e.dma_start(out=maskB[:], in_=maskB_d), w=["maskB"], dma="c0")
    p.op("sp", lambda e: e.dma_start(out=vec[:], in_=vec_d), w=["vec"], dma="c1")
    p.op("sp", lambda e: e.dma_start(out=gsub[:], in_=gsub_d), w=["gsub"], dma="c1")
    p.op("sp", lambda e: e.dma_start(out=lamv[:], in_=lamv_d), w=["lamv"], dma="c1")
    p.op("sp", lambda e: e.dma_start(out=brt[:], in_=brt_d), w=["brt"], dma="c1")
    p.op("sp", lambda e: e.dma_start(out=sel[:], in_=sel_d), w=["sel"], dma="c1")
    p.op("pool", lambda e: e.memset(ident_f[:], 1.0), w=["ident_f"])
    p.op("pool", lambda e: e.affine_select(out=ident_f[:], in_=ident_f[:], pattern=[[1, 128]], compare_op=ALU.is_equal,
                                           fill=0.0, base=0, channel_multiplier=-1), r=["ident_f"], w=["ident_f"])
    p.op("dve", lambda e: e.tensor_copy(out=ident_bf[:], in_=ident_f[:]), r=["ident_f"], w=["ident_bf"])
    p.op("dve", lambda e: e.memset(ones_bf[:], 1.0), w=["ones_bf"])
    p.op("dve", lambda e: e.tensor_scalar(out=der[:, :, 0], in0=vec[:, :, BRG], scalar1=0.5, scalar2=None, op0=ALU.mult), r=["vec"], w=["der0"])
    p.op("dve", lambda e: e.tensor_scalar(out=der[:, :, 1], in0=vec[:, :, BIG], scalar1=0.5, scalar2=None, op0=ALU.mult), r=["vec"], w=["der1"])
    p.op("act", lambda e: e.activation(out=der[:, :, 3], in_=vec[:, :, LAM], func=AF.Exp, scale=-1.0), r=["vec"], w=["der3"])
    p.op("dve", lambda e: e.tensor_scalar(out=der[:, :, 2], in0=der[:, :, 3], scalar1=0.2, scalar2=None, op0=ALU.mult), r=["der3"], w=["der2"])
    for cst in (-0.25, 1.0 / 3.0, -0.5, 1.0):
        p.op("dve", lambda e, cst=cst: e.scalar_tensor_tensor(out=der[:, :, 2], in0=der[:, :, 2], scalar=cst, in1=der[:, :, 3], op0=ALU.add, op1=ALU.mult),
             r=["der2", "der3"], w=["der2"])
    p.op("dve", lambda e: e.tensor_scalar(out=der[:, :, 2], in0=der[:, :, 2], scalar1=-4.0, scalar2=None, op0=ALU.mult), r=["der2"], w=["der2"])
    p.op("dve", lambda e: e.tensor_tensor(out=lprod[:, 0, :], in0=lamv[:, 0, :], in1=lamv[:, 1, :], op=ALU.mult), r=["lamv"], w=["lprod"])
    p.op("dve", lambda e: e.tensor_tensor(out=lprod[:, 1, :], in0=lamv[:, 2, :], in1=lamv[:, 3, :], op=ALU.mult), r=["lamv", "lprod"], w=["lprod"])
    p.op("dve", lambda e: e.reduce_sum(out=sc[:, 2:4], in_=lprod[:], axis=AX.X), r=["lprod"], w=["sc23"])
    p.op("act", lambda e: e.activation(out=sc[:, 4:6], in_=sc[:, 2:4], func=AF.Exp), r=["sc23"], w=["sc45"])
    p.op("dve", lambda e: e.tensor_tensor(out=sc[:, 0:1], in0=sc[:, 5:6], in1=sc[:, 4:5], op=ALU.subtract), r=["sc45"], w=["sc0"])
    p.op("dve", lambda e: e.tensor_scalar(out=sc[:, 0:1], in0=sc[:, 0:1], scalar1=-0.2, scalar2=None, op0=ALU.add), r=["sc0"], w=["sc0"])
    p.op("dve", lambda e: e.tensor_scalar(out=sc[:, 1:2], in0=gsub[:], scalar1=0.8, scalar2=None, op0=ALU.mult), r=["gsub"], w=["sc1"])
    CONST_KEYS = ["maskB", "vec", "sel", "brt", "ident_f", "ident_bf", "ones_bf", "der0", "der1", "der2", "sc0", "sc1"]

    def consts_barrier():
        p.barrier()

    consts_barrier()
    gmark = ar.mark()

    def load_weight(dst, dst_c0, src2d, nk, col0, ncols, stg, piece, engines=("pool",)):
        view = src2d.rearrange("(kc p) c -> p kc c", p=128)
        ns = len(stg["t"])
        for c in range(0, ncols, piece):
            s = stg["i"] % ns
            eng = engines[stg["i"] % len(engines)]
            stg["i"] += 1
            st = stg["t"][s]
            key = ("stg", stg["name"], s)
            p.op("sp", lambda e, st=st, c=c: e.dma_start(out=st[:, 0:nk, 0:piece], in_=view[:, :, col0 + c:col0 + c + piece]),
                 w=[key], dma=key)
            if eng == "act":
                p.op("act", lambda e, st=st, c=c: e.copy(out=dst[:, 0:nk, dst_c0 + c:dst_c0 + c + piece], in_=st[:, 0:nk, 0:piece]),
                     r=[key], w=[("w", id(dst), dst_c0 + c)])
            else:
                p.op(eng, lambda e, st=st, c=c: e.tensor_copy(out=dst[:, 0:nk, dst_c0 + c:dst_c0 + c + piece], in_=st[:, 0:nk, 0:piece]),
                     r=[key], w=[("w", id(dst), dst_c0 + c)])

    def wkeys(dst, c0, n, piece):
        return [("w", id(dst), c) for c in range(c0 // piece * piece, c0 + n, piece)]

    evq = [0]

    def evac(out_ap, in_ap, rk, wk):
        evq[0] += 1
        if evq[0] % 2:
            p.op("act", lambda e: e.copy(out=out_ap, in_=in_ap), r=rk, w=wk)
        else:
            p.op("dve", lambda e: e.tensor_copy(out=out_ap, in_=in_ap), r=rk, w=wk)

    def rms_stats(src, nkc, nblk, sq, t1, rstd, rk, tag, eps=1e-6, dim=1024.0):
        p.op("act", lambda e: e.activation(out=sq, in_=src, func=AF.Square), r=rk, w=[tag + "sq"])
        for kc in range(nkc):
            p.op("pe", lambda e, kc=kc: e.matmul(ps[0][:, 0:nblk], lhsT=ones_bf[:], rhs=sq[:, kc, :], start=(kc == 0), stop=(kc == nkc - 1)),
                 r=[tag + "sq"], w=[PK(0)])
        p.op("act", lambda e: e.activation(out=t1, in_=ps[0][:, 0:nblk], func=AF.Ln, bias=eps, scale=1.0 / dim), r=[PK(0)], w=[tag + "t1"])
        p.op("act", lambda e: e.activation(out=rstd, in_=t1, func=AF.Exp, scale=-0.5), r=[tag + "t1"], w=[tag + "rstd"])

    Wk = ar.alloc("Wk", [128, 8, 1024], BF16)
    Wv = ar.alloc("Wv", [128, 8, 1024], BF16)
    Wx = ar.alloc("Wx", [128, 8, 1024], BF16)
    stgA = {"t": [ar.alloc("stgA", [128, 8, 256], F32) for _ in range(3)], "i": 0, "name": "A"}
    load_weight(Wk, 0, w_in, 8, 1024, 1024, stgA, 256, engines=("pool", "act", "dve"))
    load_weight(Wv, 0, w_in, 8, 2048, 1024, stgA, 256, engines=("pool", "act", "dve"))
    load_weight(Wx, 0, w_in, 8, 3072, 1024, stgA, 256, engines=("pool", "act", "dve"))
    xf = [ar.alloc("xf", [128, 8, 512], F32) for _ in range(2)]
    sq = ar.alloc("sq", [128, 8, 512], BF16)
    t1 = ar.alloc("t1", [128, 512], F32)
    rstd = ar.alloc("rstd", [128, 512], F32)
    hT = [ar.alloc("hT", [128, 8, 512], BF16) for _ in range(2)]
    kst = [ar.alloc("kst", [128, 8, 512], BF16) for _ in range(2)]
    xst = [ar.alloc("xst", [128, 8, 512], BF16) for _ in range(2)]
    vst = [ar.alloc("vst", [128, 4, 1024], BF16) for _ in range(2)]
    xTf_v = xT_full.rearrange("(kc p) t -> p kc t", p=128)
    xTo_v = xT_own.rearrange("(kc p) t -> p kc t", p=128)
    KT_v = KT_s.rearrange("h p t -> p h t")
    XR_v = XR_s.rearrange("h p t -> p h t")
    V_v = V_s.rearrange("h p t v -> p t h v")

    def norm_block(src_view, tb, s):
        p.op("sp", lambda e: e.dma_start(out=xf[s][:], in_=src_view[:, :, tb * 512:(tb + 1) * 512]), w=[("xf", s)], dma=("xf", s))
        rms_stats(xf[s][:], 8, 512, sq[:], t1[:], rstd[:], [("xf", s)], "A")
        for kc in range(8):
            p.op("dve", lambda e, kc=kc: e.scalar_tensor_tensor(out=hT[s][:, kc, :], in0=xf[s][:, kc, :], scalar=vec[:, kc, GM:GM + 1],
                                                              in1=rstd[:], op0=ALU.mult, op1=ALU.mult),
                 r=[("xf", s), "Arstd"], w=[("hT", s, kc)])

    def a1_compute(kind, tb, s):
        if kind == "own":
            p.op("pool", lambda e: e.dma_start(out=HTO_s[:, :, tb * 512:(tb + 1) * 512], in_=hT[s][:]),
                 r=[("hT", s, kc) for kc in range(8)], w=[("HTO", tb)], dma=("hTst", s))
            return
        for (W, st, keyn) in ((Wk, kst, "kst"), (Wx, xst, "xst")):
            for h in range(8):
                b = nextbank()
                for kc in range(8):
                    p.op("pe", lambda e, W=W, b=b, kc=kc, h=h: e.matmul(ps[b][:], lhsT=W[:, kc, h * 128:(h + 1) * 128], rhs=hT[s][:, kc, :],
                                                                   start=(kc == 0), stop=(kc == 7)),
                         r=[("hT", s, kc)] + wkeys(W, h * 128, 128, 256), w=[PK(b)])
                evac(st[s][:, h, :], ps[b][:], [PK(b)], [(keyn, s, h)])
        p.op("pool", lambda e: e.dma_start(out=KT_v[:, :, tb * 512:(tb + 1) * 512], in_=kst[s][:]), r=[("kst", s, h) for h in range(8)], w=[("KT", tb)], dma=("kst", s))
        p.op("pool", lambda e: e.dma_start(out=XR_v[:, :, tb * 512:(tb + 1) * 512], in_=xst[s][:]), r=[("xst", s, h) for h in range(8)], w=[("XR", tb)], dma=("xst", s))
        for tt in range(4):
            for half in range(2):
                b = nextbank()
                for kc in range(8):
                    p.op("pe", lambda e, b=b, kc=kc, tt=tt, half=half: e.matmul(ps[b][:], lhsT=hT[s][:, kc, tt * 128:(tt + 1) * 128],
                                                                           rhs=Wv[:, kc, half * 512:(half + 1) * 512], start=(kc == 0), stop=(kc == 7)),
                         r=[("hT", s, kc)] + wkeys(Wv, half * 512, 512, 256), w=[PK(b)])
                evac(vst[s][:, tt, half * 512:(half + 1) * 512], ps[b][:], [PK(b)], [("vst", s, tt, half)])
        for tt in range(4):
            p.op("pool", lambda e, tt=tt: e.dma_start(out=V_v[:, tb * 4 + tt], in_=vst[s][:, tt, :].rearrange("p (h v) -> p h v", h=8)),
                 r=[("vst", s, tt, half) for half in range(2)], w=[("V", tb, tt)], dma=("vst", s))

    jobs = [("own", tb) for tb in range(4)] + [("full", tb) for tb in range(16)]
    norm_block(xTo_v, 0, 0)
    for n, (kind, tb) in enumerate(jobs):
        if n + 1 < len(jobs):
            k2, tb2 = jobs[n + 1]
            norm_block(xTo_v if k2 == "own" else xTf_v, tb2, (n + 1) % 2)
        a1_compute(kind, tb, n % 2)
    p.barrier()
    ar.reset(gmark)
    if STOP_AFTER == "A1":
        return finish(nc, p, outT, ar)

    Wgr = ar.alloc("Wgr", [128, 8, 1024], BF16)
    Wrg = ar.alloc("Wrg", [128, 8, 128], BF16)
    Wig = ar.alloc("Wig", [128, 8, 128], BF16)
    stgB = {"t": [ar.alloc("stgB", [128, 8, 128], F32) for _ in range(3)], "i": 0, "name": "B"}
    load_weight(Wgr, 0, w_in, 8, 4096, 1024, stgB, 128, engines=("pool", "act", "dve"))
    for (Wt, src, nm) in ((Wrg, wrg_d, "wrg"), (Wig, wig_d, "wig")):
        s = stgB["i"] % len(stgB["t"])
        stgB["i"] += 1
        st = stgB["t"][s]
        key = ("stg", "B", s)
        p.op("sp", lambda e, st=st, src=src: e.dma_start(out=st[:, :, 0:128], in_=src), w=[key], dma=key)
        p.op("pool", lambda e, st=st, Wt=Wt: e.tensor_copy(out=Wt[:], in_=st[:, :, 0:128]), r=[key], w=[nm])
    Dm = ar.alloc("Dm", [128, 8, 4, 128], BF16)
    for cc in range(8):
        for j in range(4):
            p.op("dve", lambda e, cc=cc, j=j: e.tensor_scalar(out=Dm[:, cc, j, :], in0=ident_f[:], scalar1=vec[:, cc, WC0 + j:WC0 + j + 1], scalar2=None, op0=ALU.mult),
                 w=[("Dm", cc, j)])
    xrb = [ar.alloc("xrb", [128, 4, 2048], BF16) for _ in range(2)]
    xcb = [ar.alloc("xcb", [128, 2048], BF16) for _ in range(2)]
    rt = [ar.alloc("rt", [128, 2048], F32) for _ in range(2)]
    it = [ar.alloc("it", [128, 2048], BF16) for _ in range(2)]
    aa = [ar.alloc("aa", [128, 2048], F32) for _ in range(2)]
    uu = ar.alloc("uu", [128, 2048], F32)
    hh = ar.alloc("hh", [128, 2048], F32)
    hTo2 = [ar.alloc("hTo", [128, 8, 512], BF16) for _ in range(2)]
    carry = ar.alloc("carry", [128, 1], F32)
    hsel = ar.alloc("hsel", [128, 512], F32)
    gz2 = [ar.alloc("gz", [128, 512], F32) for _ in range(2)]
    gw = ar.alloc("gw", [128, 512], F32)
    gt2 = [ar.alloc("gt", [128, 512], F32) for _ in range(2)]
    lst = [ar.alloc("lst", [128, 512], BF16) for _ in range(2)]

    def a2_stage1(cc, tb2, s):
        K = lambda nm: (nm, s)
        t0 = tb2 * 2048
        hTo = hTo2[s]
        gt = gt2[s]
        p.op("sp", lambda e: e.dma_start(out=hTo[:], in_=HTO_s[:, :, tb2 * 512:(tb2 + 1) * 512]), w=[("hTo", s)], dma=("hTo", s))
        for j in range(4):
            if tb2 == 0 and j < 3:
                p.op("dve", lambda e, j=j: e.memset(xrb[s][:, j, 0:3 - j], 0.0), w=[("xrb", s, j)])
                p.op("sp", lambda e, j=j: e.dma_start(out=xrb[s][:, j, 3 - j:2048], in_=XR_s[cc, :, 0:2048 - (3 - j)]), w=[("xrb", s, j)], dma=("xrb", s))
            else:
                p.op("sp", lambda e, j=j: e.dma_start(out=xrb[s][:, j, :], in_=XR_s[cc, :, t0 - 3 + j:t0 - 3 + j + 2048]), w=[("xrb", s, j)], dma=("xrb", s))
        cb = []
        for nb in range(4):
            sl = slice(nb * 512, (nb + 1) * 512)
            b = nextbank()
            cb.append(b)
            for j in range(4):
                p.op("pe", lambda e, b=b, j=j, sl=sl: e.matmul(ps[b][:], lhsT=Dm[:, cc, j, :], rhs=xrb[s][:, j, sl], start=(j == 0), stop=(j == 3)),
                     r=[("xrb", s, j), ("Dm", cc, j)], w=[PK(b)])
        for nb in range(4):
            sl = slice(nb * 512, (nb + 1) * 512)
            b = cb[nb]
            if nb % 2 == 0:
                p.op("act", lambda e, b=b, sl=sl: e.activation(out=xcb[s][:, sl], in_=ps[b][:], func=AF.Identity, bias=vec[:, cc, BCONV:BCONV + 1], scale=1.0),
                     r=[PK(b)], w=[("xcb", s, nb)])
            else:
                p.op("dve", lambda e, b=b, sl=sl: e.tensor_scalar(out=xcb[s][:, sl], in0=ps[b][:], scalar1=vec[:, cc, BCONV:BCONV + 1], scalar2=None, op0=ALU.add),
                     r=[PK(b)], w=[("xcb", s, nb)])
        bq = nextbank()
        for kc in range(8):
            p.op("pe", lambda e, kc=kc: e.matmul(ps[bq][:], lhsT=Wgr[:, kc, cc * 128:(cc + 1) * 128], rhs=hTo[:, kc, :], start=(kc == 0), stop=(kc == 7)),
                 r=[("hTo", s)] + wkeys(Wgr, cc * 128, 128, 128), w=[PK(bq)])
        gz = gz2[s]
        p.op("act", lambda e: e.copy(out=gz[:], in_=ps[bq][:]), r=[PK(bq)], w=[("gz", s)])
        p.op("act", lambda e: e.activation(out=gw[:], in_=ps[bq][:], func=AF.Square), r=[PK(bq)], w=["gw"])
        p.op("pool", lambda e: e.tensor_scalar(out=gw[:], in0=gw[:], scalar1=0.044715, scalar2=1.0, op0=ALU.mult, op1=ALU.add), r=["gw"], w=["gw"])
        p.op("pool", lambda e: e.tensor_tensor(out=gw[:], in0=gw[:], in1=gz[:], op=ALU.mult), r=["gw", ("gz", s)], w=["gw"])
        for nb in range(4):
            sl = slice(nb * 512, (nb + 1) * 512)
            for (Wt, nm, dst, hb) in ((Wrg, "wrg", rt, 0), (Wig, "wig", it, 1)):
                b2 = nextbank()
                p.op("pe", lambda e, b2=b2, Wt=Wt, sl=sl: e.matmul(ps[b2][:], lhsT=Wt[:, cc, :], rhs=xcb[s][:, sl], start=True, stop=True),
                     r=[("xcb", s, nb), nm], w=[PK(b2)])
                p.op("act", lambda e, b2=b2, dst=dst, sl=sl, hb=hb: e.activation(out=dst[s][:, sl], in_=ps[b2][:], func=AF.Tanh, bias=der[:, cc, hb:hb + 1], scale=0.5),
                     r=[PK(b2)], w=[("g", s, hb, nb)])
        p.op("act", lambda e: e.activation(out=gt[:], in_=gw[:], func=AF.Tanh, scale=0.7978845608028654), r=["gw"], w=[("gt", s)])
        gk = [("g", s, 0, nb) for nb in range(4)]
        p.op("act", lambda e: e.activation(out=aa[s][:], in_=rt[s][:], func=AF.Exp, bias=der[:, cc, 2:3], scale=der[:, cc, 2:3]), r=gk, w=[K("aa")])
        p.op("pool", lambda e: e.tensor_tensor(out=rt[s][:], in0=aa[s][:], in1=aa[s][:], op=ALU.mult), r=[K("aa")], w=gk)
        p.op("act", lambda e: e.activation(out=rt[s][:], in_=rt[s][:], func=AF.Sqrt, bias=0.25, scale=-0.25), r=gk, w=gk)

    def a2_stage2(cc, tb2, s, n):
        K = lambda nm: (nm, s)
        gk = [("g", s, 0, nb) for nb in range(4)]
        ik = [("g", s, 1, nb) for nb in range(4)]
        xk = [("xcb", s, nb) for nb in range(4)]
        gz = gz2[s]
        gt = gt2[s]
        p.op("dve", lambda e: e.scalar_tensor_tensor(out=gt[:], in0=gt[:], scalar=1.0, in1=gz[:], op0=ALU.add, op1=ALU.mult), r=[("gt", s), ("gz", s)], w=[("gt", s)])
        p.op("dve", lambda e: e.scalar_tensor_tensor(out=uu[:], in0=it[s][:], scalar=1.0, in1=xcb[s][:], op0=ALU.add, op1=ALU.mult), r=ik + xk, w=["uu"])
        p.op("dve", lambda e: e.tensor_tensor(out=uu[:], in0=uu[:], in1=rt[s][:], op=ALU.mult), r=["uu"] + gk, w=["uu"])
        if tb2 == 0:
            p.op("dve", lambda e: e.tensor_tensor_scan(out=hh[:], data0=aa[s][:], data1=uu[:], initial=0.0, op0=ALU.mult, op1=ALU.add), r=[K("aa"), "uu"], w=["hh"])
        else:
            p.op("dve", lambda e: e.tensor_tensor_scan(out=hh[:], data0=aa[s][:], data1=uu[:], initial=carry[:, 0:1], op0=ALU.mult, op1=ALU.add),
                 r=[K("aa"), "uu", "carry"], w=["hh"])
        p.op("dve", lambda e: e.tensor_copy(out=carry[:], in_=hh[:, 2047:2048]), r=["hh"], w=["carry"])
        h4 = hh[:].rearrange("p (i r q) -> p i r q", i=4, r=4)
        hs3 = hsel[:].rearrange("p (i q) -> p i q", i=4)
        p.op("dve", lambda e: e.tensor_scalar(out=hs3, in0=h4[:, :, 0, :], scalar1=sel[:, 0:1], scalar2=None, op0=ALU.mult), r=["hh"], w=["hsel"])
        for r_ in range(1, 4):
            p.op("dve", lambda e, r_=r_: e.scalar_tensor_tensor(out=hs3, in0=h4[:, :, r_, :], scalar=sel[:, r_:r_ + 1], in1=hs3, op0=ALU.mult, op1=ALU.add),
                 r=["hh", "hsel"], w=["hsel"])
        gt = gt2[s]
        ls = lst[n % 2]
        lk = ("lst", n % 2)
        p.op("dve", lambda e: e.scalar_tensor_tensor(out=ls[:], in0=gt[:], scalar=0.5, in1=hsel[:], op0=ALU.mult, op1=ALU.mult), r=[("gt", s), "hsel"], w=[lk])
        p.op("pool", lambda e: e.dma_start(out=LRU_s[:, cc, tb2 * 512:(tb2 + 1) * 512], in_=ls[:]), r=[lk], w=[("LRU", cc, tb2)], dma=lk)

    blocks = [(cc, tb2) for cc in range(8) for tb2 in range(4)]
    a2_stage1(blocks[0][0], blocks[0][1], 0)
    for n, (cc, tb2) in enumerate(blocks):
        if n + 1 < len(blocks):
            a2_stage1(blocks[n + 1][0], blocks[n + 1][1], (n + 1) % 2)
        a2_stage2(cc, tb2, n % 2, n)
    p.barrier()
    ar.reset(gmark)
    if STOP_AFTER == "A2":
        return finish(nc, p, outT, ar)

    Wq = ar.alloc("Wq", [128, 8, 1024], BF16)
    stgC = {"t": [ar.alloc("stgC", [128, 8, 256], F32) for _ in range(3)], "i": 0, "name": "C"}
    load_weight(Wq, 0, w_in, 8, 0, 1024, stgC, 256, engines=("pool", "act", "dve"))
    hTa = ar.alloc("hTa", [128, 8, NT], BF16)
    p.op("sp", lambda e: e.dma_start(out=hTa[:], in_=HTO_s), w=["hTa"], dma="hTa")
    ktb = [ar.alloc("ktb", [128, S], BF16) for _ in range(2)]
    vtb = [ar.alloc("vtb", [128, 64, 128], BF16) for _ in range(2)]
    qT = [ar.alloc("qT", [128, NT], BF16) for _ in range(2)]
    NPT = 3
    pT = [ar.alloc("pT", [128, 1024], BF16) for _ in range(NPT)]
    rl2 = [ar.alloc("rl2", [128, 512], F32) for _ in range(2)]
    oc = [ar.alloc("oc", [128, 512], F32) for _ in range(2)]
    rl = ar.alloc("rl", [128, 512], F32)
    oo = ar.alloc("oo", [128, 512], F32)
    osq = ar.alloc("osq", [128, 512], BF16)
    ot1 = ar.alloc("ot1", [128, 512], F32)
    ors = ar.alloc("ors", [128, 512], F32)
    ost = [ar.alloc("ost", [128, 512], BF16) for _ in range(2)]
    OB = (6, 7)
    LBS = (0, 1)
    LB, QB = 0, 1
    npt = 0
    nst = 0
    for h in range(8):
        hs = h % 2
        p.op("sp", lambda e, hs=hs, h=h: e.dma_start(out=ktb[hs][:], in_=KT_s[h]), w=[("ktb", hs)], dma=("ktb", hs))
        p.op("sp", lambda e, hs=hs, h=h: e.dma_start(out=vtb[hs][:], in_=V_s[h]), w=[("vtb", hs)], dma=("vtb", hs))
        for tb in range(4):
            b = QB
            for kc in range(8):
                p.op("pe", lambda e, b=b, kc=kc, h=h, tb=tb: e.matmul(ps[b][:], lhsT=Wq[:, kc, h * 128:(h + 1) * 128], rhs=hTa[:, kc, tb * 512:(tb + 1) * 512],
                                                                 start=(kc == 0), stop=(kc == 7)),
                     r=["hTa"] + wkeys(Wq, h * 128, 128, 256), w=[PK(b)])
            evac(qT[hs][:, tb * 512:(tb + 1) * 512], ps[b][:], [PK(b)], [("qT", hs, tb)])
        for g in range(4):
            nkt = 16 * g + 16

            def col0(kt, g=g):
                return 0 if kt < 16 * g else 128 * ((kt - 16 * g) // 4)

            def s_mm(kt, g=g, hs=hs):
                sb = kt % 2
                c0 = col0(kt)
                for c in range(2):
                    p.op("pe", lambda e, c=c: e.matmul(big[sb][:, c * 512 + c0:(c + 1) * 512], lhsT=ktb[hs][64 * c:64 * c + 64, kt * 128:(kt + 1) * 128],
                                                       rhs=qT[hs][64 * c:64 * c + 64, g * 512 + c0:(g + 1) * 512], start=True, stop=True),
                         r=[("ktb", hs), ("qT", hs, g)], w=[PK(2 + 2 * sb + c)])
            s_mm(0)
            s_mm(1)
            for kt in range(nkt):
                sb = kt % 2
                pt = pT[npt % NPT]
                pk = ("pT", npt % NPT)
                npt += 1
                c0 = col0(kt)
                if c0 == 0:
                    p.op("act", lambda e, sb=sb, pt=pt: e.activation(out=pt[:], in_=big[sb][:], func=AF.Exp, scale=0.125), r=[PK(2 + 2 * sb), PK(3 + 2 * sb)], w=[(pk, 0), (pk, 1)])
                else:
                    p.op("act", lambda e, sb=sb, pt=pt, c0=c0: e.activation(out=pt[:].rearrange("p (c q) -> p c q", c=2)[:, :, c0:512],
                                                                           in_=big[sb][:].rearrange("p (c q) -> p c q", c=2)[:, :, c0:512], func=AF.Exp, scale=0.125),
                         r=[PK(2 + 2 * sb), PK(3 + 2 * sb)], w=[(pk, 0), (pk, 1)])
                if kt >= 16 * g:
                    tau = kt - 16 * g
                    p.op("dve", lambda e, pt=pt, tau=tau, c0=c0: e.tensor_tensor(out=pt[:, c0:512], in0=pt[:, c0:512], in1=maskB[:, tau, c0:512], op=ALU.mult), r=[(pk, 0)], w=[(pk, 0)])
                    p.op("pool", lambda e, pt=pt, tau=tau, c0=c0: e.tensor_tensor(out=pt[:, 512 + c0:1024], in0=pt[:, 512 + c0:1024], in1=maskB[:, tau, c0:512], op=ALU.mult), r=[(pk, 1)], w=[(pk, 1)])
                if kt + 2 < nkt:
                    s_mm(kt + 2)
                for c in range(2):
                    p.op("pe", lambda e, pt=pt, kt=kt, hs=hs, nkt=nkt, c=c, c0=c0: e.matmul(ps[OB[c]][:, c0:512], lhsT=vtb[hs][:, kt, :], rhs=pt[:, c * 512 + c0:(c + 1) * 512], start=(kt == 0), stop=(kt == nkt - 1)),
                         r=[(pk, c), ("vtb", hs)], w=[PK(OB[c])])
                    p.op("pe", lambda e, pt=pt, kt=kt, nkt=nkt, c=c, c0=c0: e.matmul(ps[LBS[c]][:, c0:512], lhsT=ones_bf[:], rhs=pt[:, c * 512 + c0:(c + 1) * 512], start=(kt == 0), stop=(kt == nkt - 1)),
                         r=[(pk, c)], w=[PK(LBS[c])])
            for c in range(2):
                p.op("act", lambda e, lb=LBS[c], c=c: e.activation(out=rl2[c][:], in_=ps[lb][:], func=AF.Ln), r=[PK(LBS[c])], w=[("rl", c)])
                p.op("act", lambda e, c=c: e.activation(out=rl2[c][:], in_=rl2[c][:], func=AF.Exp, scale=-1.0), r=[("rl", c)], w=[("rl", c)])
                p.op("dve", lambda e, c=c: e.tensor_tensor(out=oc[c][:], in0=ps[OB[c]][:], in1=rl2[c][:], op=ALU.mult), r=[PK(OB[c]), ("rl", c)], w=[("oc", c)])
            p.op("dve", lambda e: e.scalar_tensor_tensor(out=oo[:], in0=oc[1][:], scalar=sc[:, 0:1], in1=oc[0][:], op0=ALU.mult, op1=ALU.add),
                 r=[("oc", 0), ("oc", 1)], w=["oo"])
            p.op("act", lambda e: e.activation(out=osq[:], in_=oo[:], func=AF.Square), r=["oo"], w=["osq"])
            p.op("pe", lambda e: e.matmul(ps[LB][:], lhsT=ones_bf[:], rhs=osq[:], start=True, stop=True), r=["osq"], w=[PK(LB)])
            p.op("act", lambda e: e.activation(out=ot1[:], in_=ps[LB][:], func=AF.Ln, bias=1e-5, scale=1.0 / 128.0), r=[PK(LB)], w=["ot1"])
            p.op("act", lambda e: e.activation(out=ors[:], in_=ot1[:], func=AF.Exp, scale=-0.5), r=["ot1"], w=["ors"])
            os_ = ost[nst % 2]
            ok = ("ost", nst % 2)
            nst += 1
            p.op("dve", lambda e, os_=os_: e.scalar_tensor_tensor(out=os_[:], in0=oo[:], scalar=sc[:, 1:2], in1=ors[:], op0=ALU.mult, op1=ALU.mult), r=["oo", "ors"], w=[ok])
            p.op("pool", lambda e, os_=os_, h=h, g=g: e.dma_start(out=OT_s[:, h, g * 512:(g + 1) * 512], in_=os_[:]), r=[ok], w=[("OT", h, g)], dma=ok)
    p.barrier()
    ar.reset(gmark)
    if STOP_AFTER == "A3":
        return finish(nc, p, outT, ar)

    tT = ar.alloc("tT", [128, 8, NT], BF16)
    b2mark = ar.mark()
    mixed = ar.alloc("mixed", [128, 8, NT], BF16)
    bmark = ar.mark()
    Wga = ar.alloc("Wga", [128, 8, 1024], BF16)
    Wgb = ar.alloc("Wgb", [128, 8, 1024], BF16)
    Wab = ar.alloc("Wab", [128, 8, 1024], BF16)
    Wlb = ar.alloc("Wlb", [128, 8, 1024], BF16)
    stgD = {"t": [ar.alloc("stgD", [128, 8, 128], F32) for _ in range(3)], "i": 0, "name": "D"}
    load_weight(Wga, 0, w_in, 8, 5120, 1024, stgD, 128, engines=("pool", "act", "dve"))
    load_weight(Wgb, 0, w_in, 8, 6144, 1024, stgD, 128, engines=("pool", "act", "dve"))
    load_weight(Wab, 0, wab_d, 8, 0, 1024, stgD, 128, engines=("pool", "act", "dve"))
    load_weight(Wlb, 0, wlb_d, 8, 0, 1024, stgD, 128, engines=("pool", "act", "dve"))
    hb_ = [ar.alloc("hb", [128, 8, 512], BF16)] * 2
    ob_ = [ar.alloc("ob", [128, 8, 512], BF16)] * 2
    lb_ = [ar.alloc("lb", [128, 8, 512], BF16)] * 2
    sga = ar.alloc("sga", [128, 512], F32)
    sgb = ar.alloc("sgb", [128, 512], F32)
    m1 = ar.alloc("m1", [128, 512], F32)
    m2 = ar.alloc("m2", [128, 512], F32)
    for tb in range(4):
        s = 0
        sl = slice(tb * 512, (tb + 1) * 512)
        p.op("sp", lambda e, s=s, sl=sl: e.dma_start(out=hb_[s][:], in_=HTO_s[:, :, sl]), w=[("hb", s)], dma=("hb", s))
        p.op("sp", lambda e, s=s, sl=sl: e.dma_start(out=ob_[s][:], in_=OT_s[:, :, sl]), w=[("ob", s)], dma=("ob", s))
        p.op("sp", lambda e, s=s, sl=sl: e.dma_start(out=lb_[s][:], in_=LRU_s[:, :, sl]), w=[("lb", s)], dma=("lb", s))
        for fc in range(8):
            banks = [nextbank() for _ in range(4)]
            for (W, src, sk, b) in ((Wga, hb_, "hb", banks[0]), (Wgb, hb_, "hb", banks[1]), (Wab, ob_, "ob", banks[2]), (Wlb, lb_, "lb", banks[3])):
                for kc in range(8):
                    p.op("pe", lambda e, W=W, src=src, b=b, kc=kc, fc=fc, s=s: e.matmul(ps[b][:], lhsT=W[:, kc, fc * 128:(fc + 1) * 128], rhs=src[s][:, kc, :],
                                                                                   start=(kc == 0), stop=(kc == 7)),
                         r=[(sk, s)] + wkeys(W, fc * 128, 128, 128), w=[PK(b)])
            p.op("act", lambda e, b=banks[0]: e.activation(out=sga[:], in_=ps[b][:], func=AF.Sigmoid), r=[PK(banks[0])], w=["sga"])
            p.op("act", lambda e, b=banks[1]: e.activation(out=sgb[:], in_=ps[b][:], func=AF.Sigmoid), r=[PK(banks[1])], w=["sgb"])
            p.op("dve", lambda e, b=banks[2]: e.tensor_tensor(out=m1[:], in0=ps[b][:], in1=sga[:], op=ALU.mult), r=[PK(banks[2]), "sga"], w=["m1"])
            p.op("dve", lambda e, b=banks[3]: e.tensor_tensor(out=m2[:], in0=ps[b][:], in1=sgb[:], op=ALU.mult), r=[PK(banks[3]), "sgb"], w=["m2"])
            p.op("pool", lambda e, fc=fc, sl=sl: e.tensor_tensor(out=mixed[:, fc, sl], in0=m1[:], in1=m2[:], op=ALU.add), r=["m1", "m2"], w=[("mixed", tb)])
    p.barrier()
    ar.reset(bmark)

    Wo = ar.alloc("Wo", [128, 8, 1024], BF16)
    stgE = {"t": [ar.alloc("stgE", [128, 8, 256], F32) for _ in range(3)], "i": 0, "name": "E"}
    load_weight(Wo, 0, wout_d, 8, 0, 1024, stgE, 256, engines=("pool", "act", "dve"))
    xo = [ar.alloc("xo", [128, 8, 512], F32) for _ in range(2)]
    sq2 = ar.alloc("sq2", [128, 8, 512], BF16)
    t1b = ar.alloc("t1b", [128, 512], F32)
    rstdb = ar.alloc("rstdb", [128, 512], F32)
    for tb in range(4):
        s = tb % 2
        sl = slice(tb * 512, (tb + 1) * 512)
        xk = [("xo", s, fc) for fc in range(8)]
        p.op("sp", lambda e, s=s, sl=sl: e.dma_start(out=xo[s][:], in_=xTo_v[:, :, sl]), w=xk, dma=("xo", s))
        for fc in range(8):
            b = nextbank()
            for kc in range(8):
                p.op("pe", lambda e, b=b, kc=kc, fc=fc, sl=sl: e.matmul(ps[b][:], lhsT=Wo[:, kc, fc * 128:(fc + 1) * 128], rhs=mixed[:, kc, sl], start=(kc == 0), stop=(kc == 7)),
                     r=wkeys(Wo, fc * 128, 128, 256), w=[PK(b)])
            p.op("dve", lambda e, b=b, fc=fc, s=s: e.tensor_tensor(out=xo[s][:, fc, :], in0=ps[b][:], in1=xo[s][:, fc, :], op=ALU.add),
                 r=[PK(b), ("xo", s, fc)], w=[("xo", s, fc)])
        rms_stats(xo[s][:], 8, 512, sq2[:], t1b[:], rstdb[:], xk, "B")
        for kc in range(8):
            p.op("dve", lambda e, kc=kc, sl=sl, s=s: e.scalar_tensor_tensor(out=tT[:, kc, sl], in0=xo[s][:, kc, :], scalar=vec[:, kc, GMOE:GMOE + 1], in1=rstdb[:],
                                                                      op0=ALU.mult, op1=ALU.mult), r=[("xo", s, kc), "Brstd"], w=[("tT", tb, kc)])
        p.op("pool", lambda e, sl=sl, s=s: e.dma_start(out=X1_s[:, :, sl], in_=xo[s][:]), r=xk, w=[("X1", tb)], dma=("xo", s))
    p.barrier()
    ar.reset(b2mark)
    if STOP_AFTER == "B1":
        return finish(nc, p, outT, ar)

    yy = ar.alloc("yy", [128, 16, 1024], F32)
    b3mark = ar.mark()
    Wr = ar.alloc("Wr", [128, 8, 36], BF16)
    wrs = ar.alloc("wrs", [128, 8, 36], F32)
    p.op("sp", lambda e: e.dma_start(out=wrs[:], in_=wrt_d.rearrange("(kc p) c -> p kc c", p=128)), w=["wrs"], dma="wrs")
    p.op("pool", lambda e: e.tensor_copy(out=Wr[:], in_=wrs[:]), r=["wrs"], w=["Wr"])
    lg = ar.alloc("lg", [128, 16, 36], F32)
    comb = ar.alloc("comb", [128, 16, 32], F32)
    gmx = ar.alloc("gmx", [128, 16, 1], F32)
    goh = ar.alloc("goh", [128, 16, 4], F32)
    gex = ar.alloc("gex", [128, 16, 4], F32)
    gsm = ar.alloc("gsm", [128, 16, 1], F32)
    gw_ = ar.alloc("gw_", [128, 16, 1], F32)
    fsel = ar.alloc("fsel", [128, 16, 8], F32)
    ftmp = ar.alloc("ftmp", [128, 16, 8], F32)
    m1_ = ar.alloc("m1_", [128, 16, 1], F32)
    m2_ = ar.alloc("m2_", [128, 16, 1], F32)
    oh1 = ar.alloc("oh1", [128, 16, 8], F32)
    oh2 = ar.alloc("oh2", [128, 16, 8], F32)
    w12 = ar.alloc("w12", [128, 16, 2], F32)
    wexp = ar.alloc("wexp", [128, 16, 8], F32)
    for tt in range(16):
        b = nextbank()
        for kc in range(8):
            p.op("pe", lambda e, b=b, kc=kc, tt=tt: e.matmul(ps[b][:, 0:36], lhsT=tT[:, kc, tt * 128:(tt + 1) * 128], rhs=Wr[:, kc, :], start=(kc == 0), stop=(kc == 7)),
                 r=["Wr"], w=[PK(b)])
        p.op("dve", lambda e, b=b, tt=tt: e.tensor_tensor(out=lg[:, tt, :], in0=ps[b][:, 0:36], in1=brt[:], op=ALU.add), r=[PK(b)], w=["lg"])
    p.op("dve", lambda e: e.tensor_reduce(out=gmx[:], in_=lg[:, :, 0:4], axis=AX.X, op=ALU.max), r=["lg"], w=["gmx"])
    p.op("dve", lambda e: e.tensor_tensor(out=goh[:], in0=lg[:, :, 0:4], in1=gmx[:].to_broadcast([128, 16, 4]), op=ALU.is_equal), r=["lg", "gmx"], w=["goh"])
    p.op("dve", lambda e: e.tensor_tensor(out=gex[:], in0=lg[:, :, 0:4], in1=gmx[:].to_broadcast([128, 16, 4]), op=ALU.subtract), r=["lg", "gmx"], w=["gex"])
    p.op("act", lambda e: e.activation(out=gex[:], in_=gex[:], func=AF.Exp), r=["gex"], w=["gex"])
    p.op("dve", lambda e: e.tensor_reduce(out=gsm[:], in_=gex[:], axis=AX.X, op=ALU.add), r=["gex"], w=["gsm"])
    p.op("dve", lambda e: e.reciprocal(out=gw_[:], in_=gsm[:]), r=["gsm"], w=["gw_"])
    for g in range(4):
        fin = lg[:, :, 4 + 8 * g:12 + 8 * g]
        if g == 0:
            p.op("dve", lambda e, fin=fin: e.tensor_tensor(out=fsel[:], in0=fin, in1=goh[:, :, 0:1].to_broadcast([128, 16, 8]), op=ALU.mult), r=["lg", "goh"], w=["fsel"])
        else:
            p.op("dve", lambda e, fin=fin, g=g: e.tensor_tensor(out=ftmp[:], in0=fin, in1=goh[:, :, g:g + 1].to_broadcast([128, 16, 8]), op=ALU.mult), r=["lg", "goh"], w=["ftmp"])
            p.op("dve", lambda e: e.tensor_tensor(out=fsel[:], in0=fsel[:], in1=ftmp[:], op=ALU.add), r=["fsel", "ftmp"], w=["fsel"])
    p.op("dve", lambda e: e.tensor_reduce(out=m1_[:], in_=fsel[:], axis=AX.X, op=ALU.max), r=["fsel"], w=["m1_"])
    p.op("dve", lambda e: e.tensor_tensor(out=oh1[:], in0=fsel[:], in1=m1_[:].to_broadcast([128, 16, 8]), op=ALU.is_equal), r=["fsel", "m1_"], w=["oh1"])
    p.op("dve", lambda e: e.scalar_tensor_tensor(out=ftmp[:], in0=oh1[:], scalar=-1e30, in1=fsel[:], op0=ALU.mult, op1=ALU.add), r=["oh1", "fsel"], w=["ftmp"])
    p.op("dve", lambda e: e.tensor_reduce(out=m2_[:], in_=ftmp[:], axis=AX.X, op=ALU.max), r=["ftmp"], w=["m2_"])
    p.op("dve", lambda e: e.tensor_tensor(out=oh2[:], in0=ftmp[:], in1=m2_[:].to_broadcast([128, 16, 8]), op=ALU.is_equal), r=["ftmp", "m2_"], w=["oh2"])
    p.op("dve", lambda e: e.tensor_tensor(out=w12[:, :, 0:1], in0=m2_[:], in1=m1_[:], op=ALU.subtract), r=["m1_", "m2_"], w=["w12"])
    p.op("act", lambda e: e.activation(out=w12[:, :, 0:1], in_=w12[:, :, 0:1], func=AF.Exp), r=["w12"], w=["w12"])
    p.op("dve", lambda e: e.tensor_scalar(out=w12[:, :, 0:1], in0=w12[:, :, 0:1], scalar1=1.0, scalar2=None, op0=ALU.add), r=["w12"], w=["w12"])
    p.op("dve", lambda e: e.reciprocal(out=w12[:, :, 0:1], in_=w12[:, :, 0:1]), r=["w12"], w=["w12"])
    p.op("dve", lambda e: e.tensor_tensor(out=w12[:, :, 0:1], in0=w12[:, :, 0:1], in1=gw_[:], op=ALU.mult), r=["w12", "gw_"], w=["w12"])
    p.op("dve", lambda e: e.tensor_tensor(out=w12[:, :, 1:2], in0=gw_[:], in1=w12[:, :, 0:1], op=ALU.subtract), r=["w12", "gw_"], w=["w12"])
    p.op("dve", lambda e: e.tensor_tensor(out=wexp[:], in0=oh1[:], in1=w12[:, :, 0:1].to_broadcast([128, 16, 8]), op=ALU.mult), r=["oh1", "w12"], w=["wexp"])
    p.op("dve", lambda e: e.tensor_tensor(out=ftmp[:], in0=oh2[:], in1=w12[:, :, 1:2].to_broadcast([128, 16, 8]), op=ALU.mult), r=["oh2", "w12"], w=["ftmp"])
    p.op("dve", lambda e: e.tensor_tensor(out=wexp[:], in0=wexp[:], in1=ftmp[:], op=ALU.add), r=["wexp", "ftmp"], w=["wexp"])
    for g in range(4):
        p.op("dve", lambda e, g=g: e.tensor_tensor(out=comb[:, :, 8 * g:8 * g + 8], in0=wexp[:], in1=goh[:, :, g:g + 1].to_broadcast([128, 16, 8]), op=ALU.mult),
             r=["wexp", "goh"], w=["comb"])
    p.op("dve", lambda e: e.memset(yy[:], 0.0), w=[("yy", tt_, hf_) for tt_ in range(16) for hf_ in range(2)])
    Wg_ = [ar.alloc("Wg_", [128, 8, 512], BF16) for _ in range(2)]
    Wu_ = [ar.alloc("Wu_", [128, 8, 512], BF16) for _ in range(2)]
    Wd_ = [ar.alloc("Wd_", [128, 4, 1024], BF16) for _ in range(2)]
    stgF = {"t": [ar.alloc("stgF", [128, 8, 128], F32) for _ in range(2)], "i": 0, "name": "F"}
    hid = ar.alloc("hid", [128, 4, NT], BF16)
    sg_ = [ar.alloc("sg_", [128, 512], F32) for _ in range(2)]
    nsg = 0
    for ex in range(NE):
        s = ex % 2
        load_weight(Wg_[s], 0, weg_d[ex], 8, 0, 512, stgF, 128)
        load_weight(Wu_[s], 0, weu_d[ex], 8, 0, 512, stgF, 128)
        viewd = wed_d[ex].rearrange("(kc p) c -> p kc c", p=128)
        for c in range(0, 1024, 256):
            s2 = stgF["i"] % 2
            stgF["i"] += 1
            st = stgF["t"][s2]
            key = ("stg", "F", s2)
            stv = st[:].rearrange("p k c -> p (k c)").rearrange("p (k c) -> p k c", k=4)
            p.op("sp", lambda e, stv=stv, c=c, viewd=viewd: e.dma_start(out=stv, in_=viewd[:, :, c:c + 256]), w=[key], dma=key)
            p.op("pool", lambda e, stv=stv, c=c, s=s: e.tensor_copy(out=Wd_[s][:, :, c:c + 256], in_=stv), r=[key], w=[("w", id(Wd_[s]), c)])
        for fc in range(4):
            for tb in range(4):
                sl = slice(tb * 512, (tb + 1) * 512)
                bg = nextbank()
                bu = nextbank()
                for (W, b) in ((Wg_[s], bg), (Wu_[s], bu)):
                    for kc in range(8):
                        p.op("pe", lambda e, W=W, b=b, kc=kc, fc=fc, sl=sl: e.matmul(ps[b][:], lhsT=W[:, kc, fc * 128:(fc + 1) * 128], rhs=tT[:, kc, sl], start=(kc == 0), stop=(kc == 7)),
                             r=wkeys(W, fc * 128, 128, 128), w=[PK(b)])
                sg = sg_[nsg % 2]
                sk = ("sg_", nsg % 2)
                nsg += 1
                p.op("act", lambda e, sg=sg, bg=bg: e.activation(out=sg[:], in_=ps[bg][:], func=AF.Silu), r=[PK(bg)], w=[sk])
                p.op("dve", lambda e, sg=sg, bu=bu, fc=fc, sl=sl: e.tensor_tensor(out=hid[:, fc, sl], in0=ps[bu][:], in1=sg[:], op=ALU.mult), r=[PK(bu), sk], w=[("hid", fc, tb)])
        for tt in range(16):
            for half in range(2):
                b = nextbank()
                for fc in range(4):
                    p.op("pe", lambda e, b=b, fc=fc, tt=tt, half=half, s=s: e.matmul(ps[b][:], lhsT=hid[:, fc, tt * 128:(tt + 1) * 128], rhs=Wd_[s][:, fc, half * 512:(half + 1) * 512],
                                                                                start=(fc == 0), stop=(fc == 3)),
                         r=[("hid", fc, tt // 4)] + wkeys(Wd_[s], half * 512, 512, 256), w=[PK(b)])
                ysl = yy[:, tt, half * 512:(half + 1) * 512]
                p.op("dve", lambda e, b=b, ysl=ysl, tt=tt, ex=ex: e.scalar_tensor_tensor(out=ysl, in0=ps[b][:], scalar=comb[:, tt, ex:ex + 1], in1=ysl, op0=ALU.mult, op1=ALU.add),
                     r=[PK(b), "comb", ("yy", tt, half)], w=[("yy", tt, half)])
    p.barrier()
    ar.reset(b3mark)

    Wpg = ar.alloc("Wpg", [128, 8, 1024], BF16)
    Wpp = ar.alloc("Wpp", [128, 2, 1024], BF16)
    stgG = {"t": [ar.alloc("stgG", [128, 8, 64], F32) for _ in range(2)], "i": 0, "name": "G"}
    load_weight(Wpg, 0, wpg_d, 8, 0, 1024, stgG, 64, engines=("pool", "act", "dve"))
    load_weight(Wpp, 0, wpp_d, 2, 0, 1024, stgG, 64, engines=("pool", "act", "dve"))
    pf = ar.alloc("pf", [128, 2, 512], F32)
    pb = ar.alloc("pb", [128, 2, 512], BF16)
    sq3 = ar.alloc("sq3", [128, 8, 512], BF16)
    t1c = ar.alloc("t1c", [128, 512], F32)
    rstdc = ar.alloc("rstdc", [128, 512], F32)
    t3d = [ar.alloc("t3", [128, 8, 512], BF16) for _ in range(2)]
    gte = ar.alloc("gte", [128, 512], F32)
    x1b = [ar.alloc("x1b", [128, 8, 512], F32) for _ in range(2)]
    pT_v = pT_own.rearrange("(kc p) t -> p kc t", p=128)
    outT_v = outT.rearrange("(kc p) t -> p kc t", p=128)

    def b3_front(tb):
        s = tb % 2
        sl = slice(tb * 512, (tb + 1) * 512)
        xk = [("xb", s, fc) for fc in range(8)]
        t3 = t3d[s]
        p.op("sp", lambda e: e.dma_start(out=x1b[s][:], in_=X1_s[:, :, sl]), w=xk, dma=("x1b", s))
        for fc in range(8):
            b = nextbank()
            for q in range(4):
                tt = tb * 4 + q
                p.op("pe", lambda e, b=b, q=q, tt=tt, fc=fc: e.transpose(out=ps[b][:, q * 128:(q + 1) * 128], in_=yy[:, tt, fc * 128:(fc + 1) * 128], identity=ident_f[:]),
                     r=[], w=[PK(b)])
            p.op("dve", lambda e, b=b, fc=fc: e.tensor_tensor(out=x1b[s][:, fc, :], in0=ps[b][:], in1=x1b[s][:, fc, :], op=ALU.add), r=[PK(b), ("xb", s, fc)], w=[("xb", s, fc)])
        rms_stats(x1b[s][:], 8, 512, sq3[:], t1c[:], rstdc[:], xk, "C")
        for kc in range(8):
            p.op("dve", lambda e, kc=kc: e.scalar_tensor_tensor(out=t3[:, kc, :], in0=x1b[s][:, kc, :], scalar=vec[:, kc, GPLE:GPLE + 1], in1=rstdc[:], op0=ALU.mult, op1=ALU.mult),
                 r=["Crstd", ("xb", s, kc)], w=[("t3", s, kc)])

    def b3_back(tb):
        s = tb % 2
        sl = slice(tb * 512, (tb + 1) * 512)
        xk = [("xb", s, fc) for fc in range(8)]
        t3 = t3d[s]
        p.op("sp", lambda e: e.dma_start(out=pf[:], in_=pT_v[:, :, sl]), w=["pf"], dma="pf")
        p.op("pool", lambda e: e.tensor_copy(out=pb[:], in_=pf[:]), r=["pf"], w=["pb"])
        for fc in range(8):
            bg = nextbank()
            bp = nextbank()
            for kc in range(8):
                p.op("pe", lambda e, bg=bg, kc=kc, fc=fc: e.matmul(ps[bg][:], lhsT=Wpg[:, kc, fc * 128:(fc + 1) * 128], rhs=t3[:, kc, :], start=(kc == 0), stop=(kc == 7)),
                     r=[("t3", s, kc)] + wkeys(Wpg, fc * 128, 128, 64), w=[PK(bg)])
            for kc in range(2):
                p.op("pe", lambda e, bp=bp, kc=kc, fc=fc: e.matmul(ps[bp][:], lhsT=Wpp[:, kc, fc * 128:(fc + 1) * 128], rhs=pb[:, kc, :], start=(kc == 0), stop=(kc == 1)),
                     r=["pb"] + wkeys(Wpp, fc * 128, 128, 64), w=[PK(bp)])
            p.op("act", lambda e, bg=bg: e.activation(out=gte[:], in_=ps[bg][:], func=AF.Sigmoid), r=[PK(bg)], w=["gte"])
            p.op("dve", lambda e, bp=bp: e.tensor_tensor(out=gte[:], in0=ps[bp][:], in1=gte[:], op=ALU.mult), r=[PK(bp), "gte"], w=["gte"])
            p.op("pool", lambda e, fc=fc: e.tensor_tensor(out=x1b[s][:, fc, :], in0=gte[:], in1=x1b[s][:, fc, :], op=ALU.add), r=["gte", ("xb", s, fc)], w=[("xb", s, fc)])
        rms_stats(x1b[s][:], 8, 512, sq3[:], t1c[:], rstdc[:], xk, "C")
        for kc in range(8):
            p.op("dve", lambda e, kc=kc: e.scalar_tensor_tensor(out=x1b[s][:, kc, :], in0=x1b[s][:, kc, :], scalar=vec[:, kc, GFIN:GFIN + 1], in1=rstdc[:], op0=ALU.mult, op1=ALU.mult),
                 r=[("xb", s, kc), "Crstd"], w=[("xb", s, kc)])
        p.op("sp", lambda e: e.dma_start(out=outT_v[:, :, sl], in_=x1b[s][:]), r=xk, w=[("out", tb)], dma=("x1b", s))

    b3_front(0)
    for tb in range(4):
        if tb + 1 < 4:
            b3_front(tb + 1)
        b3_back(tb)
    return finish(nc, p, outT, ar)


def finish(nc, p, outT, ar):
    p.barrier()
    p.emit()
    return nc


def _mask_for(r):
    m = np.zeros((128, 16, 512), np.float32)
    ki = np.arange(128)[:, None]
    qi = np.arange(128)[None, :]
    tri = np.where(ki > qi, 0.0, 1.0).astype(np.float32)
    m[:] = 1.0
    for tau in range(16):
        for mq in range(4):
            tq = 4 * mq + r
            if tau > tq:
                m[:, tau, mq * 128:(mq + 1) * 128] = 0.0
            elif tau == tq:
                m[:, tau, mq * 128:(mq + 1) * 128] = tri
    return m.astype(ml_dtypes.bfloat16)


def _chunked(v):
    return np.ascontiguousarray(np.asarray(v, np.float32).reshape(8, 128).T)


def prepare_inputs(x, p, g_mix, w_in, lam_q1, lam_k1, lam_q2, lam_k2, g_subln, w_conv, b_conv,
                   w_rg, b_rg, w_ig, b_ig, lru_lambda, w_attn_br, w_lru_br, w_out, g_moe,
                   w_rt_group, b_rt_group, w_rt_expert, b_rt_expert, w_e_gate, w_e_up, w_e_down,
                   g_ple, w_ple_gate, w_ple_proj, g_final):
    f = lambda a: np.asarray(a, np.float32)
    x = f(x); p = f(p)
    vecs = np.zeros((128, 8, 12), np.float32)
    vecs[:, :, 0] = _chunked(f(g_mix)[0]); vecs[:, :, 1] = _chunked(f(g_moe)[0]); vecs[:, :, 2] = _chunked(f(g_ple)[0])
    vecs[:, :, 3] = _chunked(f(g_final)); vecs[:, :, 4] = _chunked(f(b_conv)[0]); vecs[:, :, 5] = _chunked(f(b_rg)[0].reshape(-1))
    vecs[:, :, 6] = _chunked(f(b_ig)[0].reshape(-1)); vecs[:, :, 7] = _chunked(f(lru_lambda)[0])
    for j in range(4):
        vecs[:, :, 8 + j] = _chunked(f(w_conv)[0, j])

    def blockdiag(w):
        w = f(w)[0]
        o = np.zeros((128, 8, 128), np.float32)
        for n in range(16):
            cc, hf = n // 2, n % 2
            o[hf * 64:(hf + 1) * 64, cc, hf * 64:(hf + 1) * 64] = w[n]
        return o

    w_rt = np.concatenate([f(w_rt_group)[0]] + [f(w_rt_expert)[0, g] for g in range(4)], axis=1)
    b_rt = np.concatenate([f(b_rt_group)[0], f(b_rt_expert)[0].reshape(-1)])[None, :]
    common = dict(
        w_in=np.ascontiguousarray(f(w_in)[0]),
        vecs=vecs,
        gsub=np.ascontiguousarray(f(g_subln)[0][:, None]),
        lamv=np.ascontiguousarray(np.broadcast_to(np.stack([f(lam_q1)[0], f(lam_k1)[0], f(lam_q2)[0], f(lam_k2)[0]])[None], (128, 4, 64))),
        brt=np.ascontiguousarray(np.broadcast_to(b_rt, (128, 36))),
        wrg_bd=blockdiag(w_rg), wig_bd=blockdiag(w_ig),
        w_attn_br=np.ascontiguousarray(f(w_attn_br)[0]), w_lru_br=np.ascontiguousarray(f(w_lru_br)[0]), w_out=np.ascontiguousarray(f(w_out)[0]),
        w_rt=np.ascontiguousarray(w_rt),
        w_e_gate=np.ascontiguousarray(f(w_e_gate)[0]), w_e_up=np.ascontiguousarray(f(w_e_up)[0]), w_e_down=np.ascontiguousarray(f(w_e_down)[0]),
        w_ple_gate=np.ascontiguousarray(f(w_ple_gate)[0]), w_ple_proj=np.ascontiguousarray(f(w_ple_proj)[0]),
    )
    in_maps = []
    toks = []
    for c in range(8):
        b, r = c // 4, c % 4
        own = (np.arange(16)[:, None] * 4 + r) * 128 + np.arange(128)[None, :]
        own = own.reshape(-1)
        toks.append((b, own))
        selv = np.zeros((128, 4), np.float32)
        selv[:, r] = 1.0
        m = dict(common)
        m.update(
            xT_full=np.ascontiguousarray(x[b].T),
            xT_own=np.ascontiguousarray(x[b][own].T),
            pT_own=np.ascontiguousarray(p[0, b][own].T),
            maskB=_mask_for(r),
            sel=selv,
        )
        in_maps.append(m)
    return in_maps, toks


_NC_CACHE = {}


def kernel(**inputs):
    in_maps, toks = prepare_inputs(**inputs)
    if "nc" not in _NC_CACHE:
        _NC_CACHE["nc"] = build_program()
    nc = _NC_CACHE["nc"]
    res = run_bass_kernel_spmd(nc, in_maps, core_ids=list(range(8)))
    out = np.zeros((2, S, D), np.float32)
    for c in range(8):
        b, own = toks[c]
        out[b, own, :] = res.results[c]["outT"].T
    if DEBUG:
        kernel.last = res.results
    return out
```

```python
import os
import numpy as np
import ml_dtypes
import concourse.bass as bass
import concourse.mybir as mybir
from concourse.bass_utils import run_bass_kernel_spmd

AF = mybir.ActivationFunctionType
ALU = mybir.AluOpType
AX = mybir.AxisListType
F32 = mybir.dt.float32
BF16 = mybir.dt.bfloat16

S = 8192
D = 1024
NT = 2048
NE = 32
FF = 512
NEG = 0.0


class Prog:
    ENGS = ("sp", "act", "pe", "dve", "pool")

    def __init__(self, nc):
        self.nc = nc
        self.ops = []
        self.lastw = {}
        self.readers = {}
        self._nbar = 0

    def op(self, eng, fn, r=(), w=(), dma=None):
        idx = len(self.ops)
        deps = set()
        for k in r:
            lw = self.lastw.get(k)
            if lw is not None:
                deps.add(lw)
        for k in w:
            lw = self.lastw.get(k)
            if lw is not None:
                deps.add(lw)
            rd = self.readers.get(k)
            if rd:
                deps.update(rd)
        for k in r:
            self.readers.setdefault(k, []).append(idx)
        for k in w:
            self.lastw[k] = idx
            self.readers[k] = []
        self.ops.append(dict(eng=eng, fn=fn, deps=deps, dma=dma))
        return idx

    def barrier(self):
        n = self._nbar
        self._nbar += 1
        last_dma = {}
        for i, o in enumerate(self.ops):
            if o["dma"] is not None:
                last_dma[o["dma"]] = i
        for e in ("act", "pe", "dve", "pool"):
            i = self.op(e, lambda eng: eng.drain(), w=[("bar", n, e)])
            self.ops[i]["deps"].update(last_dma.values())
        for e in self.ENGS:
            self.op(e, lambda eng: eng.nop(), r=[("bar", n, x) for x in ("act", "pe", "dve", "pool")])
        self.lastw = {}
        self.readers = {}

    def emit(self):
        nc = self.nc
        ops = self.ops

        def skip(od, o):
            return od["eng"] == "pe" and o["eng"] == "pe" and od["dma"] is None and o["dma"] is None

        needed = set()
        for i, o in enumerate(ops):
            for d in o["deps"]:
                if not skip(ops[d], o):
                    needed.add(d)
        eng_cnt = {e: 0 for e in self.ENGS}
        dma_cnt = {}
        dma_keys = []
        for i, o in enumerate(ops):
            if o["dma"] is not None:
                k = o["dma"]
                if k not in dma_cnt:
                    dma_cnt[k] = 0
                    dma_keys.append(k)
                dma_cnt[k] += 16
                o["ev"] = ("dma", k, dma_cnt[k])
            elif i in needed:
                eng_cnt[o["eng"]] += 1
                o["ev"] = ("eng", o["eng"], eng_cnt[o["eng"]])
            else:
                o["ev"] = None
        cur = {}
        for i, o in enumerate(ops):
            waits = {}
            for d in o["deps"]:
                od = ops[d]
                if skip(od, o):
                    continue
                ev = od["ev"]
                if ev[0] == "dma":
                    key = ("dma", ev[1])
                    val = cur[ev[1]]
                else:
                    key = ("eng", ev[1])
                    val = ev[2]
                if waits.get(key, 0) < val:
                    waits[key] = val
            o["waits"] = waits
            if o["dma"] is not None:
                cur[o["dma"]] = o["ev"][2]
        sems = {}
        for e in ("act", "pe", "dve", "pool"):
            sems[("eng", e)] = nc.alloc_semaphore("s_" + e)
        for n, k in enumerate(dma_keys):
            sems[("dma", k)] = nc.alloc_semaphore("d%d" % n)
        streams = {e: [] for e in self.ENGS}
        for o in ops:
            streams[o["eng"]].append(o)

        def run_stream(eng_obj, lst):
            waited = {}
            for o in lst:
                for key, val in o["waits"].items():
                    if waited.get(key, 0) >= val:
                        continue
                    waited[key] = val
                    eng_obj.wait_ge(sems[key], val)
                ins = o["fn"](eng_obj)
                ev = o["ev"]
                if ev is not None:
                    if ev[0] == "dma":
                        ins.then_inc(sems[("dma", ev[1])], 16)
                    else:
                        ins.then_inc(sems[("eng", ev[1])], 1)

        with nc.Block() as block:
            block.sync(lambda e: run_stream(e, streams["sp"]))
            block.scalar(lambda e: run_stream(e, streams["act"]))
            block.tensor(lambda e: run_stream(e, streams["pe"]))
            block.vector(lambda e: run_stream(e, streams["dve"]))
            block.gpsimd(lambda e: run_stream(e, streams["pool"]))


class Arena:
    BASE = 16512
    TOP = 229344

    def __init__(self, nc):
        self.nc = nc
        self.off = self.BASE
        self.n = 0

    def alloc(self, name, shape, dt):
        nb = int(np.prod(shape[1:])) * (4 if dt == F32 else 2)
        nb = (nb + 31) // 32 * 32
        assert self.off + nb <= self.TOP, (name, self.off, nb)
        self.n += 1
        t = self.nc.alloc_sbuf_tensor_at("%s_%d" % (name, self.n), list(shape), dt, offset=self.off)
        self.off += nb
        return t

    def mark(self):
        return self.off

    def reset(self, m):
        self.off = m


DEBUG = bool(int(os.environ.get("MK_DEBUG", "0")))
STOP_AFTER = os.environ.get("MK_STOP", "")


def build_program():
    nc = bass.Bass("TRN2", target_bir_lowering=False)
    p = Prog(nc)
    ar = Arena(nc)

    def din(name, shape, dt=F32):
        return nc.dram_tensor(name, list(shape), dt, kind="ExternalInput").ap()

    skind = "ExternalOutput" if DEBUG else "Internal"

    def dscr(name, shape, dt):
        return nc.dram_tensor(name, list(shape), dt, kind=skind).ap()

    xT_full = din("xT_full", [D, S])
    xT_own = din("xT_own", [D, NT])
    pT_own = din("pT_own", [256, NT])
    maskB_d = din("maskB", [128, 16, 512], BF16)
    sel_d = din("sel", [128, 4])
    w_in = din("w_in", [D, 7168])
    vec_d = din("vecs", [128, 8, 12])
    gsub_d = din("gsub", [128, 1])
    lamv_d = din("lamv", [128, 4, 64])
    brt_d = din("brt", [128, 36])
    wrg_d = din("wrg_bd", [128, 8, 128])
    wig_d = din("wig_bd", [128, 8, 128])
    wab_d = din("w_attn_br", [D, D])
    wlb_d = din("w_lru_br", [D, D])
    wout_d = din("w_out", [D, D])
    wrt_d = din("w_rt", [D, 36])
    weg_d = din("w_e_gate", [NE, D, FF])
    weu_d = din("w_e_up", [NE, D, FF])
    wed_d = din("w_e_down", [NE, FF, D])
    wpg_d = din("w_ple_gate", [D, D])
    wpp_d = din("w_ple_proj", [256, D])
    outT = nc.dram_tensor("outT", [D, NT], F32, kind="ExternalOutput").ap()

    KT_s = dscr("KT_s", [8, 128, S], BF16)
    XR_s = dscr("XR_s", [8, 128, S], BF16)
    V_s = dscr("V_s", [8, 128, 64, 128], BF16)
    HTO_s = dscr("HTO_s", [128, 8, NT], BF16)
    LRU_s = dscr("LRU_s", [128, 8, NT], BF16)
    OT_s = dscr("OT_s", [128, 8, NT], BF16)
    X1_s = dscr("X1_s", [128, 8, NT], F32)

    class Bank:
        def __init__(self, t, off):
            self.t, self.off = t, off

        def __getitem__(self, key):
            if not isinstance(key, tuple):
                key = (key, slice(None))
            pk, ck = key
            c0 = ck.start or 0
            c1 = 512 if ck.stop is None else ck.stop
            return self.t[pk, self.off + c0:self.off + c1]

    _p01 = [nc.alloc_psum_tensor("psb%d" % i, [128, 512], F32) for i in range(2)]
    big = [nc.alloc_psum_tensor("psbig%d" % i, [128, 1024], F32) for i in range(2)]
    _p67 = [nc.alloc_psum_tensor("psb%d" % i, [128, 512], F32) for i in (6, 7)]
    ps = _p01 + [Bank(big[0], 0), Bank(big[0], 512), Bank(big[1], 0), Bank(big[1], 512)] + _p67
    rr = [0]

    def nextbank(lo=1, hi=8):
        b = lo + rr[0] % (hi - lo)
        rr[0] += 1
        return b

    def PK(i):
        return ("ps", i)

    ones_bf = ar.alloc("ones_bf", [128, 128], BF16)
    ident_bf = ar.alloc("ident_bf", [128, 128], BF16)
    ident_f = ar.alloc("ident_f", [128, 128], F32)
    maskB = ar.alloc("maskB", [128, 16, 512], BF16)
    vec = ar.alloc("vec", [128, 8, 12], F32)
    gsub = ar.alloc("gsub", [128, 1], F32)
    lamv = ar.alloc("lamv", [128, 4, 64], F32)
    brt = ar.alloc("brt", [128, 36], F32)
    sel = ar.alloc("sel", [128, 4], F32)
    der = ar.alloc("der", [128, 8, 4], F32)
    sc = ar.alloc("sc", [128, 8], F32)
    lprod = ar.alloc("lprod", [128, 2, 64], F32)
    GM, GMOE, GPLE, GFIN, BCONV, BRG, BIG, LAM, WC0 = 0, 1, 2, 3, 4, 5, 6, 7, 8

    p.op("sp", lambda e: e.dma_start(out=maskB[:], in_=maskB_d), w=["maskB"], dma="c0")
    p.op("sp", lambda e: e.dma_start(out=vec[:], in_=vec_d), w=["vec"], dma="c1")
    p.op("sp", lambda e: e.dma_start(out=gsub[:], in_=gsub_d), w=["gsub"], dma="c1")
    p.op("sp", lambda e: e.dma_start(out=lamv[:], in_=lamv_d), w=["lamv"], dma="c1")
    p.op("sp", lambda e: e.dma_start(out=brt[:], in_=brt_d), w=["brt"], dma="c1")
    p.op("sp", lambda e: e.dma_start(out=sel[:], in_=sel_d), w=["sel"], dma="c1")
    p.op("pool", lambda e: e.memset(ident_f[:], 1.0), w=["ident_f"])
    p.op("pool", lambda e: e.affine_select(out=ident_f[:], in_=ident_f[:], pattern=[[1, 128]], compare_op=ALU.is_equal,
                                           fill=0.0, base=0, channel_multiplier=-1), r=["ident_f"], w=["ident_f"])
    p.op("dve", lambda e: e.tensor_copy(out=ident_bf[:], in_=ident_f[:]), r=["ident_f"], w=["ident_bf"])
    p.op("dve", lambda e: e.memset(ones_bf[:], 1.0), w=["ones_bf"])
    p.op("dve", lambda e: e.tensor_scalar(out=der[:, :, 0], in0=vec[:, :, BRG], scalar1=0.5, scalar2=None, op0=ALU.mult), r=["vec"], w=["der0"])
    p.op("dve", lambda e: e.tensor_scalar(out=der[:, :, 1], in0=vec[:, :, BIG], scalar1=0.5, scalar2=None, op0=ALU.mult), r=["vec"], w=["der1"])
    p.op("act", lambda e: e.activation(out=der[:, :, 3], in_=vec[:, :, LAM], func=AF.Exp, scale=-1.0), r=["vec"], w=["der3"])
    p.op("dve", lambda e: e.tensor_scalar(out=der[:, :, 2], in0=der[:, :, 3], scalar1=0.2, scalar2=None, op0=ALU.mult), r=["der3"], w=["der2"])
    for cst in (-0.25, 1.0 / 3.0, -0.5, 1.0):
        p.op("dve", lambda e, cst=cst: e.scalar_tensor_tensor(out=der[:, :, 2], in0=der[:, :, 2], scalar=cst, in1=der[:, :, 3], op0=ALU.add, op1=ALU.mult),
             r=["der2", "der3"], w=["der2"])
    p.op("dve", lambda e: e.tensor_scalar(out=der[:, :, 2], in0=der[:, :, 2], scalar1=-4.0, scalar2=None, op0=ALU.mult), r=["der2"], w=["der2"])
    p.op("dve", lambda e: e.tensor_tensor(out=lprod[:, 0, :], in0=lamv[:, 0, :], in1=lamv[:, 1, :], op=ALU.mult), r=["lamv"], w=["lprod"])
    p.op("dve", lambda e: e.tensor_tensor(out=lprod[:, 1, :], in0=lamv[:, 2, :], in1=lamv[:, 3, :], op=ALU.mult), r=["lamv", "lprod"], w=["lprod"])
    p.op("dve", lambda e: e.reduce_sum(out=sc[:, 2:4], in_=lprod[:], axis=AX.X), r=["lprod"], w=["sc23"])
    p.op("act", lambda e: e.activation(out=sc[:, 4:6], in_=sc[:, 2:4], func=AF.Exp), r=["sc23"], w=["sc45"])
    p.op("dve", lambda e: e.tensor_tensor(out=sc[:, 0:1], in0=sc[:, 5:6], in1=sc[:, 4:5], op=ALU.subtract), r=["sc45"], w=["sc0"])
    p.op("dve", lambda e: e.tensor_scalar(out=sc[:, 0:1], in0=sc[:, 0:1], scalar1=-0.2, scalar2=None, op0=ALU.add), r=["sc0"], w=["sc0"])
    p.op("dve", lambda e: e.tensor_scalar(out=sc[:, 1:2], in0=gsub[:], scalar1=0.8, scalar2=None, op0=ALU.mult), r=["gsub"], w=["sc1"])
    CONST_KEYS = ["maskB", "vec", "sel", "brt", "ident_f", "ident_bf", "ones_bf", "der0", "der1", "der2", "sc0", "sc1"]

    def consts_barrier():
        p.barrier()

    gmark = ar.mark()

    def load_weight(dst, dst_c0, src2d, nk, col0, ncols, stg, piece, engines=("pool",)):
        view = src2d.rearrange("(kc p) c -> p kc c", p=128)
        ns = len(stg["t"])
        for c in range(0, ncols, piece):
            s = stg["i"] % ns
            eng = engines[stg["i"] % len(engines)]
            stg["i"] += 1
            st = stg["t"][s]
            key = ("stg", stg["name"], s)
            p.op("sp", lambda e, st=st, c=c: e.dma_start(out=st[:, 0:nk, 0:piece], in_=view[:, :, col0 + c:col0 + c + piece]),
                 w=[key], dma=key)
            if eng == "act":
                p.op("act", lambda e, st=st, c=c: e.copy(out=dst[:, 0:nk, dst_c0 + c:dst_c0 + c + piece], in_=st[:, 0:nk, 0:piece]),
                     r=[key], w=[("w", id(dst), dst_c0 + c)])
            else:
                p.op(eng, lambda e, st=st, c=c: e.tensor_copy(out=dst[:, 0:nk, dst_c0 + c:dst_c0 + c + piece], in_=st[:, 0:nk, 0:piece]),
                     r=[key], w=[("w", id(dst), dst_c0 + c)])

    def wkeys(dst, c0, n, piece):
        return [("w", id(dst), c) for c in range(c0 // piece * piece, c0 + n, piece)]

    evq = [0]

    def evac(out_ap, in_ap, rk, wk):
        evq[0] += 1
        if evq[0] % 2:
            p.op("act", lambda e: e.copy(out=out_ap, in_=in_ap), r=rk, w=wk)
        else:
            p.op("dve", lambda e: e.tensor_copy(out=out_ap, in_=in_ap), r=rk, w=wk)

    def rms_stats(src, nkc, nblk, sq, t1, rstd, rk, tag, eps=1e-6, dim=1024.0):
        p.op("act", lambda e: e.activation(out=sq, in_=src, func=AF.Square), r=rk, w=[tag + "sq"])
        for kc in range(nkc):
            p.op("pe", lambda e, kc=kc: e.matmul(ps[0][:, 0:nblk], lhsT=ones_bf[:], rhs=sq[:, kc, :], start=(kc == 0), stop=(kc == nkc - 1)),
                 r=[tag + "sq"], w=[PK(0)])
        p.op("act", lambda e: e.activation(out=t1, in_=ps[0][:, 0:nblk], func=AF.Ln, bias=eps, scale=1.0 / dim), r=[PK(0)], w=[tag + "t1"])
        p.op("act", lambda e: e.activation(out=rstd, in_=t1, func=AF.Exp, scale=-0.5), r=[tag + "t1"], w=[tag + "rstd"])

    Wk = ar.alloc("Wk", [128, 8, 1024], BF16)
    Wv = ar.alloc("Wv", [128, 8, 1024], BF16)
    Wx = ar.alloc("Wx", [128, 8, 1024], BF16)
    stgA = {"t": [ar.alloc("stgA", [128, 8, 256], F32) for _ in range(3)], "i": 0, "name": "A"}
    load_weight(Wk, 0, w_in, 8, 1024, 1024, stgA, 256, engines=("pool", "act", "dve"))
    load_weight(Wv, 0, w_in, 8, 2048, 1024, stgA, 256, engines=("pool", "act", "dve"))
    load_weight(Wx, 0, w_in, 8, 3072, 1024, stgA, 256, engines=("pool", "act", "dve"))
    consts_barrier()
    xf = [ar.alloc("xf", [128, 8, 512], F32) for _ in range(2)]
    sq = ar.alloc("sq", [128, 8, 512], BF16)
    t1 = ar.alloc("t1", [128, 512], F32)
    rstd = ar.alloc("rstd", [128, 512], F32)
    hT = [ar.alloc("hT", [128, 8, 512], BF16) for _ in range(2)]
    kst = [ar.alloc("kst", [128, 8, 512], BF16) for _ in range(2)]
    xst = [ar.alloc("xst", [128, 8, 512], BF16) for _ in range(2)]
    vst = [ar.alloc("vst", [128, 4, 1024], BF16) for _ in range(2)]
    xTf_v = xT_full.rearrange("(kc p) t -> p kc t", p=128)
    xTo_v = xT_own.rearrange("(kc p) t -> p kc t", p=128)
    KT_v = KT_s.rearrange("h p t -> p h t")
    XR_v = XR_s.rearrange("h p t -> p h t")
    V_v = V_s.rearrange("h p t v -> p t h v")

    def norm_block(src_view, tb, s):
        p.op("sp", lambda e: e.dma_start(out=xf[s][:], in_=src_view[:, :, tb * 512:(tb + 1) * 512]), w=[("xf", s)], dma=("xf", s))
        rms_stats(xf[s][:], 8, 512, sq[:], t1[:], rstd[:], [("xf", s)], "A")
        for kc in range(8):
            p.op("dve", lambda e, kc=kc: e.scalar_tensor_tensor(out=hT[s][:, kc, :], in0=xf[s][:, kc, :], scalar=vec[:, kc, GM:GM + 1],
                                                              in1=rstd[:], op0=ALU.mult, op1=ALU.mult),
                 r=[("xf", s), "Arstd"], w=[("hT", s, kc)])

    def a1_compute(kind, tb, s):
        if kind == "own":
            p.op("pool", lambda e: e.dma_start(out=HTO_s[:, :, tb * 512:(tb + 1) * 512], in_=hT[s][:]),
                 r=[("hT", s, kc) for kc in range(8)], w=[("HTO", tb)], dma=("hTst", s))
            return
        for (W, st, keyn) in ((Wk, kst, "kst"), (Wx, xst, "xst")):
            for h in range(8):
                b = nextbank()
                for kc in range(8):
                    p.op("pe", lambda e, W=W, b=b, kc=kc, h=h: e.matmul(ps[b][:], lhsT=W[:, kc, h * 128:(h + 1) * 128], rhs=hT[s][:, kc, :],
                                                                   start=(kc == 0), stop=(kc == 7)),
                         r=[("hT", s, kc)] + wkeys(W, h * 128, 128, 256), w=[PK(b)])
                evac(st[s][:, h, :], ps[b][:], [PK(b)], [(keyn, s, h)])
        p.op("pool", lambda e: e.dma_start(out=KT_v[:, :, tb * 512:(tb + 1) * 512], in_=kst[s][:]), r=[("kst", s, h) for h in range(8)], w=[("KT", tb)], dma=("kst", s))
        p.op("pool", lambda e: e.dma_start(out=XR_v[:, :, tb * 512:(tb + 1) * 512], in_=xst[s][:]), r=[("xst", s, h) for h in range(8)], w=[("XR", tb)], dma=("xst", s))
        for tt in range(4):
            for half in range(2):
                b = nextbank()
                for kc in range(8):
                    p.op("pe", lambda e, b=b, kc=kc, tt=tt, half=half: e.matmul(ps[b][:], lhsT=hT[s][:, kc, tt * 128:(tt + 1) * 128],
                                                                           rhs=Wv[:, kc, half * 512:(half + 1) * 512], start=(kc == 0), stop=(kc == 7)),
                         r=[("hT", s, kc)] + wkeys(Wv, half * 512, 512, 256), w=[PK(b)])
                evac(vst[s][:, tt, half * 512:(half + 1) * 512], ps[b][:], [PK(b)], [("vst", s, tt, half)])
        for tt in range(4):
            p.op("pool", lambda e, tt=tt: e.dma_start(out=V_v[:, tb * 4 + tt], in_=vst[s][:, tt, :].rearrange("p (h v) -> p h v", h=8)),
                 r=[("vst", s, tt, half) for half in range(2)], w=[("V", tb, tt)], dma=("vst", s))

    jobs = [("own", tb) for tb in range(4)] + [("full", tb) for tb in range(16)]
    norm_block(xTo_v, 0, 0)
    for n, (kind, tb) in enumerate(jobs):
        if n + 1 < len(jobs):
            k2, tb2 = jobs[n + 1]
            norm_block(xTo_v if k2 == "own" else xTf_v, tb2, (n + 1) % 2)
        a1_compute(kind, tb, n % 2)
    p.barrier()
    ar.reset(gmark)
    if STOP_AFTER == "A1":
        return finish(nc, p, outT, ar)

    Wgr = ar.alloc("Wgr", [128, 8, 1024], BF16)
    Wrg = ar.alloc("Wrg", [128, 8, 128], BF16)
    Wig = ar.alloc("Wig", [128, 8, 128], BF16)
    stgB = {"t": [ar.alloc("stgB", [128, 8, 128], F32) for _ in range(3)], "i": 0, "name": "B"}
    load_weight(Wgr, 0, w_in, 8, 4096, 1024, stgB, 128, engines=("pool", "act", "dve"))
    for (Wt, src, nm) in ((Wrg, wrg_d, "wrg"), (Wig, wig_d, "wig")):
        s = stgB["i"] % len(stgB["t"])
        stgB["i"] += 1
        st = stgB["t"][s]
        key = ("stg", "B", s)
        p.op("sp", lambda e, st=st, src=src: e.dma_start(out=st[:, :, 0:128], in_=src), w=[key], dma=key)
        p.op("pool", lambda e, st=st, Wt=Wt: e.tensor_copy(out=Wt[:], in_=st[:, :, 0:128]), r=[key], w=[nm])
    Dm = ar.alloc("Dm", [128, 8, 4, 128], BF16)
    for cc in range(8):
        for j in range(4):
            p.op("dve", lambda e, cc=cc, j=j: e.tensor_scalar(out=Dm[:, cc, j, :], in0=ident_f[:], scalar1=vec[:, cc, WC0 + j:WC0 + j + 1], scalar2=None, op0=ALU.mult),
                 w=[("Dm", cc, j)])
    xrb = [ar.alloc("xrb", [128, 4, 2048], BF16) for _ in range(2)]
    xcb = [ar.alloc("xcb", [128, 2048], BF16) for _ in range(2)]
    rt = [ar.alloc("rt", [128, 2048], F32) for _ in range(2)]
    it = [ar.alloc("it", [128, 2048], BF16) for _ in range(2)]
    aa = [ar.alloc("aa", [128, 2048], F32) for _ in range(2)]
    uu = ar.alloc("uu", [128, 2048], F32)
    hh = ar.alloc("hh", [128, 2048], F32)
    hTo2 = [ar.alloc("hTo", [128, 8, 512], BF16) for _ in range(2)]
    carry = ar.alloc("carry", [128, 1], F32)
    hsel = ar.alloc("hsel", [128, 512], F32)
    gz2 = [ar.alloc("gz", [128, 512], F32) for _ in range(2)]
    gw = ar.alloc("gw", [128, 512], F32)
    gt2 = [ar.alloc("gt", [128, 512], F32) for _ in range(2)]
    lst = [ar.alloc("lst", [128, 512], BF16) for _ in range(2)]

    def a2_stage1(cc, tb2, s):
        K = lambda nm: (nm, s)
        t0 = tb2 * 2048
        hTo = hTo2[s]
        gt = gt2[s]
        p.op("sp", lambda e: e.dma_start(out=hTo[:], in_=HTO_s[:, :, tb2 * 512:(tb2 + 1) * 512]), w=[("hTo", s)], dma=("hTo", s))
        for j in range(4):
            if tb2 == 0 and j < 3:
                p.op("dve", lambda e, j=j: e.memset(xrb[s][:, j, 0:3 - j], 0.0), w=[("xrb", s, j)])
                p.op("sp", lambda e, j=j: e.dma_start(out=xrb[s][:, j, 3 - j:2048], in_=XR_s[cc, :, 0:2048 - (3 - j)]), w=[("xrb", s, j)], dma=("xrb", s))
            else:
                p.op("sp", lambda e, j=j: e.dma_start(out=xrb[s][:, j, :], in_=XR_s[cc, :, t0 - 3 + j:t0 - 3 + j + 2048]), w=[("xrb", s, j)], dma=("xrb", s))
        cb = []
        for nb in range(4):
            sl = slice(nb * 512, (nb + 1) * 512)
            b = nextbank()
            cb.append(b)
            for j in range(4):
                p.op("pe", lambda e, b=b, j=j, sl=sl: e.matmul(ps[b][:], lhsT=Dm[:, cc, j, :], rhs=xrb[s][:, j, sl], start=(j == 0), stop=(j == 3)),
                     r=[("xrb", s, j), ("Dm", cc, j)], w=[PK(b)])
        for nb in range(4):
            sl = slice(nb * 512, (nb + 1) * 512)
            b = cb[nb]
            p.op("act", lambda e, b=b, sl=sl: e.activation(out=xcb[s][:, sl], in_=ps[b][:], func=AF.Identity, bias=vec[:, cc, BCONV:BCONV + 1], scale=1.0),
                 r=[PK(b)], w=[("xcb", s, nb)])
        bq = nextbank()
        for kc in range(8):
            p.op("pe", lambda e, kc=kc: e.matmul(ps[bq][:], lhsT=Wgr[:, kc, cc * 128:(cc + 1) * 128], rhs=hTo[:, kc, :], start=(kc == 0), stop=(kc == 7)),
                 r=[("hTo", s)] + wkeys(Wgr, cc * 128, 128, 128), w=[PK(bq)])
        gz = gz2[s]
        p.op("act", lambda e: e.copy(out=gz[:], in_=ps[bq][:]), r=[PK(bq)], w=[("gz", s)])
        p.op("act", lambda e: e.activation(out=gw[:], in_=ps[bq][:], func=AF.Square), r=[PK(bq)], w=["gw"])
        p.op("pool", lambda e: e.tensor_scalar(out=gw[:], in0=gw[:], scalar1=0.044715, scalar2=1.0, op0=ALU.mult, op1=ALU.add), r=["gw"], w=["gw"])
        p.op("pool", lambda e: e.tensor_tensor(out=gw[:], in0=gw[:], in1=gz[:], op=ALU.mult), r=["gw", ("gz", s)], w=["gw"])
        for nb in range(4):
            sl = slice(nb * 512, (nb + 1) * 512)
            for (Wt, nm, dst, hb) in ((Wrg, "wrg", rt, 0), (Wig, "wig", it, 1)):
                b2 = nextbank()
                p.op("pe", lambda e, b2=b2, Wt=Wt, sl=sl: e.matmul(ps[b2][:], lhsT=Wt[:, cc, :], rhs=xcb[s][:, sl], start=True, stop=True),
                     r=[("xcb", s, nb), nm], w=[PK(b2)])
                p.op("act", lambda e, b2=b2, dst=dst, sl=sl, hb=hb: e.activation(out=dst[s][:, sl], in_=ps[b2][:], func=AF.Tanh, bias=der[:, cc, hb:hb + 1], scale=0.5),
                     r=[PK(b2)], w=[("g", s, hb, nb)])
        p.op("act", lambda e: e.activation(out=gt[:], in_=gw[:], func=AF.Tanh, scale=0.7978845608028654), r=["gw"], w=[("gt", s)])
        gk = [("g", s, 0, nb) for nb in range(4)]
        p.op("act", lambda e: e.activation(out=aa[s][:], in_=rt[s][:], func=AF.Exp, bias=der[:, cc, 2:3], scale=der[:, cc, 2:3]), r=gk, w=[K("aa")])
        p.op("act", lambda e: e.activation(out=rt[s][:], in_=aa[s][:], func=AF.Square), r=[K("aa")], w=gk)
        p.op("act", lambda e: e.activation(out=rt[s][:], in_=rt[s][:], func=AF.Sqrt, bias=0.25, scale=-0.25), r=gk, w=gk)

    def a2_stage2(cc, tb2, s, n):
        K = lambda nm: (nm, s)
        gk = [("g", s, 0, nb) for nb in range(4)]
        ik = [("g", s, 1, nb) for nb in range(4)]
        xk = [("xcb", s, nb) for nb in range(4)]
        gz = gz2[s]
        gt = gt2[s]
        p.op("dve", lambda e: e.scalar_tensor_tensor(out=gt[:], in0=gt[:], scalar=1.0, in1=gz[:], op0=ALU.add, op1=ALU.mult), r=[("gt", s), ("gz", s)], w=[("gt", s)])
        p.op("dve", lambda e: e.scalar_tensor_tensor(out=uu[:], in0=it[s][:], scalar=1.0, in1=xcb[s][:], op0=ALU.add, op1=ALU.mult), r=ik + xk, w=["uu"])
        p.op("dve", lambda e: e.tensor_tensor(out=uu[:], in0=uu[:], in1=rt[s][:], op=ALU.mult), r=["uu"] + gk, w=["uu"])
        if tb2 == 0:
            p.op("dve", lambda e: e.tensor_tensor_scan(out=hh[:], data0=aa[s][:], data1=uu[:], initial=0.0, op0=ALU.mult, op1=ALU.add), r=[K("aa"), "uu"], w=["hh"])
        else:
            p.op("dve", lambda e: e.tensor_tensor_scan(out=hh[:], data0=aa[s][:], data1=uu[:], initial=carry[:, 0:1], op0=ALU.mult, op1=ALU.add),
                 r=[K("aa"), "uu", "carry"], w=["hh"])
        p.op("dve", lambda e: e.tensor_copy(out=carry[:], in_=hh[:, 2047:2048]), r=["hh"], w=["carry"])
        h4 = hh[:].rearrange("p (i r q) -> p i r q", i=4, r=4)
        hs3 = hsel[:].rearrange("p (i q) -> p i q", i=4)
        p.op("dve", lambda e: e.tensor_scalar(out=hs3, in0=h4[:, :, 0, :], scalar1=sel[:, 0:1], scalar2=None, op0=ALU.mult), r=["hh"], w=["hsel"])
        for r_ in range(1, 4):
            p.op("dve", lambda e, r_=r_: e.scalar_tensor_tensor(out=hs3, in0=h4[:, :, r_, :], scalar=sel[:, r_:r_ + 1], in1=hs3, op0=ALU.mult, op1=ALU.add),
                 r=["hh", "hsel"], w=["hsel"])
        gt = gt2[s]
        ls = lst[n % 2]
        lk = ("lst", n % 2)
        p.op("dve", lambda e: e.scalar_tensor_tensor(out=ls[:], in0=gt[:], scalar=0.5, in1=hsel[:], op0=ALU.mult, op1=ALU.mult), r=[("gt", s), "hsel"], w=[lk])
        p.op("pool", lambda e: e.dma_start(out=LRU_s[:, cc, tb2 * 512:(tb2 + 1) * 512], in_=ls[:]), r=[lk], w=[("LRU", cc, tb2)], dma=lk)

    blocks = [(cc, tb2) for cc in range(8) for tb2 in range(4)]
    a2_stage1(blocks[0][0], blocks[0][1], 0)
    for n, (cc, tb2) in enumerate(blocks):
        if n + 1 < len(blocks):
            a2_stage1(blocks[n + 1][0], blocks[n + 1][1], (n + 1) % 2)
        a2_stage2(cc, tb2, n % 2, n)
    p.barrier()
    ar.reset(gmark)
    if STOP_AFTER == "A2":
        return finish(nc, p, outT, ar)

    Wq = ar.alloc("Wq", [128, 8, 1024], BF16)
    stgC = {"t": [ar.alloc("stgC", [128, 8, 256], F32) for _ in range(3)], "i": 0, "name": "C"}
    load_weight(Wq, 0, w_in, 8, 0, 1024, stgC, 256, engines=("pool", "act", "dve"))
    hTa = ar.alloc("hTa", [128, 8, NT], BF16)
    p.op("sp", lambda e: e.dma_start(out=hTa[:], in_=HTO_s), w=["hTa"], dma="hTa")
    ktb = [ar.alloc("ktb", [128, S], BF16) for _ in range(2)]
    vtb = [ar.alloc("vtb", [128, 64, 128], BF16) for _ in range(2)]
    qT = [ar.alloc("qT", [128, NT], BF16) for _ in range(2)]
    NPT = 3
    pT = [ar.alloc("pT", [128, 1024], BF16) for _ in range(NPT)]
    rl2 = [ar.alloc("rl2", [128, 512], F32) for _ in range(2)]
    oc = [ar.alloc("oc", [128, 512], F32) for _ in range(2)]
    rl = ar.alloc("rl", [128, 512], F32)
    oo = ar.alloc("oo", [128, 512], F32)
    osq = ar.alloc("osq", [128, 512], BF16)
    ot1 = ar.alloc("ot1", [128, 512], F32)
    ors = ar.alloc("ors", [128, 512], F32)
    ost = [ar.alloc("ost", [128, 512], BF16) for _ in range(2)]
    OB = (6, 7)
    LBS = (0, 1)
    LB, QB = 0, 1
    npt = 0
    nst = 0
    for h in range(8):
        hs = h % 2
        p.op("sp", lambda e, hs=hs, h=h: e.dma_start(out=ktb[hs][:], in_=KT_s[h]), w=[("ktb", hs)], dma=("ktb", hs))
        p.op("sp", lambda e, hs=hs, h=h: e.dma_start(out=vtb[hs][:], in_=V_s[h]), w=[("vtb", hs)], dma=("vtb", hs))
        for tb in range(4):
            b = QB
            for kc in range(8):
                p.op("pe", lambda e, b=b, kc=kc, h=h, tb=tb: e.matmul(ps[b][:], lhsT=Wq[:, kc, h * 128:(h + 1) * 128], rhs=hTa[:, kc, tb * 512:(tb + 1) * 512],
                                                                 start=(kc == 0), stop=(kc == 7)),
                     r=["hTa"] + wkeys(Wq, h * 128, 128, 256), w=[PK(b)])
            evac(qT[hs][:, tb * 512:(tb + 1) * 512], ps[b][:], [PK(b)], [("qT", hs, tb)])
        for g in range(4):
            nkt = 16 * g + 16

            def col0(kt, g=g):
                return 0 if kt < 16 * g else 128 * ((kt - 16 * g) // 4)

            def s_mm(kt, g=g, hs=hs):
                sb = kt % 2
                c0 = col0(kt)
                for c in range(2):
                    p.op("pe", lambda e, c=c: e.matmul(big[sb][:, c * 512 + c0:(c + 1) * 512], lhsT=ktb[hs][64 * c:64 * c + 64, kt * 128:(kt + 1) * 128],
                                                       rhs=qT[hs][64 * c:64 * c + 64, g * 512 + c0:(g + 1) * 512], start=True, stop=True),
                         r=[("ktb", hs), ("qT", hs, g)], w=[PK(2 + 2 * sb + c)])
            s_mm(0)
            s_mm(1)
            for kt in range(nkt):
                sb = kt % 2
                pt = pT[npt % NPT]
                pk = ("pT", npt % NPT)
                npt += 1
                c0 = col0(kt)
                if c0 == 0:
                    p.op("act", lambda e, sb=sb, pt=pt: e.activation(out=pt[:], in_=big[sb][:], func=AF.Exp, scale=0.125), r=[PK(2 + 2 * sb), PK(3 + 2 * sb)], w=[(pk, 0), (pk, 1)])
                else:
                    p.op("act", lambda e, sb=sb, pt=pt, c0=c0: e.activation(out=pt[:].rearrange("p (c q) -> p c q", c=2)[:, :, c0:512],
                                                                           in_=big[sb][:].rearrange("p (c q) -> p c q", c=2)[:, :, c0:512], func=AF.Exp, scale=0.125),
                         r=[PK(2 + 2 * sb), PK(3 + 2 * sb)], w=[(pk, 0), (pk, 1)])
                if kt >= 16 * g:
                    tau = kt - 16 * g
                    p.op("dve", lambda e, pt=pt, tau=tau, c0=c0: e.tensor_tensor(out=pt[:, c0:512], in0=pt[:, c0:512], in1=maskB[:, tau, c0:512], op=ALU.mult), r=[(pk, 0)], w=[(pk, 0)])
                    p.op("pool", lambda e, pt=pt, tau=tau, c0=c0: e.tensor_tensor(out=pt[:, 512 + c0:1024], in0=pt[:, 512 + c0:1024], in1=maskB[:, tau, c0:512], op=ALU.mult), r=[(pk, 1)], w=[(pk, 1)])
                if kt + 2 < nkt:
                    s_mm(kt + 2)
                for c in range(2):
                    p.op("pe", lambda e, pt=pt, kt=kt, hs=hs, nkt=nkt, c=c, c0=c0: e.matmul(ps[OB[c]][:, c0:512], lhsT=vtb[hs][:, kt, :], rhs=pt[:, c * 512 + c0:(c + 1) * 512], start=(kt == 0), stop=(kt == nkt - 1)),
                         r=[(pk, c), ("vtb", hs)], w=[PK(OB[c])])
                    p.op("pe", lambda e, pt=pt, kt=kt, nkt=nkt, c=c, c0=c0: e.matmul(ps[LBS[c]][:, c0:512], lhsT=ones_bf[:], rhs=pt[:, c * 512 + c0:(c + 1) * 512], start=(kt == 0), stop=(kt == nkt - 1)),
                         r=[(pk, c)], w=[PK(LBS[c])])
            for c in range(2):
                p.op("act", lambda e, lb=LBS[c], c=c: e.activation(out=rl2[c][:], in_=ps[lb][:], func=AF.Ln), r=[PK(LBS[c])], w=[("rl", c)])
                p.op("act", lambda e, c=c: e.activation(out=rl2[c][:], in_=rl2[c][:], func=AF.Exp, scale=-1.0), r=[("rl", c)], w=[("rl", c)])
                p.op("dve", lambda e, c=c: e.tensor_tensor(out=oc[c][:], in0=ps[OB[c]][:], in1=rl2[c][:], op=ALU.mult), r=[PK(OB[c]), ("rl", c)], w=[("oc", c)])
            p.op("dve", lambda e: e.scalar_tensor_tensor(out=oo[:], in0=oc[1][:], scalar=sc[:, 0:1], in1=oc[0][:], op0=ALU.mult, op1=ALU.add),
                 r=[("oc", 0), ("oc", 1)], w=["oo"])
            p.op("act", lambda e: e.activation(out=osq[:], in_=oo[:], func=AF.Square), r=["oo"], w=["osq"])
            p.op("pe", lambda e: e.matmul(ps[LB][:], lhsT=ones_bf[:], rhs=osq[:], start=True, stop=True), r=["osq"], w=[PK(LB)])
            p.op("act", lambda e: e.activation(out=ot1[:], in_=ps[LB][:], func=AF.Ln, bias=1e-5, scale=1.0 / 128.0), r=[PK(LB)], w=["ot1"])
            p.op("act", lambda e: e.activation(out=ors[:], in_=ot1[:], func=AF.Exp, scale=-0.5), r=["ot1"], w=["ors"])
            os_ = ost[nst % 2]
            ok = ("ost", nst % 2)
            nst += 1
            p.op("dve", lambda e, os_=os_: e.scalar_tensor_tensor(out=os_[:], in0=oo[:], scalar=sc[:, 1:2], in1=ors[:], op0=ALU.mult, op1=ALU.mult), r=["oo", "ors"], w=[ok])
            p.op("pool", lambda e, os_=os_, h=h, g=g: e.dma_start(out=OT_s[:, h, g * 512:(g + 1) * 512], in_=os_[:]), r=[ok], w=[("OT", h, g)], dma=ok)
    p.barrier()
    ar.reset(gmark)
    if STOP_AFTER == "A3":
        return finish(nc, p, outT, ar)

    tT = ar.alloc("tT", [128, 8, NT], BF16)
    b2mark = ar.mark()
    mixed = ar.alloc("mixed", [128, 8, NT], BF16)
    bmark = ar.mark()
    Wga = ar.alloc("Wga", [128, 8, 1024], BF16)
    Wgb = ar.alloc("Wgb", [128, 8, 1024], BF16)
    Wab = ar.alloc("Wab", [128, 8, 1024], BF16)
    Wlb = ar.alloc("Wlb", [128, 8, 1024], BF16)
    stgD = {"t": [ar.alloc("stgD", [128, 8, 128], F32) for _ in range(3)], "i": 0, "name": "D"}
    load_weight(Wga, 0, w_in, 8, 5120, 1024, stgD, 128, engines=("pool", "act", "dve"))
    load_weight(Wgb, 0, w_in, 8, 6144, 1024, stgD, 128, engines=("pool", "act", "dve"))
    load_weight(Wab, 0, wab_d, 8, 0, 1024, stgD, 128, engines=("pool", "act", "dve"))
    load_weight(Wlb, 0, wlb_d, 8, 0, 1024, stgD, 128, engines=("pool", "act", "dve"))
    hb_ = [ar.alloc("hb", [128, 8, 512], BF16)] * 2
    ob_ = [ar.alloc("ob", [128, 8, 512], BF16)] * 2
    lb_ = [ar.alloc("lb", [128, 8, 512], BF16)] * 2
    sga = ar.alloc("sga", [128, 512], F32)
    sgb = ar.alloc("sgb", [128, 512], F32)
    m1 = ar.alloc("m1", [128, 512], F32)
    m2 = ar.alloc("m2", [128, 512], F32)
    for tb in range(4):
        s = 0
        sl = slice(tb * 512, (tb + 1) * 512)
        p.op("sp", lambda e, s=s, sl=sl: e.dma_start(out=hb_[s][:], in_=HTO_s[:, :, sl]), w=[("hb", s)], dma=("hb", s))
        p.op("sp", lambda e, s=s, sl=sl: e.dma_start(out=ob_[s][:], in_=OT_s[:, :, sl]), w=[("ob", s)], dma=("ob", s))
        p.op("sp", lambda e, s=s, sl=sl: e.dma_start(out=lb_[s][:], in_=LRU_s[:, :, sl]), w=[("lb", s)], dma=("lb", s))
        for fc in range(8):
            banks = [nextbank() for _ in range(4)]
            for (W, src, sk, b) in ((Wga, hb_, "hb", banks[0]), (Wgb, hb_, "hb", banks[1]), (Wab, ob_, "ob", banks[2]), (Wlb, lb_, "lb", banks[3])):
                for kc in range(8):
                    p.op("pe", lambda e, W=W, src=src, b=b, kc=kc, fc=fc, s=s: e.matmul(ps[b][:], lhsT=W[:, kc, fc * 128:(fc + 1) * 128], rhs=src[s][:, kc, :],
                                                                                   start=(kc == 0), stop=(kc == 7)),
                         r=[(sk, s)] + wkeys(W, fc * 128, 128, 128), w=[PK(b)])
            p.op("act", lambda e, b=banks[0]: e.activation(out=sga[:], in_=ps[b][:], func=AF.Sigmoid), r=[PK(banks[0])], w=["sga"])
            p.op("act", lambda e, b=banks[1]: e.activation(out=sgb[:], in_=ps[b][:], func=AF.Sigmoid), r=[PK(banks[1])], w=["sgb"])
            p.op("dve", lambda e, b=banks[2]: e.tensor_tensor(out=m1[:], in0=ps[b][:], in1=sga[:], op=ALU.mult), r=[PK(banks[2]), "sga"], w=["m1"])
            p.op("dve", lambda e, b=banks[3]: e.tensor_tensor(out=m2[:], in0=ps[b][:], in1=sgb[:], op=ALU.mult), r=[PK(banks[3]), "sgb"], w=["m2"])
            p.op("pool", lambda e, fc=fc, sl=sl: e.tensor_tensor(out=mixed[:, fc, sl], in0=m1[:], in1=m2[:], op=ALU.add), r=["m1", "m2"], w=[("mixed", tb)])
    p.barrier()
    ar.reset(bmark)

    Wo = ar.alloc("Wo", [128, 8, 1024], BF16)
    stgE = {"t": [ar.alloc("stgE", [128, 8, 256], F32) for _ in range(3)], "i": 0, "name": "E"}
    load_weight(Wo, 0, wout_d, 8, 0, 1024, stgE, 256, engines=("pool", "act", "dve"))
    xo = [ar.alloc("xo", [128, 8, 512], F32) for _ in range(2)]
    sq2 = ar.alloc("sq2", [128, 8, 512], BF16)
    t1b = ar.alloc("t1b", [128, 512], F32)
    rstdb = ar.alloc("rstdb", [128, 512], F32)
    for tb in range(4):
        s = tb % 2
        sl = slice(tb * 512, (tb + 1) * 512)
        xk = [("xo", s, fc) for fc in range(8)]
        p.op("sp", lambda e, s=s, sl=sl: e.dma_start(out=xo[s][:], in_=xTo_v[:, :, sl]), w=xk, dma=("xo", s))
        for fc in range(8):
            b = nextbank()
            for kc in range(8):
                p.op("pe", lambda e, b=b, kc=kc, fc=fc, sl=sl: e.matmul(ps[b][:], lhsT=Wo[:, kc, fc * 128:(fc + 1) * 128], rhs=mixed[:, kc, sl], start=(kc == 0), stop=(kc == 7)),
                     r=wkeys(Wo, fc * 128, 128, 256), w=[PK(b)])
            p.op("dve", lambda e, b=b, fc=fc, s=s: e.tensor_tensor(out=xo[s][:, fc, :], in0=ps[b][:], in1=xo[s][:, fc, :], op=ALU.add),
                 r=[PK(b), ("xo", s, fc)], w=[("xo", s, fc)])
        rms_stats(xo[s][:], 8, 512, sq2[:], t1b[:], rstdb[:], xk, "B")
        for kc in range(8):
            p.op("dve", lambda e, kc=kc, sl=sl, s=s: e.scalar_tensor_tensor(out=tT[:, kc, sl], in0=xo[s][:, kc, :], scalar=vec[:, kc, GMOE:GMOE + 1], in1=rstdb[:],
                                                                      op0=ALU.mult, op1=ALU.mult), r=[("xo", s, kc), "Brstd"], w=[("tT", tb, kc)])
        p.op("pool", lambda e, sl=sl, s=s: e.dma_start(out=X1_s[:, :, sl], in_=xo[s][:]), r=xk, w=[("X1", tb)], dma=("xo", s))
    p.barrier()
    ar.reset(b2mark)
    if STOP_AFTER == "B1":
        return finish(nc, p, outT, ar)

    yy = ar.alloc("yy", [128, 16, 1024], F32)
    b3mark = ar.mark()
    Wr = ar.alloc("Wr", [128, 8, 36], BF16)
    wrs = ar.alloc("wrs", [128, 8, 36], F32)
    p.op("sp", lambda e: e.dma_start(out=wrs[:], in_=wrt_d.rearrange("(kc p) c -> p kc c", p=128)), w=["wrs"], dma="wrs")
    p.op("pool", lambda e: e.tensor_copy(out=Wr[:], in_=wrs[:]), r=["wrs"], w=["Wr"])
    lg = ar.alloc("lg", [128, 16, 36], F32)
    comb = ar.alloc("comb", [128, 16, 32], F32)
    gmx = ar.alloc("gmx", [128, 16, 1], F32)
    goh = ar.alloc("goh", [128, 16, 4], F32)
    gex = ar.alloc("gex", [128, 16, 4], F32)
    gsm = ar.alloc("gsm", [128, 16, 1], F32)
    gw_ = ar.alloc("gw_", [128, 16, 1], F32)
    fsel = ar.alloc("fsel", [128, 16, 8], F32)
    ftmp = ar.alloc("ftmp", [128, 16, 8], F32)
    m1_ = ar.alloc("m1_", [128, 16, 1], F32)
    m2_ = ar.alloc("m2_", [128, 16, 1], F32)
    oh1 = ar.alloc("oh1", [128, 16, 8], F32)
    oh2 = ar.alloc("oh2", [128, 16, 8], F32)
    w12 = ar.alloc("w12", [128, 16, 2], F32)
    wexp = ar.alloc("wexp", [128, 16, 8], F32)
    for tt in range(16):
        b = nextbank()
        for kc in range(8):
            p.op("pe", lambda e, b=b, kc=kc, tt=tt: e.matmul(ps[b][:, 0:36], lhsT=tT[:, kc, tt * 128:(tt + 1) * 128], rhs=Wr[:, kc, :], start=(kc == 0), stop=(kc == 7)),
                 r=["Wr"], w=[PK(b)])
        p.op("dve", lambda e, b=b, tt=tt: e.tensor_tensor(out=lg[:, tt, :], in0=ps[b][:, 0:36], in1=brt[:], op=ALU.add), r=[PK(b)], w=["lg"])
    p.op("dve", lambda e: e.tensor_reduce(out=gmx[:], in_=lg[:, :, 0:4], axis=AX.X, op=ALU.max), r=["lg"], w=["gmx"])
    p.op("dve", lambda e: e.tensor_tensor(out=goh[:], in0=lg[:, :, 0:4], in1=gmx[:].to_broadcast([128, 16, 4]), op=ALU.is_equal), r=["lg", "gmx"], w=["goh"])
    p.op("dve", lambda e: e.tensor_tensor(out=gex[:], in0=lg[:, :, 0:4], in1=gmx[:].to_broadcast([128, 16, 4]), op=ALU.subtract), r=["lg", "gmx"], w=["gex"])
    p.op("act", lambda e: e.activation(out=gex[:], in_=gex[:], func=AF.Exp), r=["gex"], w=["gex"])
    p.op("dve", lambda e: e.tensor_reduce(out=gsm[:], in_=gex[:], axis=AX.X, op=ALU.add), r=["gex"], w=["gsm"])
    p.op("dve", lambda e: e.reciprocal(out=gw_[:], in_=gsm[:]), r=["gsm"], w=["gw_"])
    for g in range(4):
        fin = lg[:, :, 4 + 8 * g:12 + 8 * g]
        if g == 0:
            p.op("dve", lambda e, fin=fin: e.tensor_tensor(out=fsel[:], in0=fin, in1=goh[:, :, 0:1].to_broadcast([128, 16, 8]), op=ALU.mult), r=["lg", "goh"], w=["fsel"])
        else:
            p.op("dve", lambda e, fin=fin, g=g: e.tensor_tensor(out=ftmp[:], in0=fin, in1=goh[:, :, g:g + 1].to_broadcast([128, 16, 8]), op=ALU.mult), r=["lg", "goh"], w=["ftmp"])
            p.op("dve", lambda e: e.tensor_tensor(out=fsel[:], in0=fsel[:], in1=ftmp[:], op=ALU.add), r=["fsel", "ftmp"], w=["fsel"])
    p.op("dve", lambda e: e.tensor_reduce(out=m1_[:], in_=fsel[:], axis=AX.X, op=ALU.max), r=["fsel"], w=["m1_"])
    p.op("dve", lambda e: e.tensor_tensor(out=oh1[:], in0=fsel[:], in1=m1_[:].to_broadcast([128, 16, 8]), op=ALU.is_equal), r=["fsel", "m1_"], w=["oh1"])
    p.op("dve", lambda e: e.scalar_tensor_tensor(out=ftmp[:], in0=oh1[:], scalar=-1e30, in1=fsel[:], op0=ALU.mult, op1=ALU.add), r=["oh1", "fsel"], w=["ftmp"])
    p.op("dve", lambda e: e.tensor_reduce(out=m2_[:], in_=ftmp[:], axis=AX.X, op=ALU.max), r=["ftmp"], w=["m2_"])
    p.op("dve", lambda e: e.tensor_tensor(out=oh2[:], in0=ftmp[:], in1=m2_[:].to_broadcast([128, 16, 8]), op=ALU.is_equal), r=["ftmp", "m2_"], w=["oh2"])
    p.op("dve", lambda e: e.tensor_tensor(out=w12[:, :, 0:1], in0=m2_[:], in1=m1_[:], op=ALU.subtract), r=["m1_", "m2_"], w=["w12"])
    p.op("act", lambda e: e.activation(out=w12[:, :, 0:1], in_=w12[:, :, 0:1], func=AF.Exp), r=["w12"], w=["w12"])
    p.op("dve", lambda e: e.tensor_scalar(out=w12[:, :, 0:1], in0=w12[:, :, 0:1], scalar1=1.0, scalar2=None, op0=ALU.add), r=["w12"], w=["w12"])
    p.op("dve", lambda e: e.reciprocal(out=w12[:, :, 0:1], in_=w12[:, :, 0:1]), r=["w12"], w=["w12"])
    p.op("dve", lambda e: e.tensor_tensor(out=w12[:, :, 0:1], in0=w12[:, :, 0:1], in1=gw_[:], op=ALU.mult), r=["w12", "gw_"], w=["w12"])
    p.op("dve", lambda e: e.tensor_tensor(out=w12[:, :, 1:2], in0=gw_[:], in1=w12[:, :, 0:1], op=ALU.subtract), r=["w12", "gw_"], w=["w12"])
    p.op("dve", lambda e: e.tensor_tensor(out=wexp[:], in0=oh1[:], in1=w12[:, :, 0:1].to_broadcast([128, 16, 8]), op=ALU.mult), r=["oh1", "w12"], w=["wexp"])
    p.op("dve", lambda e: e.tensor_tensor(out=ftmp[:], in0=oh2[:], in1=w12[:, :, 1:2].to_broadcast([128, 16, 8]), op=ALU.mult), r=["oh2", "w12"], w=["ftmp"])
    p.op("dve", lambda e: e.tensor_tensor(out=wexp[:], in0=wexp[:], in1=ftmp[:], op=ALU.add), r=["wexp", "ftmp"], w=["wexp"])
    for g in range(4):
        p.op("dve", lambda e, g=g: e.tensor_tensor(out=comb[:, :, 8 * g:8 * g + 8], in0=wexp[:], in1=goh[:, :, g:g + 1].to_broadcast([128, 16, 8]), op=ALU.mult),
             r=["wexp", "goh"], w=["comb"])
    p.op("dve", lambda e: e.memset(yy[:], 0.0), w=[("yy", tt_, hf_) for tt_ in range(16) for hf_ in range(2)])
    Wg_ = [ar.alloc("Wg_", [128, 8, 512], BF16) for _ in range(2)]
    Wu_ = [ar.alloc("Wu_", [128, 8, 512], BF16) for _ in range(2)]
    Wd_ = [ar.alloc("Wd_", [128, 4, 1024], BF16) for _ in range(2)]
    stgF = {"t": [ar.alloc("stgF", [128, 8, 128], F32) for _ in range(2)], "i": 0, "name": "F"}
    hid = ar.alloc("hid", [128, 4, NT], BF16)
    sg_ = [ar.alloc("sg_", [128, 512], F32) for _ in range(2)]
    nsg = 0
    for ex in range(NE):
        s = ex % 2
        load_weight(Wg_[s], 0, weg_d[ex], 8, 0, 512, stgF, 128)
        load_weight(Wu_[s], 0, weu_d[ex], 8, 0, 512, stgF, 128)
        viewd = wed_d[ex].rearrange("(kc p) c -> p kc c", p=128)
        for c in range(0, 1024, 256):
            s2 = stgF["i"] % 2
            stgF["i"] += 1
            st = stgF["t"][s2]
            key = ("stg", "F", s2)
            stv = st[:].rearrange("p k c -> p (k c)").rearrange("p (k c) -> p k c", k=4)
            p.op("sp", lambda e, stv=stv, c=c, viewd=viewd: e.dma_start(out=stv, in_=viewd[:, :, c:c + 256]), w=[key], dma=key)
            p.op("pool", lambda e, stv=stv, c=c, s=s: e.tensor_copy(out=Wd_[s][:, :, c:c + 256], in_=stv), r=[key], w=[("w", id(Wd_[s]), c)])
        for fc in range(4):
            for tb in range(4):
                sl = slice(tb * 512, (tb + 1) * 512)
                bg = nextbank()
                bu = nextbank()
                for (W, b) in ((Wg_[s], bg), (Wu_[s], bu)):
                    for kc in range(8):
                        p.op("pe", lambda e, W=W, b=b, kc=kc, fc=fc, sl=sl: e.matmul(ps[b][:], lhsT=W[:, kc, fc * 128:(fc + 1) * 128], rhs=tT[:, kc, sl], start=(kc == 0), stop=(kc == 7)),
                             r=wkeys(W, fc * 128, 128, 128), w=[PK(b)])
                sg = sg_[nsg % 2]
                sk = ("sg_", nsg % 2)
                nsg += 1
                p.op("act", lambda e, sg=sg, bg=bg: e.activation(out=sg[:], in_=ps[bg][:], func=AF.Silu), r=[PK(bg)], w=[sk])
                p.op("dve", lambda e, sg=sg, bu=bu, fc=fc, sl=sl: e.tensor_tensor(out=hid[:, fc, sl], in0=ps[bu][:], in1=sg[:], op=ALU.mult), r=[PK(bu), sk], w=[("hid", fc, tb)])
        for tt in range(16):
            for half in range(2):
                b = nextbank()
                for fc in range(4):
                    p.op("pe", lambda e, b=b, fc=fc, tt=tt, half=half, s=s: e.matmul(ps[b][:], lhsT=hid[:, fc, tt * 128:(tt + 1) * 128], rhs=Wd_[s][:, fc, half * 512:(half + 1) * 512],
                                                                                start=(fc == 0), stop=(fc == 3)),
                         r=[("hid", fc, tt // 4)] + wkeys(Wd_[s], half * 512, 512, 256), w=[PK(b)])
                ysl = yy[:, tt, half * 512:(half + 1) * 512]
                p.op("dve", lambda e, b=b, ysl=ysl, tt=tt, ex=ex: e.scalar_tensor_tensor(out=ysl, in0=ps[b][:], scalar=comb[:, tt, ex:ex + 1], in1=ysl, op0=ALU.mult, op1=ALU.add),
                     r=[PK(b), "comb", ("yy", tt, half)], w=[("yy", tt, half)])
    p.barrier()
    ar.reset(b3mark)

    Wpg = ar.alloc("Wpg", [128, 8, 1024], BF16)
    Wpp = ar.alloc("Wpp", [128, 2, 1024], BF16)
    stgG = {"t": [ar.alloc("stgG", [128, 8, 64], F32) for _ in range(2)], "i": 0, "name": "G"}
    load_weight(Wpg, 0, wpg_d, 8, 0, 1024, stgG, 64, engines=("pool", "act", "dve"))
    load_weight(Wpp, 0, wpp_d, 2, 0, 1024, stgG, 64, engines=("pool", "act", "dve"))
    pf = ar.alloc("pf", [128, 2, 512], F32)
    pb = ar.alloc("pb", [128, 2, 512], BF16)
    sq3 = ar.alloc("sq3", [128, 8, 512], BF16)
    t1c = ar.alloc("t1c", [128, 512], F32)
    rstdc = ar.alloc("rstdc", [128, 512], F32)
    t3d = [ar.alloc("t3", [128, 8, 512], BF16) for _ in range(2)]
    gte = ar.alloc("gte", [128, 512], F32)
    x1b = [ar.alloc("x1b", [128, 8, 512], F32) for _ in range(2)]
    pT_v = pT_own.rearrange("(kc p) t -> p kc t", p=128)
    outT_v = outT.rearrange("(kc p) t -> p kc t", p=128)

    def b3_front(tb):
        s = tb % 2
        sl = slice(tb * 512, (tb + 1) * 512)
        xk = [("xb", s, fc) for fc in range(8)]
        t3 = t3d[s]
        p.op("sp", lambda e: e.dma_start(out=x1b[s][:], in_=X1_s[:, :, sl]), w=xk, dma=("x1b", s))
        for fc in range(8):
            b = nextbank()
            for q in range(4):
                tt = tb * 4 + q
                p.op("pe", lambda e, b=b, q=q, tt=tt, fc=fc: e.transpose(out=ps[b][:, q * 128:(q + 1) * 128], in_=yy[:, tt, fc * 128:(fc + 1) * 128], identity=ident_f[:]),
                     r=[], w=[PK(b)])
            p.op("dve", lambda e, b=b, fc=fc: e.tensor_tensor(out=x1b[s][:, fc, :], in0=ps[b][:], in1=x1b[s][:, fc, :], op=ALU.add), r=[PK(b), ("xb", s, fc)], w=[("xb", s, fc)])
        rms_stats(x1b[s][:], 8, 512, sq3[:], t1c[:], rstdc[:], xk, "C")
        for kc in range(8):
            p.op("dve", lambda e, kc=kc: e.scalar_tensor_tensor(out=t3[:, kc, :], in0=x1b[s][:, kc, :], scalar=vec[:, kc, GPLE:GPLE + 1], in1=rstdc[:], op0=ALU.mult, op1=ALU.mult),
                 r=["Crstd", ("xb", s, kc)], w=[("t3", s, kc)])

    def b3_back(tb):
        s = tb % 2
        sl = slice(tb * 512, (tb + 1) * 512)
        xk = [("xb", s, fc) for fc in range(8)]
        t3 = t3d[s]
        p.op("sp", lambda e: e.dma_start(out=pf[:], in_=pT_v[:, :, sl]), w=["pf"], dma="pf")
        p.op("pool", lambda e: e.tensor_copy(out=pb[:], in_=pf[:]), r=["pf"], w=["pb"])
        for fc in range(8):
            bg = nextbank()
            bp = nextbank()
            for kc in range(8):
                p.op("pe", lambda e, bg=bg, kc=kc, fc=fc: e.matmul(ps[bg][:], lhsT=Wpg[:, kc, fc * 128:(fc + 1) * 128], rhs=t3[:, kc, :], start=(kc == 0), stop=(kc == 7)),
                     r=[("t3", s, kc)] + wkeys(Wpg, fc * 128, 128, 64), w=[PK(bg)])
            for kc in range(2):
                p.op("pe", lambda e, bp=bp, kc=kc, fc=fc: e.matmul(ps[bp][:], lhsT=Wpp[:, kc, fc * 128:(fc + 1) * 128], rhs=pb[:, kc, :], start=(kc == 0), stop=(kc == 1)),
                     r=["pb"] + wkeys(Wpp, fc * 128, 128, 64), w=[PK(bp)])
            p.op("act", lambda e, bg=bg: e.activation(out=gte[:], in_=ps[bg][:], func=AF.Sigmoid), r=[PK(bg)], w=["gte"])
            p.op("dve", lambda e, bp=bp: e.tensor_tensor(out=gte[:], in0=ps[bp][:], in1=gte[:], op=ALU.mult), r=[PK(bp), "gte"], w=["gte"])
            p.op("pool", lambda e, fc=fc: e.tensor_tensor(out=x1b[s][:, fc, :], in0=gte[:], in1=x1b[s][:, fc, :], op=ALU.add), r=["gte", ("xb", s, fc)], w=[("xb", s, fc)])
        rms_stats(x1b[s][:], 8, 512, sq3[:], t1c[:], rstdc[:], xk, "C")
        for kc in range(8):
            p.op("dve", lambda e, kc=kc: e.scalar_tensor_tensor(out=x1b[s][:, kc, :], in0=x1b[s][:, kc, :], scalar=vec[:, kc, GFIN:GFIN + 1], in1=rstdc[:], op0=ALU.mult, op1=ALU.mult),
                 r=[("xb", s, kc), "Crstd"], w=[("xb", s, kc)])
        p.op("sp", lambda e: e.dma_start(out=outT_v[:, :, sl], in_=x1b[s][:]), r=xk, w=[("out", tb)], dma=("x1b", s))

    b3_front(0)
    for tb in range(4):
        if tb + 1 < 4:
            b3_front(tb + 1)
        b3_back(tb)
    return finish(nc, p, outT, ar)


def finish(nc, p, outT, ar):
    p.barrier()
    p.emit()
    return nc


def _mask_for(r):
    m = np.zeros((128, 16, 512), np.float32)
    ki = np.arange(128)[:, None]
    qi = np.arange(128)[None, :]
    tri = np.where(ki > qi, 0.0, 1.0).astype(np.float32)
    m[:] = 1.0
    for tau in range(16):
        for mq in range(4):
            tq = 4 * mq + r
            if tau > tq:
                m[:, tau, mq * 128:(mq + 1) * 128] = 0.0
            elif tau == tq:
                m[:, tau, mq * 128:(mq + 1) * 128] = tri
    return m.astype(ml_dtypes.bfloat16)


def _chunked(v):
    return np.ascontiguousarray(np.asarray(v, np.float32).reshape(8, 128).T)


def prepare_inputs(x, p, g_mix, w_in, lam_q1, lam_k1, lam_q2, lam_k2, g_subln, w_conv, b_conv,
                   w_rg, b_rg, w_ig, b_ig, lru_lambda, w_attn_br, w_lru_br, w_out, g_moe,
                   w_rt_group, b_rt_group, w_rt_expert, b_rt_expert, w_e_gate, w_e_up, w_e_down,
                   g_ple, w_ple_gate, w_ple_proj, g_final):
    f = lambda a: np.asarray(a, np.float32)
    x = f(x); p = f(p)
    vecs = np.zeros((128, 8, 12), np.float32)
    vecs[:, :, 0] = _chunked(f(g_mix)[0]); vecs[:, :, 1] = _chunked(f(g_moe)[0]); vecs[:, :, 2] = _chunked(f(g_ple)[0])
    vecs[:, :, 3] = _chunked(f(g_final)); vecs[:, :, 4] = _chunked(f(b_conv)[0]); vecs[:, :, 5] = _chunked(f(b_rg)[0].reshape(-1))
    vecs[:, :, 6] = _chunked(f(b_ig)[0].reshape(-1)); vecs[:, :, 7] = _chunked(f(lru_lambda)[0])
    for j in range(4):
        vecs[:, :, 8 + j] = _chunked(f(w_conv)[0, j])

    def blockdiag(w):
        w = f(w)[0]
        o = np.zeros((128, 8, 128), np.float32)
        for n in range(16):
            cc, hf = n // 2, n % 2
            o[hf * 64:(hf + 1) * 64, cc, hf * 64:(hf + 1) * 64] = w[n]
        return o

    w_rt = np.concatenate([f(w_rt_group)[0]] + [f(w_rt_expert)[0, g] for g in range(4)], axis=1)
    b_rt = np.concatenate([f(b_rt_group)[0], f(b_rt_expert)[0].reshape(-1)])[None, :]
    common = dict(
        w_in=np.ascontiguousarray(f(w_in)[0]),
        vecs=vecs,
        gsub=np.ascontiguousarray(f(g_subln)[0][:, None]),
        lamv=np.ascontiguousarray(np.broadcast_to(np.stack([f(lam_q1)[0], f(lam_k1)[0], f(lam_q2)[0], f(lam_k2)[0]])[None], (128, 4, 64))),
        brt=np.ascontiguousarray(np.broadcast_to(b_rt, (128, 36))),
        wrg_bd=blockdiag(w_rg), wig_bd=blockdiag(w_ig),
        w_attn_br=np.ascontiguousarray(f(w_attn_br)[0]), w_lru_br=np.ascontiguousarray(f(w_lru_br)[0]), w_out=np.ascontiguousarray(f(w_out)[0]),
        w_rt=np.ascontiguousarray(w_rt),
        w_e_gate=np.ascontiguousarray(f(w_e_gate)[0]), w_e_up=np.ascontiguousarray(f(w_e_up)[0]), w_e_down=np.ascontiguousarray(f(w_e_down)[0]),
        w_ple_gate=np.ascontiguousarray(f(w_ple_gate)[0]), w_ple_proj=np.ascontiguousarray(f(w_ple_proj)[0]),
    )
    in_maps = []
    toks = []
    for c in range(8):
        b, r = c // 4, c % 4
        own = (np.arange(16)[:, None] * 4 + r) * 128 + np.arange(128)[None, :]
        own = own.reshape(-1)
        toks.append((b, own))
        selv = np.zeros((128, 4), np.float32)
        selv[:, r] = 1.0
        m = dict(common)
        m.update(
            xT_full=np.ascontiguousarray(x[b].T),
            xT_own=np.ascontiguousarray(x[b][own].T),
            pT_own=np.ascontiguousarray(p[0, b][own].T),
            maskB=_mask_for(r),
            sel=selv,
        )
        in_maps.append(m)
    return in_maps, toks


_NC_CACHE = {}


def kernel(**inputs):
    in_maps, toks = prepare_inputs(**inputs)
    if "nc" not in _NC_CACHE:
        _NC_CACHE["nc"] = build_program()
    nc = _NC_CACHE["nc"]
    res = run_bass_kernel_spmd(nc, in_maps, core_ids=list(range(8)))
    out = np.zeros((2, S, D), np.float32)
    for c in range(8):
        b, own = toks[c]
        out[b, own, :] = res.results[c]["outT"].T
    if DEBUG:
        kernel.last = res.results
    return out
```
